# Optimizing a Trainium2 kernel written in Bass

```python
import jax, jax.numpy as jnp
from jax import lax
import numpy as np

D_MODEL = 1024
BATCH = 8
SEQ = 2048
DEPTH = 4

N_MIXERS = 4
EPS = 1e-6
CONV_WIDTH = 31
HGRN_EXPAND = 128
HGRN_HEADS = D_MODEL // HGRN_EXPAND
HGRN_CHUNK = 64
POOL_WINDOWS = (2, 4, 8, 16)
POOL_GROUPS = len(POOL_WINDOWS)
POOL_DIM = D_MODEL // POOL_GROUPS
SB_HEADS = 16
SB_HEAD_DIM = D_MODEL // SB_HEADS
SB_BLOCK = 128
MOE_GROUPS = 4
MOE_EXPERTS_PER_GROUP = 8
MOE_EXPERTS = MOE_GROUPS * MOE_EXPERTS_PER_GROUP
MOE_TOP_K = 2
MOE_HIDDEN = D_MODEL // 2
MOE_BLOCK = 128

kernel_name = "hybrid_interleaved_conv_hgrn2_pool_stickbreak_hmoe"


def _n_layers_of(m):
    return (DEPTH - m + N_MIXERS - 1) // N_MIXERS


def rmsnorm(x, g):
    xf = x.astype(jnp.float32)
    y = xf * lax.rsqrt(jnp.mean(xf * xf, axis=-1, keepdims=True) + EPS) * g.astype(jnp.float32)
    return y.astype(x.dtype)


def layernorm(x, g, b):
    xf = x.astype(jnp.float32)
    mu = jnp.mean(xf, axis=-1, keepdims=True)
    xc = xf - mu
    y = xc * lax.rsqrt(jnp.mean(xc * xc, axis=-1, keepdims=True) + EPS) * g + b
    return y.astype(x.dtype)


def conformer_conv(h, w_in, b_in, w_dw, b_dw, ln_g, ln_b, w_out):
    u = h @ w_in + b_in
    a, gt = jnp.split(u, 2, axis=-1)
    u = a * jax.nn.sigmoid(gt)
    y = lax.conv_general_dilated(
        u, w_dw[:, None, :], window_strides=(1,),
        padding=[(CONV_WIDTH - 1, 0)],
        dimension_numbers=('NWC', 'WIO', 'NWC'),
        feature_group_count=D_MODEL) + b_dw
    y = jax.nn.silu(layernorm(y, ln_g, ln_b))
    return (y @ w_out).astype(h.dtype)


def hgrn_lower_bounds(logits):
    p = jax.nn.softmax(logits.astype(jnp.float32), axis=0)
    cum = jnp.cumsum(p, axis=0)
    return cum - cum[0:1]


def hgrn2(h, w_in, lb, norm_g, w_out):
    B, S, _ = h.shape
    H, dh, C = HGRN_HEADS, HGRN_EXPAND, HGRN_CHUNK
    n_chunk = S // C
    q, f, i_in, g = jnp.split((h @ w_in).astype(jnp.float32), 4, axis=-1)
    q = jax.nn.silu(q)
    forget = lb + (1.0 - lb) * jax.nn.sigmoid(f)
    log_f = jnp.log(forget)
    k = (1.0 - lb) * jax.nn.sigmoid(-f)

    def chunks(t):
        return t.reshape(B, n_chunk, C, H, dh).transpose(1, 0, 3, 2, 4)

    causal = jnp.tril(jnp.ones((C, C), bool))[:, :, None]

    def step(state, inp):
        qc, kc, vc, lfc = inp
        b = jnp.cumsum(lfc, axis=2)
        o_inter = jnp.einsum('bhtk,bhkv->bhtv', qc * jnp.exp(b), state)
        rel = jnp.where(causal, b[:, :, :, None, :] - b[:, :, None, :, :], -jnp.inf)
        scores = jnp.einsum('bhtk,bhsk,bhtsk->bhts', qc, kc, jnp.exp(rel))
        o_intra = jnp.einsum('bhts,bhsv->bhtv', scores, vc)
        b_end = b[:, :, -1:, :]
        state = (jnp.exp(b_end[:, :, 0, :, None]) * state
                 + jnp.einsum('bhsk,bhsv->bhkv', kc * jnp.exp(b_end - b), vc))
        return state, o_inter + o_intra

    state0 = jnp.zeros((B, H, dh, dh), jnp.float32)
    _, o = lax.scan(step, state0, (chunks(q), chunks(k), chunks(i_in), chunks(log_f)))
    o = o.transpose(1, 0, 3, 2, 4).reshape(B, S, H, dh)
    o = o * lax.rsqrt(jnp.mean(o * o, axis=-1, keepdims=True) + EPS) * norm_g.reshape(H, dh)
    o = o.reshape(B, S, D_MODEL) * jax.nn.silu(g)
    return (o.astype(h.dtype) @ w_out).astype(h.dtype)


def multiscale_pool(h, w_grp, b_grp, ls):
    B, S, _ = h.shape
    hf = h.astype(jnp.float32).reshape(B, S, POOL_GROUPS, POOL_DIM)
    cs0 = jnp.pad(jnp.cumsum(hf, axis=1), ((0, 0), (1, 0), (0, 0), (0, 0)))
    pos = jnp.arange(S)
    outs = []
    for gi, w in enumerate(POOL_WINDOWS):
        c_g = cs0[:, :, gi]
        upper = c_g[:, 1:]
        lower = jnp.pad(c_g, ((0, 0), (w - 1, 0), (0, 0)))[:, :S]
        count = jnp.minimum(pos + 1, w).astype(jnp.float32)[None, :, None]
        outs.append((upper - lower) / count - hf[:, :, gi])
    pooled = jnp.stack(outs, axis=2)
    y = jnp.einsum('bsgc,gcd->bsgd', pooled, w_grp.astype(jnp.float32))
    y = y.reshape(B, S, D_MODEL) + b_grp
    return (y * ls).astype(h.dtype)


def stick_breaking_attention(h, w_qkv, w_out):
    B, S, _ = h.shape
    qkv = (h @ w_qkv).reshape(B, S, 3, SB_HEADS, SB_HEAD_DIM)
    q = qkv[:, :, 0].transpose(0, 2, 1, 3)
    k = qkv[:, :, 1].transpose(0, 2, 1, 3)
    v = qkv[:, :, 2].transpose(0, 2, 1, 3)
    n_blk = S // SB_BLOCK
    q_blocks = q.reshape(B, SB_HEADS, n_blk, SB_BLOCK, SB_HEAD_DIM).transpose(2, 0, 1, 3, 4)
    key_pos = jnp.arange(S)
    scale = SB_HEAD_DIM ** -0.5

    def one_block(args):
        qb, bi = args
        z = jnp.einsum('bhtd,bhsd->bhts', qb, k).astype(jnp.float32) * scale
        q_pos = bi * SB_BLOCK + jnp.arange(SB_BLOCK)
        strict = key_pos[None, :] < q_pos[:, None]
        log_keep = jnp.where(strict, jax.nn.log_sigmoid(-z), 0.0)
        later = lax.cumsum(log_keep, axis=3, reverse=True) - log_keep
        weights = jnp.where(strict, jnp.exp(jax.nn.log_sigmoid(z) + later), 0.0)
        return jnp.einsum('bhts,bhsd->bhtd', weights.astype(v.dtype), v)

    o = lax.map(one_block, (q_blocks, jnp.arange(n_blk)))
    o = o.transpose(1, 0, 3, 2, 4).reshape(B, S, D_MODEL)
    return (o @ w_out).astype(h.dtype)


def hier_moe(h, w_rg, b_rg, w_re, b_re, w_gate, w_up, w_down):
    B, S, D = h.shape
    T = B * S
    xt = h.reshape(T, D)
    grp_logits = (xt @ w_rg + b_rg).astype(jnp.float32)
    grp = jnp.argmax(grp_logits, axis=-1)
    grp_p = jnp.take_along_axis(jax.nn.softmax(grp_logits, axis=-1), grp[:, None], axis=1)[:, 0]
    exp_logits = (xt @ w_re + b_re).astype(jnp.float32).reshape(T, MOE_GROUPS, MOE_EXPERTS_PER_GROUP)
    in_grp = jnp.take_along_axis(exp_logits, grp[:, None, None], axis=1)[:, 0]
    top_val, top_idx = lax.top_k(in_grp, MOE_TOP_K)
    gate = grp_p[:, None] * jax.nn.softmax(top_val, axis=-1)
    expert = grp[:, None] * MOE_EXPERTS_PER_GROUP + top_idx

    n_assign = T * MOE_TOP_K
    e_flat = expert.reshape(n_assign).astype(jnp.int32)
    g_flat = gate.reshape(n_assign)
    t_flat = jnp.repeat(jnp.arange(T, dtype=jnp.int32), MOE_TOP_K)
    order = jnp.argsort(e_flat)
    e_s, t_s, g_s = e_flat[order], t_flat[order], g_flat[order]
    counts = jnp.bincount(e_flat, length=MOE_EXPERTS).astype(jnp.int32)
    padded = ((counts + MOE_BLOCK - 1) // MOE_BLOCK) * MOE_BLOCK
    seg_start = jnp.cumsum(counts) - counts
    pad_end = jnp.cumsum(padded)
    pad_start = pad_end - padded
    dest = pad_start[e_s] + (jnp.arange(n_assign, dtype=jnp.int32) - seg_start[e_s])
    n_slots = n_assign + MOE_EXPERTS * MOE_BLOCK
    n_blk = n_slots // MOE_BLOCK
    slot_tok = jnp.full((n_slots,), T, jnp.int32).at[dest].set(t_s)
    slot_gate = jnp.zeros((n_slots,), jnp.float32).at[dest].set(g_s)
    blk_start = jnp.arange(n_blk, dtype=jnp.int32) * MOE_BLOCK
    blk_expert = jnp.minimum(jnp.searchsorted(pad_end, blk_start, side='right'),
                             MOE_EXPERTS - 1).astype(jnp.int32)
    x_pad = jnp.concatenate([xt, jnp.zeros((1, D), xt.dtype)], axis=0)
    xs = x_pad[slot_tok].reshape(n_blk, MOE_BLOCK, D)

    def run_block(args):
        xb, e = args
        hid = jax.nn.silu(xb @ w_gate[e]) * (xb @ w_up[e])
        return hid @ w_down[e]

    ys = lax.map(run_block, (xs, blk_expert)).reshape(n_slots, D)
    out = jnp.zeros((T + 1, D), jnp.float32).at[slot_tok].add(
        ys.astype(jnp.float32) * slot_gate[:, None])[:T]
    return out.reshape(B, S, D).astype(h.dtype)


def setup_inputs(seed: int = 0) -> dict:
    key = jax.random.key(seed)
    ks = iter(jax.random.split(key, 40))

    def nrm(shape, scale):
        return jax.random.normal(next(ks), shape, jnp.float32) * scale

    D = D_MODEL
    NA, NB, NC, ND = (_n_layers_of(m) for m in range(N_MIXERS))
    inv = D ** -0.5
    E, F = MOE_EXPERTS, MOE_HIDDEN
    return {
        "x": nrm((BATCH, SEQ, D), 1.0),
        "c": nrm((BATCH, D), 1.0),
        "ada_w": nrm((DEPTH, D, 6 * D), 0.5 * inv),
        "ada_b": nrm((DEPTH, 6 * D), 0.02),
        "norm_mix_g": 1.0 + nrm((DEPTH, D), 0.05),
        "norm_ffn_g": 1.0 + nrm((DEPTH, D), 0.05),
        "final_g": 1.0 + nrm((D,), 0.05),
        "conv_w_in": nrm((NA, D, 2 * D), inv),
        "conv_b_in": nrm((NA, 2 * D), 0.02),
        "conv_w_dw": nrm((NA, CONV_WIDTH, D), CONV_WIDTH ** -0.5),
        "conv_b_dw": nrm((NA, D), 0.02),
        "conv_ln_g": 1.0 + nrm((NA, D), 0.05),
        "conv_ln_b": nrm((NA, D), 0.02),
        "conv_w_out": nrm((NA, D, D), inv),
        "hgrn_w_in": nrm((NB, D, 4 * D), inv),
        "hgrn_lb_logits": nrm((DEPTH, D), 0.5),
        "hgrn_norm_g": 1.0 + nrm((NB, D), 0.05),
        "hgrn_w_out": nrm((NB, D, D), inv),
        "pool_w": nrm((NC, POOL_GROUPS, POOL_DIM, POOL_DIM), POOL_DIM ** -0.5),
        "pool_b": nrm((NC, D), 0.02),
        "pool_scale": 1.0 + nrm((NC, D), 0.1),
        "sb_w_qkv": nrm((ND, D, 3 * D), inv),
        "sb_w_out": nrm((ND, D, D), inv),
        "moe_w_rg": nrm((DEPTH, D, MOE_GROUPS), inv),
        "moe_b_rg": nrm((DEPTH, MOE_GROUPS), 0.01),
        "moe_w_re": nrm((DEPTH, D, E), inv),
        "moe_b_re": nrm((DEPTH, E), 0.01),
        "moe_w_gate": nrm((DEPTH, E, D, F), inv),
        "moe_w_up": nrm((DEPTH, E, D, F), inv),
        "moe_w_down": nrm((DEPTH, E, F, D), F ** -0.5),
    }


def reference(x, c, ada_w, ada_b, norm_mix_g, norm_ffn_g, final_g,
              conv_w_in, conv_b_in, conv_w_dw, conv_b_dw, conv_ln_g, conv_ln_b, conv_w_out,
              hgrn_w_in, hgrn_lb_logits, hgrn_norm_g, hgrn_w_out,
              pool_w, pool_b, pool_scale,
              sb_w_qkv, sb_w_out,
              moe_w_rg, moe_b_rg, moe_w_re, moe_b_re, moe_w_gate, moe_w_up, moe_w_down):
    c_act = jax.nn.silu(c)
    lb_all = hgrn_lower_bounds(hgrn_lb_logits)
    for i in range(DEPTH):
        m, j = i % N_MIXERS, i // N_MIXERS
        mod = c_act @ ada_w[i] + ada_b[i]
        sh1, sc1, g1, sh2, sc2, g2 = [t[:, None, :] for t in jnp.split(mod, 6, axis=-1)]
        h = rmsnorm(x, norm_mix_g[i]) * (1 + sc1) + sh1
        if m == 0:
            y = conformer_conv(h, conv_w_in[j], conv_b_in[j], conv_w_dw[j], conv_b_dw[j],
                               conv_ln_g[j], conv_ln_b[j], conv_w_out[j])
        elif m == 1:
            y = hgrn2(h, hgrn_w_in[j], lb_all[i], hgrn_norm_g[j], hgrn_w_out[j])
        elif m == 2:
            y = multiscale_pool(h, pool_w[j], pool_b[j], pool_scale[j])
        else:
            y = stick_breaking_attention(h, sb_w_qkv[j], sb_w_out[j])
        x = x + g1 * y
        h = rmsnorm(x, norm_ffn_g[i]) * (1 + sc2) + sh2
        x = x + g2 * hier_moe(h, moe_w_rg[i], moe_b_rg[i], moe_w_re[i], moe_b_re[i],
                              moe_w_gate[i], moe_w_up[i], moe_w_down[i])
    return rmsnorm(x, final_g)
```

```python
import numpy as np
from contextlib import ExitStack
import concourse.bass as bass
import concourse.mybir as mybir
from concourse.bass_utils import run_bass_kernel_spmd

F32 = mybir.dt.float32
BF16 = mybir.dt.bfloat16
AF = mybir.ActivationFunctionType
ALU = mybir.AluOpType
AX = mybir.AxisListType

D = 1024
S = 2048
KC = 8
DEPTH = 4
EPS = 1e-6
NEXP = 32
FH = 512
L0 = 98304
M0 = L0 + 24576
ARENA_BYTES = 204800
BIG = 1.0e30
NSTEP = 48
NSUB = 2 * NSTEP
NSLOT = NSTEP * 256
NCB = 84
I32 = mybir.dt.int32

ENGS = ("pe", "act", "dve", "pool", "sp")


class Slot:
    __slots__ = ("sem", "cnt", "eng")

    def __init__(self, sem):
        self.sem = sem
        self.cnt = 0
        self.eng = None


class Res:
    __slots__ = ("name", "w", "r", "slot")

    def __init__(self, name):
        self.name = name
        self.w = None
        self.r = {}
        self.slot = None


class Prog:
    def __init__(self, nc, es):
        self.nc = nc
        self.es = es
        self.q = {e: [] for e in ENGS}
        self.cnt = {e: 0 for e in ENGS}
        self.seen = {e: {} for e in ENGS}
        self.esem = {e: es.enter_context(nc.semaphore("sem_" + e)) for e in ("pe", "act", "dve", "pool")}
        self.slots = []
        self.free = {}
        self.live = []
        self.regs = {}

    def res(self, name):
        return Res(name)

    def _sem_of(self, key):
        return self.esem[key] if isinstance(key, str) else key.sem

    def _wait(self, e, toks, strict=False):
        need = {}
        for t in toks:
            if t is None:
                continue
            k, v = t
            if (not strict) and k == e and e == "pe":
                continue
            if need.get(k, 0) < v:
                need[k] = v
        for k, v in need.items():
            if self.seen[e].get(k, 0) >= v:
                continue
            self.seen[e][k] = v
            sem = self._sem_of(k)
            self.q[e].append(lambda eng, sem=sem, v=v: eng.wait_ge(sem, v))

    def _collect(self, r, w):
        toks = []
        for x in r:
            toks.append(x.w)
        for x in w:
            toks.append(x.w)
            toks.extend(x.r.items())
        return toks

    def op(self, e, fns, r=(), w=()):
        if callable(fns):
            fns = [fns]
        self._wait(e, self._collect(r, w))
        self.cnt[e] += 1
        n = self.cnt[e]
        sem = self.esem[e]
        last = len(fns) - 1
        for i, fn in enumerate(fns):
            if i == last:
                self.q[e].append(lambda eng, fn=fn, sem=sem: fn(eng).then_inc(sem, 1))
            else:
                self.q[e].append(fn)
        tok = (e, n)
        for x in r:
            if x.r.get(e, 0) < n:
                x.r[e] = n
        for x in w:
            x.w = tok
            x.r = {}
        return tok

    def dma(self, e, out, in_, r=(), w=(), dres=None, nowait_w=()):
        return self.dmaf(e, lambda eng, out=out, in_=in_: eng.dma_start(out=out, in_=in_), r, w, dres, nowait_w)

    def dmaf(self, e, fn, r=(), w=(), dres=None, nowait_w=()):
        if dres is None:
            dres = w[0]
        if dres.slot is None:
            fl = self.free.setdefault(e, [])
            if fl:
                dres.slot = fl.pop()
            else:
                dres.slot = Slot(self.es.enter_context(self.nc.semaphore("dsem_%s_%d" % (e, len(self.slots)))))
                dres.slot.eng = e
                self.slots.append(dres.slot)
            self.live.append(dres)
        slot = dres.slot
        assert slot.eng == e, (dres.name, slot.eng, e)
        self._wait(e, self._collect(r, w), strict=True)
        slot.cnt += 16
        v = slot.cnt
        sem = slot.sem
        self.q[e].append(lambda eng, fn=fn, sem=sem: fn(eng).then_inc(sem, 16))
        tok = (slot, v)
        for x in r:
            x.r[slot] = v
        for x in w:
            x.w = tok
            x.r = {}
        for x in nowait_w:
            x.w = tok
        return tok

    def barrier(self):
        toks = [(e, self.cnt[e]) for e in ("pe", "act", "dve", "pool") if self.cnt[e] > 0]
        toks += [(d, d.cnt) for d in self.slots if d.cnt > 0]
        for e in ENGS:
            self._wait(e, toks)
        for d in self.live:
            self.free.setdefault(d.slot.eng, []).append(d.slot)
            d.slot = None
        self.live = []

    def replay(self, block):
        def mk(name):
            def f(eng):
                for fn in self.q[name]:
                    fn(eng)
            return f
        block.tensor(mk("pe"))
        block.scalar(mk("act"))
        block.vector(mk("dve"))
        block.gpsimd(mk("pool"))
        block.sync(mk("sp"))


def MM(o, l, r, st=True, sp=True):
    return lambda e: e.matmul(o, lhsT=l, rhs=r, start=st, stop=sp)


def TR(o, i, ident):
    return lambda e: e.transpose(o, i, ident)


def ACT(o, i, f, bias=None, scale=None):
    kw = {}
    if bias is not None:
        kw["bias"] = bias
    if scale is not None:
        kw["scale"] = scale
    return lambda e: e.activation(out=o, in_=i, func=f, **kw)


def TS(o, i, s1, s2, op0, op1=None):
    if op1 is None:
        return lambda e: e.tensor_scalar(out=o, in0=i, scalar1=s1, scalar2=None, op0=op0)
    return lambda e: e.tensor_scalar(out=o, in0=i, scalar1=s1, scalar2=s2, op0=op0, op1=op1)


def TT(o, a, b, op):
    return lambda e: e.tensor_tensor(out=o, in0=a, in1=b, op=op)


def STT(o, a, s, b, op0, op1):
    return lambda e: e.scalar_tensor_tensor(out=o, in0=a, scalar=s, in1=b, op0=op0, op1=op1)


def CP(o, i):
    return lambda e: e.tensor_copy(out=o, in_=i)


def MS(o, v):
    return lambda e: e.memset(o, v)


def _vec_layout():
    off = {}
    n = 0

    def add(name, cols):
        nonlocal n
        off[name] = n
        n += cols
    for i in range(DEPTH):
        add("gmix%d" % i, 8)
        add("gffn%d" % i, 8)
        add("adab%d" % i, 48)
        add("lbl%d" % i, 8)
    add("fin", 8)
    add("cbin", 16)
    add("cbdw", 8)
    add("clng", 8)
    add("clnb", 8)
    add("hng", 8)
    add("pb", 8)
    add("psc", 8)
    add("wdw", 248)
    add("c", 8)
    return off, n


VOFF, NVEC = _vec_layout()


def _fm(v):
    v = np.asarray(v, np.float32).reshape(-1, 128)
    return np.ascontiguousarray(v.T)


def build_program(phases, dbg=False):
    nc = bass.Bass("TRN2", target_bir_lowering=False)
    dt_ = nc.dram_tensor
    xT_d = dt_("xT", [128, KC, S], F32, kind="ExternalInput").ap()
    vec_d = dt_("vecs", [128, NVEC], F32, kind="ExternalInput").ap()
    br_d = dt_("br", [128, DEPTH * 36], F32, kind="ExternalInput").ap()
    wr_d = dt_("wr", [128, DEPTH, KC, 36], F32, kind="ExternalInput").ap()
    cA_d = dt_("cA", [128, 384], F32, kind="ExternalInput").ap()
    sbm_d = dt_("sbmask", [128, 2048], F32, kind="ExternalInput").ap()
    scm_d = dt_("scanmask", [128, 2048], F32, kind="ExternalInput").ap()
    invc_d = dt_("invc", [128, 64], F32, kind="ExternalInput").ap()
    cB_d = dt_("cB", [128, NCB], F32, kind="ExternalInput").ap()
    adaw_d = dt_("ada_w", [DEPTH, D, 6 * D], F32, kind="ExternalInput").ap()
    cwin_d = dt_("conv_w_in", [D, 2 * D], F32, kind="ExternalInput").ap()
    cwout_d = dt_("conv_w_out", [D, D], F32, kind="ExternalInput").ap()
    hwin_d = dt_("hgrn_w_in", [D, 4 * D], F32, kind="ExternalInput").ap()
    hwout_d = dt_("hgrn_w_out", [D, D], F32, kind="ExternalInput").ap()
    pw_d = dt_("pool_w", [4, 256, 256], F32, kind="ExternalInput").ap()
    sqkv_d = dt_("sb_w_qkv", [D, 3 * D], F32, kind="ExternalInput").ap()
    swout_d = dt_("sb_w_out", [D, D], F32, kind="ExternalInput").ap()
    wgL_d = dt_("wgL", [DEPTH * NEXP * 128, 4096], F32, kind="ExternalInput").ap()
    wuL_d = dt_("wuL", [DEPTH * NEXP * 128, 4096], F32, kind="ExternalInput").ap()
    wdL_d = dt_("wdL", [DEPTH * NEXP * 128, 4096], F32, kind="ExternalInput").ap()
    xs_d = dt_("xs_scr", [NSLOT, D], BF16, kind="Internal").ap()
    ys_d = dt_("ys_scr", [NSLOT, D], BF16, kind="Internal").ap()
    out_d = dt_("outT", [128, KC, S], F32, kind="ExternalOutput").ap()

    es = ExitStack()
    with es:
        arena = es.enter_context(nc.sbuf_tensor("arena", [128, ARENA_BYTES // 2], BF16))

        def carve(off, n, dt=BF16, parts=128):
            if dt == BF16:
                a = arena[0:parts, off // 2: off // 2 + n]
            else:
                a = arena[0:parts, off // 2: off // 2 + 2 * n].bitcast(F32)
            return a

        def v3(ap, a):
            return ap.rearrange("p (a b) -> p a b", a=a)

        sb = lambda name, shape, dt: es.enter_context(nc.sbuf_tensor(name, shape, dt))
        VEC = sb("VEC", [128, NVEC], F32)
        BRB = sb("BRB", [128, DEPTH * 36], F32)
        MOD = sb("MOD", [128, DEPTH * 48], F32)
        DER = sb("DER", [128, DEPTH * 16 + 32], F32)
        CST = sb("CST", [128, 384], F32)
        IDB = sb("IDB", [128, 128], BF16)
        ONESB = sb("ONESB", [128, 128], BF16)
        TRIB = sb("TRIB", [128, 128], BF16)
        CA = sb("CA", [128, 8], F32)
        SMALL = sb("SMALL", [128, 64], F32)
        CSTB = sb("CSTB", [128, NCB], F32)
        DIDX = sb("DIDX", [128, 32], I32)
        WIDX = sb("WIDX", [128, NSTEP], I32)
        GATE = sb("GATE", [128, 32], F32)
        NEGM = sb("NEGM", [128, 128], BF16)
        TH16 = CSTB[:, 0:16]
        BIDX = CSTB[:, 16:80]
        PIDX = CSTB[:, 80:81]
        banks = [es.enter_context(nc.psum_tensor("bank%d" % i, [128, 512], F32)) for i in range(8)]
        IDF = CST[:, 0:128]
        MASK2 = CST[:, 256:384]

        P = Prog(nc, es)
        bres = [P.res("bank%d" % i) for i in range(8)]
        bptr = [0]

        def psum():
            i = bptr[0]
            bptr[0] = (i + 1) % 8
            return banks[i], bres[i]

        XT = v3(carve(0, KC * S, F32), KC)
        HT = v3(carve(65536, KC * S, BF16), KC)
        rXT = [P.res("XT%d" % i) for i in range(4)]
        rHT = [P.res("HT%d" % i) for i in range(4)]
        rC = P.res("consts")
        rVEC = P.res("VEC")
        rMOD = P.res("MOD")

        def V(name, c0=0, n=1):
            o = VOFF[name] + c0
            return VEC[:, o:o + n]

        P.dma("sp", VEC[:], vec_d, w=[rVEC])
        P.dma("sp", BRB[:], br_d, w=[rC])
        P.dma("sp", CST[:], cA_d, w=[rC])
        P.dma("sp", CSTB[:], cB_d, w=[rC])

        def _mk_regs(eng):
            P.regs["wb"] = eng.alloc_register("wbound")
            eng.reg_mov(P.regs["wb"], DEPTH * NEXP * 128 - 1)
            P.regs["xb"] = eng.alloc_register("xbound")
            eng.reg_mov(P.regs["xb"], NSLOT - 1)
        P.q["pool"].append(_mk_regs)
        for i in range(4):
            P.dma("sp", XT[:, :, i * 512:(i + 1) * 512], xT_d[:, :, i * 512:(i + 1) * 512], w=[rXT[i]])
        HTflat = carve(65536, KC * S, BF16)
        rZ = P.res("Z")
        P.op("pool", MS(HTflat, 0.0), w=rHT)
        for j in range(NSLOT // 2048):
            P.dma("sp", xs_d[j * 2048:(j + 1) * 2048, :].rearrange("(p a) n -> p (a n)", p=128), HTflat,
                  r=rHT, w=[], dres=rZ, nowait_w=[rZ])
        P.op("dve", CP(IDB[:], CST[:, 0:128]), r=[rC], w=[rC])
        P.op("dve", CP(TRIB[:], CST[:, 128:256]), r=[rC], w=[rC])
        P.op("dve", MS(ONESB[:], 1.0), w=[rC])
        P.op("act", ACT(CA[:], V("c", 0, 8), AF.Silu), r=[rVEC], w=[rC])

        NAS = 2
        adaw_slots = [v3(carve(M0 + s_ * 16384, KC * 512, F32), KC) for s_ in range(NAS)]
        r_adaw = [P.res("adaw%d" % s_) for s_ in range(NAS)]
        ROW = carve(M0 + 32768, 6 * D, F32, parts=1)
        CAb = CA
        rROW = P.res("ROW")
        P.op("dve", MS(SMALL[:, 0:1], EPS), w=[rC])
        P.op("dve", MS(SMALL[:, 1:2], 1.0), w=[rC])
        if "mods" in phases:
            for i in range(DEPTH):
                for blk in range(12):
                    sl = (i * 12 + blk) % NAS
                    P.dma("sp", adaw_slots[sl][:],
                          adaw_d[i, :, blk * 512:(blk + 1) * 512].rearrange("(k p) n -> p k n", p=128),
                          w=[r_adaw[sl]])
                    ps, pr = psum()
                    P.op("pe", [MM(ps[0:1, :], CAb[:, k:k + 1], adaw_slots[sl][:, k, :], st=(k == 0), sp=(k == KC - 1))
                                for k in range(KC)], r=[r_adaw[sl], rC], w=[pr])
                    P.op("act", ACT(ROW[0:1, blk * 512:(blk + 1) * 512], ps[0:1, :], AF.Copy), r=[pr], w=[rROW])
                ps, pr = psum()
                P.op("pe", [MM(ps[:, j:j + 1], ROW[0:1, j * 128:(j + 1) * 128], SMALL[0:1, 1:2]) for j in range(48)],
                     r=[rROW, rC], w=[pr])
                P.op("dve", TT(MOD[:, i * 48:(i + 1) * 48], ps[:, 0:48], V("adab%d" % i, 0, 48), ALU.add),
                     r=[pr, rVEC], w=[rMOD])
                P.op("dve", STT(DER[:, i * 16:i * 16 + 8], MOD[:, i * 48 + 8:i * 48 + 16], 1.0, V("gmix%d" % i, 0, 8),
                                ALU.add, ALU.mult), r=[rMOD, rVEC], w=[rMOD])
                P.op("dve", STT(DER[:, i * 16 + 8:i * 16 + 16], MOD[:, i * 48 + 32:i * 48 + 40], 1.0,
                                V("gffn%d" % i, 0, 8), ALU.add, ALU.mult), r=[rMOD, rVEC], w=[rMOD])
        P.barrier()

        def modv(i, which, c):
            o = i * 48 + which * 8 + c
            return MOD[:, o:o + 1]

        def norm_phase(A_of, B_of, out_mode, layer=0):
            base = M0
            SQ = v3(carve(base, KC * 256, BF16), KC)
            TMPs = [v3(carve(base + 4096 + s_ * 8192, KC * 256, F32), KC) for s_ in range(2)]
            RSTDs = [carve(base + 4096 + 16384 + s_ * 1024, 256, F32) for s_ in range(2)]
            rSQ = P.res("SQ")
            rTMP = [P.res("TMP0"), P.res("TMP1")]
            rRS = [P.res("RS0"), P.res("RS1")]
            rLOG = P.res("LOG")
            LOG = None
            if out_mode == "ffn":
                LOG = v3(carve(base + 24576, 16 * 36, F32), 16)
                WR = v3(carve(base + 24576 + 4096, KC * 36, F32), KC)
                rWR = P.res("WR")
                P.dma("sp", WR[:], wr_d[:, layer, :, :], w=[rWR])
            rOUT = [P.res("OUT0"), P.res("OUT1")]
            def n1a(tt):
                sl = slice(tt * 256, (tt + 1) * 256)
                xt_r = rXT[tt // 2]
                P.op("act", ACT(SQ[:], XT[:, :, sl], AF.Square), r=[xt_r], w=[rSQ])
                ps, pr = psum()
                P.op("pe", [MM(ps[:, 0:256], ONESB[:], SQ[:, c, :], st=(c == 0), sp=(c == KC - 1)) for c in range(KC)],
                     r=[rSQ, rC], w=[pr])
                return ps, pr

            def n1b(tt, ps, pr):
                sl = slice(tt * 256, (tt + 1) * 256)
                xt_r = rXT[tt // 2]
                s_ = tt % 2
                P.op("act", ACT(RSTDs[s_], ps[:, 0:256], AF.Sqrt, bias=SMALL[:, 0:1], scale=1.0 / D), r=[pr, rC], w=[rRS[s_]])
                P.op("dve", lambda e, o=RSTDs[s_]: e.reciprocal(out=o, in_=o), r=[rRS[s_]], w=[rRS[s_]])
                for c in range(KC):
                    P.op("dve", STT(TMPs[s_][:, c, :], XT[:, c, sl], A_of(c), RSTDs[s_], ALU.mult, ALU.mult),
                         r=[xt_r, rRS[s_], rMOD], w=[rTMP[s_]])

            def n2(tt):
                sl = slice(tt * 256, (tt + 1) * 256)
                s_ = tt % 2
                if out_mode == "mix":
                    for c in range(KC):
                        P.op("act", ACT(HT[:, c, sl], TMPs[s_][:, c, :], AF.Identity, bias=B_of(c), scale=1.0),
                             r=[rTMP[s_], rMOD], w=[rHT[tt // 2]])
                elif out_mode == "ffn":
                    for c in range(KC):
                        P.op("act", ACT(TMPs[s_][:, c, :], TMPs[s_][:, c, :], AF.Identity, bias=B_of(c), scale=1.0),
                             r=[rMOD], w=[rTMP[s_]])
                    P.op("pool", CP(HT[:, :, sl], TMPs[s_][:]), r=[rTMP[s_]], w=[rHT[tt // 2]])
                    for sub in range(2):
                        ps2, pr2 = psum()
                        P.op("pe", [MM(ps2[:, 0:36], TMPs[s_][:, k, sub * 128:(sub + 1) * 128], WR[:, k, :],
                                       st=(k == 0), sp=(k == KC - 1)) for k in range(KC)],
                             r=[rTMP[s_], rWR], w=[pr2])
                        P.op("dve", TT(LOG[:, tt * 2 + sub, :], ps2[:, 0:36], BRB[:, layer * 36:(layer + 1) * 36], ALU.add),
                             r=[pr2, rC], w=[rLOG])
                else:
                    P.dma("sp", out_d[:, :, sl], TMPs[s_][:], r=[rTMP[s_]], w=[], dres=rOUT[s_])

            pend = n1a(0)
            n1b(0, *pend)
            for tt in range(8):
                if tt + 1 < 8:
                    pend = n1a(tt + 1)
                n2(tt)
                if tt + 1 < 8:
                    n1b(tt + 1, *pend)
            return LOG, rLOG, rOUT

        def moe_phase(layer, LOG, rLOG):
            base = M0
            rb = base + 32768
            nR = [0]

            def rt(n):
                a = carve(rb + nR[0], n, F32)
                nR[0] += n * 4
                return a
            rR = P.res("route")
            GL = LOG[:, :, 0:4]
            EL = LOG[:, :, 4:36]
            GMAX = rt(16)
            GSH = v3(rt(64), 16)
            OHG = v3(rt(64), 16)
            GE = v3(rt(64), 16)
            GSUM = rt(16)
            GP = rt(16)
            PEN = v3(rt(64), 16)
            ELM = v3(rt(512), 16)
            V1 = rt(16)
            D1 = v3(rt(512), 16)
            OH1 = v3(rt(512), 16)
            ELM2 = v3(rt(512), 16)
            V2 = rt(16)
            OH2 = v3(rt(512), 16)
            DV = rt(16)
            E21 = rt(16)
            P1 = rt(16)
            C1 = rt(16)
            C2 = rt(16)

            def bc3(a, n):
                return a.unsqueeze(2).to_broadcast([128, 16, n])

            def dv(fn, r=(), w=()):
                P.op("dve", fn, r=list(r) + [rLOG, rR], w=list(w) + [rR])
            dv(lambda e: e.tensor_reduce(out=GMAX, in_=GL, axis=AX.X, op=ALU.max))
            dv(TT(GSH[:], GL, bc3(GMAX, 4), ALU.subtract))
            dv(TS(OHG[:], GSH[:], 0.0, None, ALU.is_equal))
            P.op("act", ACT(GE[:], GSH[:], AF.Exp), r=[rR], w=[rR])
            dv(lambda e: e.tensor_reduce(out=GSUM, in_=GE[:], axis=AX.X, op=ALU.add))
            dv(lambda e: e.reciprocal(out=GP, in_=GSUM))
            dv(TS(PEN[:], OHG[:], -1.0, BIG, ALU.add, ALU.mult))
            ELM4 = ELM[:].rearrange("p a (g x) -> p a g x", g=4)
            EL4 = EL.rearrange("p a (g x) -> p a g x", g=4)
            dv(TT(ELM4, EL4, PEN[:].unsqueeze(3).to_broadcast([128, 16, 4, 8]), ALU.add))
            dv(lambda e: e.tensor_reduce(out=V1, in_=ELM[:], axis=AX.X, op=ALU.max))
            dv(TT(D1[:], ELM[:], bc3(V1, 32), ALU.subtract))
            dv(TS(OH1[:], D1[:], 0.0, None, ALU.is_equal))
            dv(STT(ELM2[:], OH1[:], -BIG, ELM[:], ALU.mult, ALU.add))
            dv(lambda e: e.tensor_reduce(out=V2, in_=ELM2[:], axis=AX.X, op=ALU.max))
            dv(TT(D1[:], ELM2[:], bc3(V2, 32), ALU.subtract))
            dv(TS(OH2[:], D1[:], 0.0, None, ALU.is_equal))
            dv(TT(DV, V2, V1, ALU.subtract))
            P.op("act", ACT(E21, DV, AF.Exp), r=[rR], w=[rR])
            dv(TS(P1, E21, 1.0, None, ALU.add))
            dv(lambda e: e.reciprocal(out=P1, in_=P1))
            dv(TT(C1, P1, GP, ALU.mult))
            dv(TT(C2, E21, C1, ALU.mult))
            rIDX = P.res("IDX")
            MBo = rb + nR[0]
            nR[0] += 1024
            MB = v3(carve(MBo, 512, BF16), 16)
            CNT = rt(32)
            CMP3 = v3(rt(512), 32)
            NBK = rt(32)
            T3 = v3(rt(1024), 32)
            LE3 = v3(rt(1024), 32)
            PEND = rt(32)
            PST = rt(32)
            DA = v3(rt(512), 16)
            DF = rt(32)
            CMPB3 = v3(rt(NSTEP * 32), NSTEP)
            EBf = rt(NSTEP)
            INV = rt(NSTEP)
            WF = rt(NSTEP)
            BST = BIDX[:, 0:NSTEP]
            dv(TT(MB[:], OH1[:], OH2[:], ALU.add))
            psR, prR = psum()
            fns = []
            for t in range(16):
                fns.append(MM(psR[:, t * 32:(t + 1) * 32], TRIB[:], MB[:, t, :], st=True, sp=(t == 15)))
                for t2 in range(t + 1, 16):
                    fns.append(MM(psR[:, t * 32:(t + 1) * 32], ONESB[:], MB[:, t2, :], st=False, sp=(t2 == 15)))
            P.op("pe", fns, r=[rR, rC], w=[prR])
            psC, prC = psum()
            P.op("pe", [MM(psC[:, 0:32], ONESB[:], MB[:, t, :], st=(t == 0), sp=(t == 15)) for t in range(16)],
                 r=[rR, rC], w=[prC])
            dv(CP(CNT, psC[:, 0:32]), r=[prC])
            dv(TT(CMP3[:], CNT.unsqueeze(2).to_broadcast([128, 32, 16]), TH16.unsqueeze(1).to_broadcast([128, 32, 16]),
                  ALU.is_gt), r=[rC])
            dv(lambda e: e.tensor_reduce(out=NBK, in_=CMP3[:], axis=AX.X, op=ALU.add))
            IO32 = BIDX[:, 0:32]
            dv(TT(LE3[:], IO32.unsqueeze(1).to_broadcast([128, 32, 32]), IO32.unsqueeze(2).to_broadcast([128, 32, 32]),
                  ALU.is_le), r=[rC])
            dv(TT(T3[:], LE3[:], NBK.unsqueeze(1).to_broadcast([128, 32, 32]), ALU.mult))
            dv(lambda e: e.tensor_reduce(out=PEND, in_=T3[:], axis=AX.X, op=ALU.add))
            dv(TT(PST, PEND, NBK, ALU.subtract))
            dv(TS(PST, PST, 256.0, None, ALU.mult))
            dv(TT(DA[:], v3(psR[:], 16), PST.unsqueeze(1).to_broadcast([128, 16, 32]), ALU.add), r=[prR])
            dv(TT(D1[:], OH1[:], DA[:], ALU.mult))
            dv(lambda e: e.tensor_reduce(out=DF[:, 0:16], in_=D1[:], axis=AX.X, op=ALU.add))
            dv(TT(D1[:], OH2[:], DA[:], ALU.mult))
            dv(lambda e: e.tensor_reduce(out=DF[:, 16:32], in_=D1[:], axis=AX.X, op=ALU.add))
            dv(CP(DIDX[:], DF), w=[rIDX])
            dv(CP(GATE[:, 0:16], C1), w=[rIDX])
            dv(CP(GATE[:, 16:32], C2), w=[rIDX])
            dv(TT(CMPB3[:], PEND.unsqueeze(1).to_broadcast([128, NSTEP, 32]), BST.unsqueeze(2).to_broadcast([128, NSTEP, 32]),
                  ALU.is_le), r=[rC])
            dv(lambda e: e.tensor_reduce(out=EBf, in_=CMPB3[:], axis=AX.X, op=ALU.add))
            dv(TS(INV, BST, PEND[:, 31:32], None, ALU.is_ge), r=[rC])
            dv(TS(EBf, EBf, 31.0, None, ALU.min))
            dv(TS(WF, EBf, float(layer * NEXP), 128.0, ALU.add, ALU.mult))
            dv(TS(WF, WF, PIDX, None, ALU.add), r=[rC])
            dv(STT(WF, INV, 1.0e6, WF, ALU.mult, ALU.add))
            dv(CP(WIDX[:], WF), w=[rIDX])
            P.barrier()

            IOA = bass.IndirectOffsetOnAxis
            Wslots = [L0, M0]

            def wviews(slot):
                o = Wslots[slot]
                return carve(o, 4096, BF16), carve(o + 8192, 4096, BF16), carve(o + 16384, 4096, BF16)
            rWGU = [P.res("WGU0"), P.res("WGU1")]
            rWD = [P.res("WD0"), P.res("WD1")]

            def wgather(dst, src, bi):
                return lambda eng: eng.indirect_dma_start(out=dst, out_offset=None, in_=src,
                                                          in_offset=IOA(ap=WIDX[:, bi:bi + 1], axis=0),
                                                          bounds_check=P.regs["wb"], oob_is_err=False)

            def load_wgu(st):
                wg, wu, _ = wviews(st % 2)
                P.dmaf("pool", wgather(wg, wgL_d, st), r=[rIDX], w=[rWGU[st % 2]])
                P.dmaf("pool", wgather(wu, wuL_d, st), r=[rIDX], w=[rWGU[st % 2]])

            def load_wd(st):
                _, _, wdn = wviews(st % 2)
                P.dmaf("pool", wgather(wdn, wdL_d, st), r=[rIDX], w=[rWD[st % 2]])

            eb = M0 + 24576
            HTOK = [carve(eb + s_ * 2048, 1024, BF16) for s_ in range(2)]
            NXS = 4
            XSL = [carve(eb + 49152 + s_ * 2048, 1024, BF16) for s_ in range(NXS)]
            XF = [carve(eb + 8192 + s_ * 2048, 1024, BF16) for s_ in range(2)]
            SS = [carve(eb + 12288 + s_ * 2048, 512, F32) for s_ in range(2)]
            HIDS = [carve(eb + 16384 + s_ * 1024, 512, BF16) for s_ in range(2)]
            HIDT = [carve(eb + 18432 + s_ * 1024, 512, BF16) for s_ in range(2)]
            YS = [carve(eb + 20480 + s_ * 2048, 1024, BF16) for s_ in range(2)]
            NYG = 4
            YG = [[carve(eb + 24576 + (k * NYG + s_) * 2048, 1024, BF16) for s_ in range(NYG)] for k in range(2)]
            YC = [carve(eb + 40960 + s_ * 4096, 1024, F32) for s_ in range(2)]
            two = lambda n: [P.res(n + "0"), P.res(n + "1")]
            rHTOK, rXF, rSS, rHIDS, rHIDT, rYS = two("HTOK"), two("XF"), two("SS"), two("HIDS"), two("HIDT"), two("YS")
            rXSL = [P.res("XSL%d" % i) for i in range(NXS)]
            rXs, rYs = two("Xs"), two("Ys")
            rYG = [[P.res("YG%d_%d" % (k, i)) for i in range(NYG)] for k in range(2)]
            rYC = two("YC")

            load_wgu(0)
            load_wd(0)
            load_wgu(1)
            load_wd(1)
            for t16 in range(16):
                b = t16 % 2
                ps, pr = psum()
                pb_ = ps[:].bitcast(BF16)
                P.op("pe", [TR(pb_[:, k * 128:(k + 1) * 128], HT[:, k, t16 * 128:(t16 + 1) * 128], IDB[:]) for k in range(KC)],
                     r=[rHT[t16 // 4], rC], w=[pr])
                if b == 0:
                    P.op("act", ACT(HTOK[b], pb_[:, 0:1024], AF.Copy), r=[pr], w=[rHTOK[b]])
                else:
                    P.op("dve", CP(HTOK[b], pb_[:, 0:1024]), r=[pr], w=[rHTOK[b]])
                for k in range(2):
                    c = k * 16 + t16
                    P.dmaf("pool", lambda eng, b=b, c=c: eng.indirect_dma_start(
                        out=xs_d, out_offset=IOA(ap=DIDX[:, c:c + 1], axis=0), in_=HTOK[b], in_offset=None,
                        bounds_check=P.regs["xb"], oob_is_err=False),
                        r=[rHTOK[b], rIDX], w=[], dres=rXs[b], nowait_w=[rXs[b]])

            def xload(bi):
                x4 = bi % NXS
                P.dma("sp", XSL[x4], xs_d[bi * 128:(bi + 1) * 128, :], r=[rXs[0], rXs[1]], w=[rXSL[x4]])

            def stage_Tx(bi):
                s2 = bi % 2
                x4 = bi % NXS
                ps, pr = psum()
                pb_ = ps[:].bitcast(BF16)
                P.op("pe", [TR(pb_[:, k * 128:(k + 1) * 128], XSL[x4][:, k * 128:(k + 1) * 128], IDB[:]) for k in range(KC)],
                     r=[rXSL[x4], rC], w=[pr])
                P.op("act", ACT(XF[s2], pb_[:, 0:1024], AF.Copy), r=[pr], w=[rXF[s2]])

            def stage_GU(bi):
                s2 = bi % 2
                ws = (bi // 2) % 2
                wg, wu, _ = wviews(ws)
                psg, prg = psum()
                P.op("pe", [MM(psg[:], XF[s2][:, k * 128:(k + 1) * 128], wg[:, k * 512:(k + 1) * 512], st=(k == 0), sp=(k == KC - 1))
                            for k in range(KC)], r=[rXF[s2], rWGU[ws]], w=[prg])
                psu, pru = psum()
                P.op("pe", [MM(psu[:], XF[s2][:, k * 128:(k + 1) * 128], wu[:, k * 512:(k + 1) * 512], st=(k == 0), sp=(k == KC - 1))
                            for k in range(KC)], r=[rXF[s2], rWGU[ws]], w=[pru])
                P.op("act", ACT(SS[s2], psg[:], AF.Silu), r=[prg], w=[rSS[s2]])
                P.op("dve", TT(HIDS[s2], psu[:], SS[s2], ALU.mult), r=[pru, rSS[s2]], w=[rHIDS[s2]])

            def stage_Th(bi):
                s2 = bi % 2
                ps, pr = psum()
                pb_ = ps[:].bitcast(BF16)
                P.op("pe", [TR(pb_[:, f * 128:(f + 1) * 128], HIDS[s2][:, f * 128:(f + 1) * 128], IDB[:]) for f in range(4)],
                     r=[rHIDS[s2], rC], w=[pr])
                P.op("dve", CP(HIDT[s2], pb_[:, 0:512]), r=[pr], w=[rHIDT[s2]])

            def stage_D(bi):
                s2 = bi % 2
                ws = (bi // 2) % 2
                _, _, wdn = wviews(ws)
                for half in range(2):
                    ps, pr = psum()
                    P.op("pe", [MM(ps[:], HIDT[s2][:, f * 128:(f + 1) * 128],
                                   wdn[:, f * 1024 + half * 512:f * 1024 + (half + 1) * 512], st=(f == 0), sp=(f == 3))
                                for f in range(4)], r=[rHIDT[s2], rWD[ws]], w=[pr])
                    if half == 0:
                        P.op("act", ACT(YS[s2][:, 0:512], ps[:], AF.Copy), r=[pr], w=[rYS[s2]])
                    else:
                        P.op("dve", CP(YS[s2][:, 512:1024], ps[:]), r=[pr], w=[rYS[s2]])
                P.dma("sp", ys_d[bi * 128:(bi + 1) * 128, :], YS[s2], r=[rYS[s2]], w=[], dres=rYs[s2], nowait_w=[rYs[s2]])

            for bi in range(NXS - 1):
                xload(bi)
            stage_Tx(0)
            for i in range(NSUB + 1):
                if i >= 1:
                    stage_Th(i - 1)
                if i + NXS - 1 < NSUB:
                    xload(i + NXS - 1)
                if i + 1 < NSUB:
                    stage_Tx(i + 1)
                if i < NSUB:
                    stage_GU(i)
                    if i % 2 == 1 and i // 2 + 2 < NSTEP:
                        load_wgu(i // 2 + 2)
                if i >= 1:
                    stage_D(i - 1)
                    if (i - 1) % 2 == 1 and (i - 1) // 2 + 2 < NSTEP:
                        load_wd((i - 1) // 2 + 2)
            P.barrier()

            def issue_gather(t16):
                g = t16 % NYG
                for k in range(2):
                    c = k * 16 + t16
                    P.dmaf("pool", lambda eng, g=g, c=c, k=k: eng.indirect_dma_start(
                        out=YG[k][g], out_offset=None, in_=ys_d, in_offset=IOA(ap=DIDX[:, c:c + 1], axis=0),
                        bounds_check=P.regs["xb"], oob_is_err=False),
                        r=[rYs[0], rYs[1], rIDX], w=[rYG[k][g]])
            def k1(t16):
                b = t16 % 2
                g = t16 % NYG
                P.op("act", ACT(YC[b], YG[0][g], AF.Copy, scale=GATE[:, t16:t16 + 1]), r=[rYG[0][g], rIDX], w=[rYC[b]])
                P.op("dve", STT(YC[b], YG[1][g], GATE[:, 16 + t16:17 + t16], YC[b], ALU.mult, ALU.add),
                     r=[rYG[1][g], rIDX], w=[rYC[b]])

            def k2(t16):
                b = t16 % 2
                tok = slice(t16 * 128, (t16 + 1) * 128)
                for half in range(2):
                    ps, pr = psum()
                    P.op("pe", [TR(ps[:, j * 128:(j + 1) * 128], YC[b][:, (half * 4 + j) * 128:(half * 4 + j + 1) * 128], IDF)
                                for j in range(4)], r=[rYC[b], rC], w=[pr])
                    for j in range(4):
                        dc = half * 4 + j
                        P.op("dve", STT(XT[:, dc, tok], ps[:, j * 128:(j + 1) * 128], modv(layer, 5, dc), XT[:, dc, tok],
                                        ALU.mult, ALU.add), r=[pr, rMOD], w=[rXT[t16 // 4]])

            for t16 in range(NYG - 1):
                issue_gather(t16)
            k1(0)
            for t16 in range(16):
                if t16 + NYG - 1 < 16:
                    issue_gather(t16 + NYG - 1)
                if t16 + 1 < 16:
                    k1(t16 + 1)
                k2(t16)
            P.barrier()

        def mixer_conv(layer):
            UT = v3(carve(M0, KC * 2080, BF16), KC)
            o = M0 + 33280
            WIN = [v3(carve(o + s_ * 2048, KC * 128, BF16), KC) for s_ in range(4)]
            o += 8192
            SIG = [carve(o + s_ * 2048, 512, F32) for s_ in range(2)]
            o += 4096
            DG = [v3(carve(o + s_ * 8192, 31 * 128, BF16), 31) for s_ in range(2)]
            WOUT = v3(carve(o, KC * D, BF16), KC)
            o += 16384
            YSQ = v3(carve(o, KC * 512, BF16), KC)
            o += 8192
            T1 = [carve(o + s_ * 2048, 512, F32) for s_ in range(2)]
            o += 4096
            MEAN = carve(o, 512, F32)
            MSQ = carve(o + 2048, 512, F32)
            RSTD = carve(o + 4096, 512, F32)
            o += 6144
            rUT = P.res("UT")
            rWIN = [P.res("WIN%d" % s_) for s_ in range(4)]
            rSIG = [P.res("SIG0"), P.res("SIG1")]
            rDG = [P.res("DG0"), P.res("DG1")]
            Y = HT
            P.op("pool", MS(UT[:, :, 0:32], 0.0), w=[rUT])
            for c in range(KC):
                sa, sg = (2 * c) % 4, (2 * c + 1) % 4
                P.dma("pool", WIN[sa][:], cwin_d[:, c * 128:(c + 1) * 128].rearrange("(k p) n -> p k n", p=128), w=[rWIN[sa]])
                P.dma("pool", WIN[sg][:], cwin_d[:, D + c * 128:D + (c + 1) * 128].rearrange("(k p) n -> p k n", p=128),
                      w=[rWIN[sg]])
                for tt in range(4):
                    sl = slice(tt * 512, (tt + 1) * 512)
                    s2 = (c * 4 + tt) % 2
                    psa, pra = psum()
                    P.op("pe", [MM(psa[:], WIN[sa][:, k, :], HT[:, k, sl], st=(k == 0), sp=(k == KC - 1)) for k in range(KC)],
                         r=[rWIN[sa], rHT[tt]], w=[pra])
                    psg, prg = psum()
                    P.op("pe", [MM(psg[:], WIN[sg][:, k, :], HT[:, k, sl], st=(k == 0), sp=(k == KC - 1)) for k in range(KC)],
                         r=[rWIN[sg], rHT[tt]], w=[prg])
                    P.op("act", ACT(SIG[s2], psg[:], AF.Sigmoid, bias=V("cbin", 8 + c), scale=1.0), r=[prg, rVEC], w=[rSIG[s2]])
                    P.op("dve", STT(UT[:, c, 32 + tt * 512:32 + (tt + 1) * 512], psa[:], V("cbin", c), SIG[s2], ALU.add, ALU.mult),
                         r=[pra, rSIG[s2], rVEC], w=[rUT])
            P.barrier()
            for c in range(KC):
                ds = c % 2
                P.op("dve", [TS(DG[ds][:, j, :], IDB[:], V("wdw", c * 31 + j), None, ALU.mult) for j in range(31)],
                     r=[rVEC, rC], w=[rDG[ds]])
                for tt in range(4):
                    ps, pr = psum()
                    P.op("pe", [MM(ps[:], DG[ds][:, j, :], UT[:, c, tt * 512 + j + 2: tt * 512 + j + 2 + 512], st=(j == 0), sp=(j == 30))
                                for j in range(31)], r=[rDG[ds], rUT], w=[pr])
                    P.op("act", ACT(Y[:, c, tt * 512:(tt + 1) * 512], ps[:], AF.Identity, bias=V("cbdw", c), scale=1.0),
                         r=[pr, rVEC], w=[rHT[tt]])
            P.barrier()
            rW_ = P.res("WOUT")
            P.dma("pool", WOUT[:], cwout_d.rearrange("(k p) n -> p k n", p=128), w=[rW_])
            rYSQ = P.res("YSQ")
            rST = P.res("stats")
            rT1 = [P.res("T1a"), P.res("T1b")]
            for tt in range(4):
                sl = slice(tt * 512, (tt + 1) * 512)
                P.op("act", ACT(YSQ[:], Y[:, :, sl], AF.Square), r=[rHT[tt]], w=[rYSQ])
                p1, r1 = psum()
                P.op("pe", [MM(p1[:], ONESB[:], Y[:, c, sl], st=(c == 0), sp=(c == KC - 1)) for c in range(KC)],
                     r=[rHT[tt], rC], w=[r1])
                p2, r2 = psum()
                P.op("pe", [MM(p2[:], ONESB[:], YSQ[:, c, :], st=(c == 0), sp=(c == KC - 1)) for c in range(KC)],
                     r=[rYSQ, rC], w=[r2])
                P.op("dve", TS(MEAN, p1[:], 1.0 / D, None, ALU.mult), r=[r1], w=[rST])
                P.op("dve", TT(MSQ, MEAN, MEAN, ALU.mult), r=[rST], w=[rST])
                P.op("dve", STT(RSTD, p2[:], 1.0 / D, MSQ, ALU.mult, ALU.subtract), r=[r2, rST], w=[rST])
                P.op("act", ACT(RSTD, RSTD, AF.Sqrt, bias=SMALL[:, 0:1], scale=1.0), r=[rST, rC], w=[rST])
                P.op("dve", lambda e: e.reciprocal(out=RSTD, in_=RSTD), r=[rST], w=[rST])
                for c in range(KC):
                    s2 = c % 2
                    P.op("dve", TT(T1[s2], Y[:, c, sl], MEAN, ALU.subtract), r=[rHT[tt], rST], w=[rT1[s2]])
                    P.op("pool", TT(T1[s2], T1[s2], RSTD, ALU.mult), r=[rST], w=[rT1[s2]])
                    P.op("act", ACT(Y[:, c, sl], T1[s2], AF.Silu, bias=V("clnb", c), scale=V("clng", c)),
                         r=[rT1[s2], rVEC], w=[rHT[tt]])
            for tt in range(4):
                sl = slice(tt * 512, (tt + 1) * 512)
                for dc in range(KC):
                    ps, pr = psum()
                    P.op("pe", [MM(ps[:], WOUT[:, c, dc * 128:(dc + 1) * 128], Y[:, c, sl], st=(c == 0), sp=(c == KC - 1))
                                for c in range(KC)], r=[rW_, rHT[tt]], w=[pr])
                    P.op("dve", STT(XT[:, dc, sl], ps[:], modv(layer, 2, dc), XT[:, dc, sl], ALU.mult, ALU.add),
                         r=[pr, rMOD], w=[rXT[tt]])
            P.barrier()

        def mixer_pool(layer):
            o = M0
            SA = [carve(o + s_ * 8192, S, F32) for s_ in range(2)]
            SB = [carve(o + 16384 + s_ * 8192, S, F32) for s_ in range(2)]
            PL = v3(carve(o + 32768, KC * S, BF16), KC)
            o2 = o + 32768 + 32768
            PW = carve(o2, 4 * 2 * 256, BF16).rearrange("p (g k n) -> p g k n", g=4, k=2)
            INVC = carve(o2 + 4096, 64, F32)
            T16 = [carve(o2 + 4096 + 256 + s_ * 64, 16, F32) for s_ in range(2)]
            YT = [carve(o2 + 8192 + s_ * 2048, 512, F32) for s_ in range(2)]
            GL_ = carve(o2 + 8192 + 4096, 16, F32)
            rPW = P.res("PW")
            rIN = P.res("INVC")
            rS = [P.res("S0"), P.res("S1")]
            rPL = [P.res("PL%d" % c) for c in range(KC)]
            rYT = [P.res("YT0"), P.res("YT1")]
            rGL = P.res("GL")
            rT16 = [P.res("T16a"), P.res("T16b")]
            P.dma("pool", PW, pw_d.rearrange("g (k p) n -> p g k n", p=128), w=[rPW])
            P.dma("sp", INVC, invc_d, w=[rIN])
            for c in range(KC):
                P.op("dve", TT(GL_[:, c:c + 1], modv(layer, 2, c), V("psc", c), ALU.mult), r=[rMOD, rVEC], w=[rGL])
                P.op("dve", TT(GL_[:, 8 + c:9 + c], GL_[:, c:c + 1], V("pb", c), ALU.mult), r=[rVEC], w=[rGL])
            for c in range(KC):
                gi = c // 2
                wnd = 2 << gi
                en = "dve" if c % 2 == 0 else "pool"
                s_ = c % 2
                bufs = [SA[s_], SB[s_]]
                cur = HT[:, c, :]
                sh = 1
                bi = 0
                while sh < wnd:
                    nxt = bufs[bi]
                    P.op(en, [TT(nxt[:, sh:], cur[:, sh:], cur[:, 0:S - sh], ALU.add), CP(nxt[:, 0:sh], cur[:, 0:sh])],
                         r=[rHT[0], rHT[1], rHT[2], rHT[3]], w=[rS[s_]])
                    cur = nxt
                    bi ^= 1
                    sh *= 2
                oth = bufs[bi]
                P.op(en, TS(oth, cur, 1.0 / wnd, None, ALU.mult), r=[], w=[rS[s_]])
                P.op(en, TT(PL[:, c, :], oth, HT[:, c, :], ALU.subtract), r=[rHT[0], rHT[1], rHT[2], rHT[3], rS[s_]], w=[rPL[c]])
                P.op(en, TT(T16[s_], cur[:, 0:16], INVC[:, gi * 16:(gi + 1) * 16], ALU.mult), r=[rIN, rS[s_]], w=[rT16[s_]])
                P.op(en, TT(PL[:, c, 0:16], T16[s_], HT[:, c, 0:16], ALU.subtract), r=[rT16[s_]], w=[rPL[c]])
            i_ = 0
            for gi in range(4):
                for dn in range(2):
                    dc = gi * 2 + dn
                    for tt in range(4):
                        sl = slice(tt * 512, (tt + 1) * 512)
                        ps, pr = psum()
                        P.op("pe", [MM(ps[:], PW[:, gi, k, dn * 128:(dn + 1) * 128], PL[:, gi * 2 + k, sl], st=(k == 0), sp=(k == 1))
                                    for k in range(2)], r=[rPW, rPL[gi * 2], rPL[gi * 2 + 1]], w=[pr])
                        s2 = i_ % 2
                        i_ += 1
                        P.op("act", ACT(YT[s2], ps[:], AF.Identity, bias=GL_[:, 8 + dc:9 + dc], scale=GL_[:, dc:dc + 1]),
                             r=[pr, rGL], w=[rYT[s2]])
                        P.op("dve", TT(XT[:, dc, sl], XT[:, dc, sl], YT[s2], ALU.add), r=[rYT[s2]], w=[rXT[tt]])
            P.barrier()

        def mixer_hgrn(layer):
            o = M0
            A_ = carve(o, S, F32); o += 8192
            B_ = carve(o, S, F32); o += 8192
            C_ = carve(o, S, F32); o += 8192
            D_ = carve(o, S, F32); o += 8192
            QTb = carve(o, S, BF16); o += 4096
            KTb = carve(o, S, BF16); o += 4096
            KHT = carve(o, S, BF16); o += 4096
            KHtok = v3(carve(o, 16 * 128, BF16), 16); o += 4096
            Vt = v3(carve(o, 16 * 128, BF16), 16); o += 4096
            Gb = carve(o, S, BF16); o += 4096
            WQ = v3(carve(o, KC * 512, BF16), KC); o += 8192
            SCM = carve(o, S, F32); o += 8192
            WO = [carve(o + s_ * 2048, D, BF16) for s_ in range(2)]; o += 4096
            SF = carve(o, 128, F32); o += 512
            SBF = [carve(o + s_ * 256, 128, BF16) for s_ in range(2)]; o += 512
            SC = [carve(o + s_ * 256, 128, BF16) for s_ in range(2)]; o += 512
            LB = carve(o, 64, F32); o += 256
            RS = carve(o, 512, F32); o += 2048
            OSQ = KHT
            OG = KTb
            rA, rB, rCc, rD = P.res("A"), P.res("B"), P.res("C"), P.res("D")
            rQT, rKT, rKHT, rKHtok, rV, rG = P.res("QT"), P.res("KT"), P.res("KHT"), P.res("KHtok"), P.res("V"), P.res("G")
            rWQ, rSCM, rLB = P.res("WQ"), P.res("SCM"), P.res("LB")
            rWO = [P.res("WO0"), P.res("WO1")]
            rSF = P.res("SF")
            rSBF = [P.res("SBF0"), P.res("SBF1")]
            rSC = [P.res("SC0"), P.res("SC1")]
            rRS = P.res("RS")
            P.dma("sp", SCM, scm_d, w=[rSCM])
            EX = carve(o, 64, F32); o += 256
            for i in range(DEPTH):
                P.op("act", ACT(EX[:, i * 8:(i + 1) * 8], V("lbl%d" % i, 0, 8), AF.Exp), r=[rVEC], w=[rLB])
            P.op("dve", TT(LB[:, 24:32], EX[:, 0:8], EX[:, 8:16], ALU.add), r=[rLB], w=[rLB])
            P.op("dve", TT(LB[:, 24:32], LB[:, 24:32], EX[:, 16:24], ALU.add), r=[rLB], w=[rLB])
            P.op("dve", TT(LB[:, 24:32], LB[:, 24:32], EX[:, 24:32], ALU.add), r=[rLB], w=[rLB])
            P.op("dve", lambda e: e.reciprocal(out=LB[:, 24:32], in_=LB[:, 24:32]), r=[rLB], w=[rLB])
            P.op("dve", MS(LB[:, 0:8], 0.0), w=[rLB])
            for i in range(1, layer + 1):
                P.op("dve", TT(LB[:, 0:8], LB[:, 0:8], EX[:, i * 8:(i + 1) * 8], ALU.add), r=[rLB], w=[rLB])
            P.op("dve", TT(LB[:, 0:8], LB[:, 0:8], LB[:, 24:32], ALU.mult), r=[rLB], w=[rLB])
            P.op("dve", TS(LB[:, 8:16], LB[:, 0:8], -1.0, 1.0, ALU.mult, ALU.add), r=[rLB], w=[rLB])
            P.op("dve", TS(LB[:, 16:24], LB[:, 8:16], -1.0, None, ALU.mult), r=[rLB], w=[rLB])
            for h in range(KC):
                for part in range(4):
                    P.dma("pool", WQ[:, :, part * 128:(part + 1) * 128],
                          hwin_d[:, part * D + h * 128: part * D + (h + 1) * 128].rearrange("(k p) n -> p k n", p=128),
                          w=[rWQ])
                P.dma("pool", WO[h % 2], hwout_d[h * 128:(h + 1) * 128, :], w=[rWO[h % 2]])
                for tt in range(4):
                    sl = slice(tt * 512, (tt + 1) * 512)
                    for part, dst in ((0, "q"), (1, "f"), (3, "g")):
                        ps, pr = psum()
                        P.op("pe", [MM(ps[:], WQ[:, k, part * 128:(part + 1) * 128], HT[:, k, sl], st=(k == 0), sp=(k == KC - 1))
                                    for k in range(KC)], r=[rWQ, rHT[tt]], w=[pr])
                        if dst == "q":
                            P.op("act", ACT(D_[:, sl], ps[:], AF.Silu), r=[pr], w=[rD])
                        elif dst == "f":
                            P.op("act", ACT(A_[:, sl], ps[:], AF.Sigmoid), r=[pr], w=[rA])
                        else:
                            P.op("act", ACT(Gb[:, sl], ps[:], AF.Silu), r=[pr], w=[rG])
                for t16 in range(16):
                    ps, pr = psum()
                    P.op("pe", [MM(ps[:, 0:128], HT[:, k, t16 * 128:(t16 + 1) * 128], WQ[:, k, 256:384], st=(k == 0), sp=(k == KC - 1))
                                for k in range(KC)], r=[rWQ, rHT[t16 // 4]], w=[pr])
                    P.op("act", ACT(Vt[:, t16, :], ps[:, 0:128], AF.Copy), r=[pr], w=[rV])
                lb, oml, noml = LB[:, h:h + 1], LB[:, 8 + h:9 + h], LB[:, 16 + h:17 + h]
                P.op("dve", TS(B_, A_, oml, lb, ALU.mult, ALU.add), r=[rA, rLB], w=[rB])
                P.op("act", ACT(B_, B_, AF.Ln), r=[], w=[rB])
                P.op("act", ACT(A_, A_, AF.Identity, bias=oml, scale=noml), r=[rLB, rB], w=[rA])
                P.op("dve", lambda e: e.tensor_tensor_scan(out=C_, data0=SCM, data1=B_, initial=0.0, op0=ALU.mult, op1=ALU.add),
                     r=[rB, rSCM], w=[rCc])
                P.op("act", ACT(B_, C_, AF.Exp), r=[rCc], w=[rB])
                P.op("dve", TS(C_, C_, -1.0, 80.0, ALU.mult, ALU.min), r=[rB], w=[rCc])
                P.op("act", ACT(C_, C_, AF.Exp), r=[], w=[rCc])
                P.op("dve", TT(QTb, D_, B_, ALU.mult), r=[rD, rB], w=[rQT])
                P.op("dve", TT(A_, A_, C_, ALU.mult), r=[rCc], w=[rA])
                P.op("act", ACT(KTb, A_, AF.Copy), r=[rA], w=[rKT])
                A3 = A_.rearrange("p (n c) -> p n c", c=64)
                B3 = B_.rearrange("p (n c) -> p n c", c=64)
                K3 = KHT.rearrange("p (n c) -> p n c", c=64)
                P.op("dve", TT(K3, A3, B3[:, :, 63:64].to_broadcast([128, 32, 64]), ALU.mult), r=[rA, rB], w=[rKHT])
                for t16 in range(16):
                    ps, pr = psum()
                    pb_ = ps[:].bitcast(BF16)
                    P.op("pe", TR(pb_[:, 0:128], KHT[:, t16 * 128:(t16 + 1) * 128], IDB[:]), r=[rKHT, rC], w=[pr])
                    P.op("act", ACT(KHtok[:, t16, :], pb_[:, 0:128], AF.Copy), r=[pr], w=[rKHtok])
                P.op("dve", MS(SF, 0.0), w=[rSF])
                P.op("dve", MS(SBF[0], 0.0), w=[rSBF[0]])
                sv = 0
                for t16 in range(16):
                    tsl = slice(t16 * 128, (t16 + 1) * 128)
                    sc_i = t16 % 2
                    ps, pr = psum()
                    P.op("pe", MM(ps[:, 0:128], KTb[:, tsl], QTb[:, tsl]), r=[rKT, rQT], w=[pr])
                    P.op("dve", TT(SC[sc_i], ps[:, 0:128], MASK2, ALU.mult), r=[pr, rC], w=[rSC[sc_i]])
                    pa, pra = psum()
                    P.op("pe", [MM(pa[:, 0:64], SBF[sv], QTb[:, t16 * 128:t16 * 128 + 64], st=True, sp=False),
                                MM(pa[:, 0:64], Vt[0:64, t16, :], SC[sc_i][0:64, 0:64], st=False, sp=True)],
                         r=[rSBF[sv], rQT, rV, rSC[sc_i]], w=[pra])
                    pu, pru = psum()
                    P.op("pe", MM(pu[:, 0:128], KHtok[0:64, t16, :], Vt[0:64, t16, :]), r=[rKHtok, rV], w=[pru])
                    P.op("dve", STT(SF, SF, B_[:, t16 * 128 + 63:t16 * 128 + 64], pu[:, 0:128], ALU.mult, ALU.add),
                         r=[pru, rB], w=[rSF])
                    P.op("act", ACT(SBF[1 - sv], SF, AF.Copy), r=[rSF], w=[rSBF[1 - sv]])
                    sv = 1 - sv
                    pb2, prb = psum()
                    P.op("pe", [MM(pb2[:, 0:64], SBF[sv], QTb[:, t16 * 128 + 64:t16 * 128 + 128], st=True, sp=False),
                                MM(pb2[:, 0:64], Vt[:, t16, :], SC[sc_i][:, 64:128], st=False, sp=True)],
                         r=[rSBF[sv], rQT, rV, rSC[sc_i]], w=[prb])
                    pu2, pru2 = psum()
                    P.op("pe", MM(pu2[:, 0:128], KHtok[64:128, t16, :], Vt[64:128, t16, :]), r=[rKHtok, rV], w=[pru2])
                    P.op("dve", STT(SF, SF, B_[:, t16 * 128 + 127:t16 * 128 + 128], pu2[:, 0:128], ALU.mult, ALU.add),
                         r=[pru2, rB], w=[rSF])
                    P.op("act", ACT(SBF[1 - sv], SF, AF.Copy), r=[rSF], w=[rSBF[1 - sv]])
                    sv = 1 - sv
                    P.op("act", ACT(D_[:, t16 * 128:t16 * 128 + 64], pa[:, 0:64], AF.Copy), r=[pra, rQT], w=[rD])
                    P.op("act", ACT(D_[:, t16 * 128 + 64:t16 * 128 + 128], pb2[:, 0:64], AF.Copy), r=[prb], w=[rD])
                for tt in range(4):
                    sl = slice(tt * 512, (tt + 1) * 512)
                    P.op("act", ACT(OSQ[:, sl], D_[:, sl], AF.Square), r=[rD, rKHtok], w=[rKHT])
                    ps, pr = psum()
                    P.op("pe", MM(ps[:], ONESB[:], OSQ[:, sl]), r=[rKHT, rC], w=[pr])
                    P.op("act", ACT(RS, ps[:], AF.Sqrt, bias=SMALL[:, 0:1], scale=1.0 / 128), r=[pr, rC], w=[rRS])
                    P.op("dve", lambda e: e.reciprocal(out=RS, in_=RS), r=[rRS], w=[rRS])
                    P.op("dve", STT(D_[:, sl], D_[:, sl], V("hng", h), RS, ALU.mult, ALU.mult), r=[rRS, rVEC], w=[rD])
                    P.op("pool", TT(OG[:, sl], D_[:, sl], Gb[:, sl], ALU.mult), r=[rD, rG, rSC[0], rSC[1]], w=[rKT])
                    for dc in range(KC):
                        ps2, pr2 = psum()
                        P.op("pe", MM(ps2[:], WO[h % 2][:, dc * 128:(dc + 1) * 128], OG[:, sl]), r=[rWO[h % 2], rKT], w=[pr2])
                        P.op("dve", STT(XT[:, dc, sl], ps2[:], modv(layer, 2, dc), XT[:, dc, sl], ALU.mult, ALU.add),
                             r=[pr2, rMOD], w=[rXT[tt]])
            P.barrier()

        def mixer_sb(layer):
            scale = 64 ** -0.5
            o = L0
            QT = [carve(o + s_ * 4096, S, BF16) for s_ in range(2)]; o += 8192
            KT = [carve(o + s_ * 4096, S, BF16) for s_ in range(2)]; o += 8192
            VP = [v3(carve(o + hp * 4096, 16 * 128, BF16), 16) for hp in range(2)]; o += 8192
            OT = [carve(o + s_ * 4096, S, BF16) for s_ in range(2)]; o += 8192
            WQ = [v3(carve(o + s_ * 6144, KC * 384, BF16), KC) for s_ in range(2)]; o += 12288
            WO = [carve(o + s_ * 2048, D, BF16) for s_ in range(2)]; o += 4096
            SP = [carve(o + s_ * 8192, S, F32) for s_ in range(2)]; o += 16384
            A_ = [carve(o + s_ * 8192, S, F32) for s_ in range(2)]; o += 16384
            C_ = carve(o, S, F32); o += 8192
            W_ = [carve(o + s_ * 4096, S, BF16) for s_ in range(2)]; o += 8192
            WT = [v3(carve(o + s_ * 4096, 16 * 128, BF16), 16) for s_ in range(2)]; o += 8192
            assert o <= ARENA_BYTES, o
            NT = [SMALL[:, 8:9], SMALL[:, 9:10]]
            two = lambda n: [P.res(n + "0"), P.res(n + "1")]
            rQT, rKT, rOT, rWQ, rWO = two("QT"), two("KT"), two("OT"), two("WQ"), two("WO")
            rVP = P.res("VP")
            rSP = two("SP")
            rCc = P.res("C")
            rA, rW, rWT, rNT = two("A"), two("W"), two("WT"), two("NT")
            rNEG = P.res("NEGM")
            P.op("dve", TS(NEGM[:], CST[:, 128:256], -1.0, 30000.0, ALU.add, ALU.mult), r=[rC], w=[rNEG])
            po, pro = banks[7], bres[7]
            P.op("pool", MS(VP[0][:, :, 64:128], 0.0), w=[rVP])
            P.op("pool", MS(VP[1][:, :, 0:64], 0.0), w=[rVP])
            zb = [0]
            tb = [0]

            def zbank():
                i = zb[0]
                zb[0] = (i + 1) % 4
                return banks[i], bres[i]

            def tbank():
                i = 4 + tb[0]
                tb[0] = (tb[0] + 1) % 3
                return banks[i], bres[i]

            def load_weights(c):
                s_ = c % 2
                for part in range(3):
                    P.dma("pool", WQ[s_][:, :, part * 128:(part + 1) * 128],
                          sqkv_d[:, part * D + c * 128: part * D + (c + 1) * 128].rearrange("(k p) n -> p k n", p=128),
                          w=[rWQ[s_]])
                P.dma("pool", WO[s_], swout_d[c * 128:(c + 1) * 128, :], w=[rWO[s_]])

            def qk_proj(c):
                s_ = c % 2
                for tt in range(4):
                    sl = slice(tt * 512, (tt + 1) * 512)
                    for part, dst, rd in ((0, QT[s_], rQT[s_]), (1, KT[s_], rKT[s_])):
                        ps, pr = tbank()
                        P.op("pe", [MM(ps[:], WQ[s_][:, k, part * 128:(part + 1) * 128], HT[:, k, sl], st=(k == 0), sp=(k == KC - 1))
                                    for k in range(KC)], r=[rWQ[s_], rHT[tt]], w=[pr])
                        P.op("act", ACT(dst[:, sl], ps[:], AF.Copy), r=[pr], w=[rd])

            def v_proj(c):
                s_ = c % 2
                for t16 in range(16):
                    ps, pr = tbank()
                    P.op("pe", [MM(ps[:, 0:128], HT[:, k, t16 * 128:(t16 + 1) * 128], WQ[s_][:, k, 256:384], st=(k == 0), sp=(k == KC - 1))
                                for k in range(KC)], r=[rWQ[s_], rHT[t16 // 4]], w=[pr])
                    P.op("dve", [CP(VP[0][:, t16, 0:64], ps[:, 0:64]), CP(VP[1][:, t16, 64:128], ps[:, 64:128])], r=[pr], w=[rVP])

            def stageA(u, c, qb, hp):
                s_ = c % 2
                b = u % 2
                hs = slice(hp * 64, (hp + 1) * 64)
                tq = slice(qb * 128, (qb + 1) * 128)
                nk = (qb + 1) * 128
                nch = (nk + 511) // 512
                zs = []
                for ch in range(nch):
                    w = min(512, nk - ch * 512)
                    cs = slice(ch * 512, ch * 512 + w)
                    pz, prz = zbank()
                    fns = [MM(pz[:, 0:w], QT[s_][hs, tq], KT[s_][hs, cs], st=True, sp=(ch != nch - 1))]
                    if ch == nch - 1:
                        fns.append(MM(pz[:, w - 128:w], IDB[:], NEGM[:], st=False, sp=True))
                    P.op("pe", fns, r=[rQT[s_], rKT[s_], rNEG, rC], w=[prz])
                    P.op("act", ACT(SP[b][:, cs], pz[:, 0:w], AF.Exp, scale=scale), r=[prz], w=[rSP[b]])
                    zs.append((pz, prz, w, cs))
                P.op("act", ACT(SP[b][:, 0:nk], SP[b][:, 0:nk], AF.Ln, bias=SMALL[:, 1:2], scale=1.0), r=[rC], w=[rSP[b]])
                for pz, prz, w, cs in zs:
                    P.op("dve", STT(A_[b][:, cs], pz[:, 0:w], scale, SP[b][:, cs], ALU.mult, ALU.subtract), r=[prz, rSP[b]], w=[rA[b]])

            def stageB(u, c, qb, hp):
                b = u % 2
                nk = (qb + 1) * 128
                e_scan, e_add = ("dve", "pool")
                P.op(e_scan, lambda e, b=b, nk=nk: e.tensor_tensor_scan(
                    out=C_[:, 0:nk], data0=SMALL[:, 1:2].to_broadcast([128, nk]), data1=SP[b][:, 0:nk], initial=0.0,
                    op0=ALU.mult, op1=ALU.add), r=[rSP[b], rC], w=[rCc])
                P.op(e_scan, TS(NT[b], C_[:, nk - 1:nk], -1.0, None, ALU.mult), r=[rCc], w=[rNT[b]])
                P.op(e_add, TT(A_[b][:, 0:nk], A_[b][:, 0:nk], C_[:, 0:nk], ALU.add), r=[rCc], w=[rA[b]])

            def stageC1(u, c, qb, hp):
                b = u % 2
                nk = (qb + 1) * 128
                P.op("act", ACT(W_[b][:, 0:nk], A_[b][:, 0:nk], AF.Exp, bias=NT[b], scale=1.0), r=[rA[b], rNT[b]], w=[rW[b]])

            def stageC(u, c, qb, hp):
                s_ = c % 2
                b = u % 2
                tq = slice(qb * 128, (qb + 1) * 128)
                nk = (qb + 1) * 128
                nb_ = qb + 1
                for g0 in range(0, nb_, 8):
                    n = min(8, nb_ - g0)
                    pt, prt = tbank()
                    ptb = pt[:].bitcast(BF16)
                    P.op("pe", [TR(ptb[:, j * 128:(j + 1) * 128], W_[b][:, (g0 + j) * 128:(g0 + j + 1) * 128], IDB[:]) for j in range(n)],
                         r=[rW[b], rC], w=[prt])
                    dst = WT[b][:, g0:g0 + n, :].rearrange("p a b -> p (a b)")
                    if g0 == 0:
                        P.op("act", ACT(dst, ptb[:, 0:n * 128], AF.Copy), r=[prt], w=[rWT[b]])
                    else:
                        P.op("dve", CP(dst, ptb[:, 0:n * 128]), r=[prt], w=[rWT[b]])
                pcol = slice((qb % 4) * 128, (qb % 4 + 1) * 128)
                P.op("pe", [MM(po[:, pcol], VP[hp][:, jb, :], WT[b][:, jb, :], st=(hp == 0 and jb == 0), sp=(hp == 1 and jb == qb))
                            for jb in range(nb_)], r=[rVP, rWT[b]], w=[pro])
                if hp == 1:
                    P.op("dve", CP(OT[s_][:, tq], po[:, pcol]), r=[pro], w=[rOT[s_]])
                    if qb % 4 == 3:
                        I = qb // 4
                        qsl = slice(I * 512, (I + 1) * 512)
                        for dc in range(KC):
                            ps2, pr2 = tbank()
                            P.op("pe", MM(ps2[:], WO[s_][:, dc * 128:(dc + 1) * 128], OT[s_][:, qsl]), r=[rWO[s_], rOT[s_]], w=[pr2])
                            P.op("dve", STT(XT[:, dc, qsl], ps2[:], modv(layer, 2, dc), XT[:, dc, qsl], ALU.mult, ALU.add),
                                 r=[pr2, rMOD], w=[rXT[I]])

            units = [(c, qb, hp) for c in range(KC) for qb in range(16) for hp in range(2)]
            load_weights(0)
            load_weights(1)
            nu = len(units)
            for i in range(nu + 2):
                if 2 <= i:
                    stageC1(i - 2, *units[i - 2])
                if 1 <= i <= nu:
                    stageB(i - 1, *units[i - 1])
                if i < nu:
                    c, qb, hp = units[i]
                    if qb == 0 and hp == 0:
                        qk_proj(c)
                    stageA(i, c, qb, hp)
                if 2 <= i:
                    c, qb, hp = units[i - 2]
                    if qb == 0 and hp == 0:
                        v_proj(c)
                    stageC(i - 2, c, qb, hp)
                    if qb == 15 and hp == 1 and c + 2 < KC:
                        load_weights(c + 2)
            P.barrier()

        b7 = [0]

        def psum7():
            i = b7[0]
            b7[0] = (i + 1) % 7
            return banks[i], bres[i]

        mixers = [mixer_conv, mixer_hgrn, mixer_pool, mixer_sb]
        for i in range(DEPTH):
            if ("mix%d" % i) in phases:
                norm_phase(lambda c, i=i: DER[:, i * 16 + c:i * 16 + c + 1], lambda c, i=i: modv(i, 0, c), "mix")
                P.barrier()
                mixers[i](i)
            if ("hdump%d" % i) in phases:
                norm_phase(lambda c, i=i: DER[:, i * 16 + c:i * 16 + c + 1], lambda c, i=i: modv(i, 0, c), "mix")
                P.barrier()
                for tt in range(4):
                    P.op("act", ACT(XT[:, :, tt * 512:(tt + 1) * 512], HT[:, :, tt * 512:(tt + 1) * 512], AF.Copy),
                         r=[rHT[tt]], w=[rXT[tt]])
                P.barrier()
            if ("fdump%d" % i) in phases:
                LOG, rLOG, _ = norm_phase(lambda c, i=i: DER[:, i * 16 + 8 + c:i * 16 + 9 + c], lambda c, i=i: modv(i, 3, c),
                                          "ffn", layer=i)
                P.barrier()
                for tt in range(4):
                    P.op("act", ACT(XT[:, :, tt * 512:(tt + 1) * 512], HT[:, :, tt * 512:(tt + 1) * 512], AF.Copy),
                         r=[rHT[tt]], w=[rXT[tt]])
                P.op("act", ACT(XT[:, 0, 0:576], LOG[:].rearrange("p a b -> p (a b)"), AF.Copy), r=[rLOG], w=[rXT[0]])
                P.barrier()
            if ("ffn%d" % i) in phases:
                LOG, rLOG, _ = norm_phase(lambda c, i=i: DER[:, i * 16 + 8 + c:i * 16 + 9 + c], lambda c, i=i: modv(i, 3, c),
                                          "ffn", layer=i)
                moe_phase(i, LOG, rLOG)
        if dbg:
            rOUT = P.res("OUT")
            for i in range(4):
                P.dma("sp", out_d[:, :, i * 512:(i + 1) * 512], XT[:, :, i * 512:(i + 1) * 512], r=[rXT[i]], w=[], dres=rOUT)
        else:
            norm_phase(lambda c: V("fin", c), None, "final")
        P.barrier()
        block = es.enter_context(nc.Block())
        P.replay(block)
    return nc


ALL_PHASES = ["mods"] + [p for i in range(DEPTH) for p in ("mix%d" % i, "ffn%d" % i)]


def _consts():
    ident = np.eye(128, dtype=np.float32)
    j = np.arange(128)[:, None]
    s = np.arange(128)[None, :]
    tri = (j > s).astype(np.float32)
    mask2 = ((j <= s) & ((j // 64) == (s // 64))).astype(np.float32)
    cA = np.concatenate([ident, tri, mask2], axis=1)
    t = np.arange(512)[None, :]
    sbm = np.concatenate([((r * 128 + j) < t).astype(np.float32) for r in range(4)], axis=1)
    scm = np.broadcast_to((np.arange(S) % 64 != 0).astype(np.float32)[None, :], (128, S)).copy()
    invc = np.zeros((128, 64), np.float32)
    for gi, w in enumerate((2, 4, 8, 16)):
        invc[:, gi * 16:(gi + 1) * 16] = 1.0 / np.minimum(np.arange(16) + 1, w)
    cB = np.zeros((128, NCB), np.float32)
    cB[:, 0:16] = 256.0 * np.arange(16)[None, :]
    cB[:, 16:80] = np.arange(64)[None, :]
    cB[:, 80] = np.arange(128)
    return cA, sbm, scm, invc, cB


_CACHE = {}


def kernel(**inp):
    return run(inp, ALL_PHASES, False)


def run(inp, phases, dbg, cores=8):
    f = lambda a: np.ascontiguousarray(np.asarray(a, np.float32))
    key = (tuple(phases), dbg)
    if key not in _CACHE:
        _CACHE[key] = build_program(phases, dbg)
    nc = _CACHE[key]
    cA, sbm, scm, invc, cB = _consts()
    x = f(inp["x"])
    shared = {
        "cA": cA, "sbmask": sbm, "scanmask": scm, "invc": invc, "cB": cB,
        "ada_w": f(inp["ada_w"]),
        "conv_w_in": f(inp["conv_w_in"][0]), "conv_w_out": f(inp["conv_w_out"][0]),
        "hgrn_w_in": f(inp["hgrn_w_in"][0]), "hgrn_w_out": f(inp["hgrn_w_out"][0]),
        "pool_w": f(inp["pool_w"][0]),
        "sb_w_qkv": f(inp["sb_w_qkv"][0]), "sb_w_out": f(inp["sb_w_out"][0]),
    }
    shared["wgL"] = np.ascontiguousarray(
        f(inp["moe_w_gate"]).reshape(DEPTH, NEXP, KC, 128, FH).transpose(0, 1, 3, 2, 4)).reshape(DEPTH * NEXP * 128, KC * FH)
    shared["wuL"] = np.ascontiguousarray(
        f(inp["moe_w_up"]).reshape(DEPTH, NEXP, KC, 128, FH).transpose(0, 1, 3, 2, 4)).reshape(DEPTH * NEXP * 128, KC * FH)
    shared["wdL"] = np.ascontiguousarray(
        f(inp["moe_w_down"]).reshape(DEPTH, NEXP, 4, 128, D).transpose(0, 1, 3, 2, 4)).reshape(DEPTH * NEXP * 128, 4 * D)
    wr = np.concatenate([f(inp["moe_w_rg"]), f(inp["moe_w_re"])], axis=2)
    shared["wr"] = np.ascontiguousarray(wr.reshape(DEPTH, KC, 128, 36).transpose(2, 0, 1, 3))
    br = np.concatenate([f(inp["moe_b_rg"]), f(inp["moe_b_re"])], axis=1).reshape(1, DEPTH * 36)
    shared["br"] = np.ascontiguousarray(np.broadcast_to(br, (128, DEPTH * 36)))
    in_maps = []
    for b in range(cores):
        vec = np.zeros((128, NVEC), np.float32)

        def put(name, arr):
            a = _fm(arr)
            vec[:, VOFF[name]:VOFF[name] + a.shape[1]] = a
        for i in range(DEPTH):
            put("gmix%d" % i, inp["norm_mix_g"][i])
            put("gffn%d" % i, inp["norm_ffn_g"][i])
            put("adab%d" % i, inp["ada_b"][i])
            put("lbl%d" % i, inp["hgrn_lb_logits"][i])
        put("fin", inp["final_g"])
        put("cbin", inp["conv_b_in"][0])
        put("cbdw", inp["conv_b_dw"][0])
        put("clng", inp["conv_ln_g"][0])
        put("clnb", inp["conv_ln_b"][0])
        put("hng", inp["hgrn_norm_g"][0])
        put("pb", inp["pool_b"][0])
        put("psc", inp["pool_scale"][0])
        wdw = f(inp["conv_w_dw"][0])
        vec[:, VOFF["wdw"]:VOFF["wdw"] + 248] = wdw.reshape(31, KC, 128).transpose(2, 1, 0).reshape(128, 248)
        put("c", inp["c"][b])
        m = dict(shared)
        m["vecs"] = vec
        m["xT"] = np.ascontiguousarray(x[b].T.reshape(KC, 128, S).transpose(1, 0, 2))
        in_maps.append(m)
    res = run_bass_kernel_spmd(nc, in_maps, core_ids=list(range(cores)))
    outs = []
    for b in range(cores):
        oT = res.results[b]["outT"]
        outs.append(np.ascontiguousarray(oT.transpose(1, 0, 2).reshape(D, S).T))
    return np.stack(outs, axis=0).astype(np.float32)
```

```python
import numpy as np
from contextlib import ExitStack
import concourse.bass as bass
import concourse.mybir as mybir
from concourse.bass_utils import run_bass_kernel_spmd

F32 = mybir.dt.float32
BF16 = mybir.dt.bfloat16
AF = mybir.ActivationFunctionType
ALU = mybir.AluOpType
AX = mybir.AxisListType

D = 1024
S = 2048
KC = 8
DEPTH = 4
EPS = 1e-6
NEXP = 32
FH = 512
L0 = 98304
M0 = L0 + 24576
ARENA_BYTES = 204800
BIG = 1.0e30
NSTEP = 48
NSUB = 2 * NSTEP
NSLOT = NSTEP * 256
NCB = 84
I32 = mybir.dt.int32

ENGS = ("pe", "act", "dve", "pool", "sp")


class Slot:
    __slots__ = ("sem", "cnt", "eng")

    def __init__(self, sem):
        self.sem = sem
        self.cnt = 0
        self.eng = None


class Res:
    __slots__ = ("name", "w", "r", "slot")

    def __init__(self, name):
        self.name = name
        self.w = None
        self.r = {}
        self.slot = None


class Prog:
    def __init__(self, nc, es):
        self.nc = nc
        self.es = es
        self.q = {e: [] for e in ENGS}
        self.cnt = {e: 0 for e in ENGS}
        self.seen = {e: {} for e in ENGS}
        self.esem = {e: es.enter_context(nc.semaphore("sem_" + e)) for e in ("pe", "act", "dve", "pool")}
        self.slots = []
        self.free = {}
        self.live = []
        self.regs = {}

    def res(self, name):
        return Res(name)

    def _sem_of(self, key):
        return self.esem[key] if isinstance(key, str) else key.sem

    def _wait(self, e, toks, strict=False):
        need = {}
        for t in toks:
            if t is None:
                continue
            k, v = t
            if (not strict) and k == e and e == "pe":
                continue
            if need.get(k, 0) < v:
                need[k] = v
        for k, v in need.items():
            if self.seen[e].get(k, 0) >= v:
                continue
            self.seen[e][k] = v
            sem = self._sem_of(k)
            self.q[e].append(lambda eng, sem=sem, v=v: eng.wait_ge(sem, v))

    def _collect(self, r, w):
        toks = []
        for x in r:
            toks.append(x.w)
        for x in w:
            toks.append(x.w)
            toks.extend(x.r.items())
        return toks

    def op(self, e, fns, r=(), w=()):
        if callable(fns):
            fns = [fns]
        self._wait(e, self._collect(r, w))
        self.cnt[e] += 1
        n = self.cnt[e]
        sem = self.esem[e]
        last = len(fns) - 1
        for i, fn in enumerate(fns):
            if i == last:
                self.q[e].append(lambda eng, fn=fn, sem=sem: fn(eng).then_inc(sem, 1))
            else:
                self.q[e].append(fn)
        tok = (e, n)
        for x in r:
            if x.r.get(e, 0) < n:
                x.r[e] = n
        for x in w:
            x.w = tok
            x.r = {}
        return tok

    def dma(self, e, out, in_, r=(), w=(), dres=None, nowait_w=()):
        return self.dmaf(e, lambda eng, out=out, in_=in_: eng.dma_start(out=out, in_=in_), r, w, dres, nowait_w)

    def dmaf(self, e, fn, r=(), w=(), dres=None, nowait_w=()):
        if dres is None:
            dres = w[0]
        if dres.slot is None:
            fl = self.free.setdefault(e, [])
            if fl:
                dres.slot = fl.pop()
            else:
                dres.slot = Slot(self.es.enter_context(self.nc.semaphore("dsem_%s_%d" % (e, len(self.slots)))))
                dres.slot.eng = e
                self.slots.append(dres.slot)
            self.live.append(dres)
        slot = dres.slot
        assert slot.eng == e, (dres.name, slot.eng, e)
        self._wait(e, self._collect(r, w), strict=True)
        slot.cnt += 16
        v = slot.cnt
        sem = slot.sem
        self.q[e].append(lambda eng, fn=fn, sem=sem: fn(eng).then_inc(sem, 16))
        tok = (slot, v)
        for x in r:
            x.r[slot] = v
        for x in w:
            x.w = tok
            x.r = {}
        for x in nowait_w:
            x.w = tok
        return tok

    def barrier(self):
        toks = [(e, self.cnt[e]) for e in ("pe", "act", "dve", "pool") if self.cnt[e] > 0]
        toks += [(d, d.cnt) for d in self.slots if d.cnt > 0]
        for e in ENGS:
            self._wait(e, toks)
        for d in self.live:
            self.free.setdefault(d.slot.eng, []).append(d.slot)
            d.slot = None
        self.live = []

    def replay(self, block):
        def mk(name):
            def f(eng):
                for fn in self.q[name]:
                    fn(eng)
            return f
        block.tensor(mk("pe"))
        block.scalar(mk("act"))
        block.vector(mk("dve"))
        block.gpsimd(mk("pool"))
        block.sync(mk("sp"))


def MM(o, l, r, st=True, sp=True):
    return lambda e: e.matmul(o, lhsT=l, rhs=r, start=st, stop=sp)


def TR(o, i, ident):
    return lambda e: e.transpose(o, i, ident)


def ACT(o, i, f, bias=None, scale=None):
    kw = {}
    if bias is not None:
        kw["bias"] = bias
    if scale is not None:
        kw["scale"] = scale
    return lambda e: e.activation(out=o, in_=i, func=f, **kw)


def TS(o, i, s1, s2, op0, op1=None):
    if op1 is None:
        return lambda e: e.tensor_scalar(out=o, in0=i, scalar1=s1, scalar2=None, op0=op0)
    return lambda e: e.tensor_scalar(out=o, in0=i, scalar1=s1, scalar2=s2, op0=op0, op1=op1)


def TT(o, a, b, op):
    return lambda e: e.tensor_tensor(out=o, in0=a, in1=b, op=op)


def STT(o, a, s, b, op0, op1):
    return lambda e: e.scalar_tensor_tensor(out=o, in0=a, scalar=s, in1=b, op0=op0, op1=op1)


def CP(o, i):
    return lambda e: e.tensor_copy(out=o, in_=i)


def MS(o, v):
    return lambda e: e.memset(o, v)


def _vec_layout():
    off = {}
    n = 0

    def add(name, cols):
        nonlocal n
        off[name] = n
        n += cols
    for i in range(DEPTH):
        add("gmix%d" % i, 8)
        add("gffn%d" % i, 8)
        add("adab%d" % i, 48)
        add("lbl%d" % i, 8)
    add("fin", 8)
    add("cbin", 16)
    add("cbdw", 8)
    add("clng", 8)
    add("clnb", 8)
    add("hng", 8)
    add("pb", 8)
    add("psc", 8)
    add("wdw", 248)
    add("c", 8)
    return off, n


VOFF, NVEC = _vec_layout()


def _fm(v):
    v = np.asarray(v, np.float32).reshape(-1, 128)
    return np.ascontiguousarray(v.T)


def build_program(phases, dbg=False):
    nc = bass.Bass("TRN2", target_bir_lowering=False)
    dt_ = nc.dram_tensor
    xT_d = dt_("xT", [128, KC, S], F32, kind="ExternalInput").ap()
    vec_d = dt_("vecs", [128, NVEC], F32, kind="ExternalInput").ap()
    br_d = dt_("br", [128, DEPTH * 36], F32, kind="ExternalInput").ap()
    wr_d = dt_("wr", [128, DEPTH, KC, 36], F32, kind="ExternalInput").ap()
    cA_d = dt_("cA", [128, 384], F32, kind="ExternalInput").ap()
    sbm_d = dt_("sbmask", [128, 2048], F32, kind="ExternalInput").ap()
    scm_d = dt_("scanmask", [128, 2048], F32, kind="ExternalInput").ap()
    invc_d = dt_("invc", [128, 64], F32, kind="ExternalInput").ap()
    cB_d = dt_("cB", [128, NCB], F32, kind="ExternalInput").ap()
    adaw_d = dt_("ada_w", [DEPTH, D, 6 * D], F32, kind="ExternalInput").ap()
    cwin_d = dt_("conv_w_in", [D, 2 * D], F32, kind="ExternalInput").ap()
    cwout_d = dt_("conv_w_out", [D, D], F32, kind="ExternalInput").ap()
    hwin_d = dt_("hgrn_w_in", [D, 4 * D], F32, kind="ExternalInput").ap()
    hwout_d = dt_("hgrn_w_out", [D, D], F32, kind="ExternalInput").ap()
    pw_d = dt_("pool_w", [4, 256, 256], F32, kind="ExternalInput").ap()
    sqkv_d = dt_("sb_w_qkv", [D, 3 * D], F32, kind="ExternalInput").ap()
    swout_d = dt_("sb_w_out", [D, D], F32, kind="ExternalInput").ap()
    wgL_d = dt_("wgL", [DEPTH * NEXP * 128, 4096], F32, kind="ExternalInput").ap()
    wuL_d = dt_("wuL", [DEPTH * NEXP * 128, 4096], F32, kind="ExternalInput").ap()
    wdL_d = dt_("wdL", [DEPTH * NEXP * 128, 4096], F32, kind="ExternalInput").ap()
    xs_d = dt_("xs_scr", [NSLOT, D], BF16, kind="Internal").ap()
    ys_d = dt_("ys_scr", [NSLOT, D], BF16, kind="Internal").ap()
    out_d = dt_("outT", [128, KC, S], F32, kind="ExternalOutput").ap()

    es = ExitStack()
    with es:
        arena = es.enter_context(nc.sbuf_tensor("arena", [128, ARENA_BYTES // 2], BF16))

        def carve(off, n, dt=BF16, parts=128):
            if dt == BF16:
                a = arena[0:parts, off // 2: off // 2 + n]
            else:
                a = arena[0:parts, off // 2: off // 2 + 2 * n].bitcast(F32)
            return a

        def v3(ap, a):
            return ap.rearrange("p (a b) -> p a b", a=a)

        sb = lambda name, shape, dt: es.enter_context(nc.sbuf_tensor(name, shape, dt))
        VEC = sb("VEC", [128, NVEC], F32)
        BRB = sb("BRB", [128, DEPTH * 36], F32)
        MOD = sb("MOD", [128, DEPTH * 48], F32)
        DER = sb("DER", [128, DEPTH * 16 + 32], F32)
        CST = sb("CST", [128, 384], F32)
        IDB = sb("IDB", [128, 128], BF16)
        ONESB = sb("ONESB", [128, 128], BF16)
        TRIB = sb("TRIB", [128, 128], BF16)
        CA = sb("CA", [128, 8], F32)
        SMALL = sb("SMALL", [128, 64], F32)
        CSTB = sb("CSTB", [128, NCB], F32)
        DIDX = sb("DIDX", [128, 32], I32)
        WIDX = sb("WIDX", [128, NSTEP], I32)
        GATE = sb("GATE", [128, 32], F32)
        NEGM = sb("NEGM", [128, 128], BF16)
        TH16 = CSTB[:, 0:16]
        BIDX = CSTB[:, 16:80]
        PIDX = CSTB[:, 80:81]
        banks = [es.enter_context(nc.psum_tensor("bank%d" % i, [128, 512], F32)) for i in range(8)]
        IDF = CST[:, 0:128]
        MASK2 = CST[:, 256:384]

        P = Prog(nc, es)
        bres = [P.res("bank%d" % i) for i in range(8)]
        bptr = [0]

        def psum():
            i = bptr[0]
            bptr[0] = (i + 1) % 8
            return banks[i], bres[i]

        XT = v3(carve(0, KC * S, F32), KC)
        HT = v3(carve(65536, KC * S, BF16), KC)
        rXT = [P.res("XT%d" % i) for i in range(4)]
        rHT = [P.res("HT%d" % i) for i in range(4)]
        rC = P.res("consts")
        rVEC = P.res("VEC")
        rMOD = P.res("MOD")

        def V(name, c0=0, n=1):
            o = VOFF[name] + c0
            return VEC[:, o:o + n]

        P.dma("sp", VEC[:], vec_d, w=[rVEC])
        P.dma("sp", BRB[:], br_d, w=[rC])
        P.dma("sp", CST[:], cA_d, w=[rC])
        P.dma("sp", CSTB[:], cB_d, w=[rC])

        def _mk_regs(eng):
            P.regs["wb"] = eng.alloc_register("wbound")
            eng.reg_mov(P.regs["wb"], DEPTH * NEXP * 128 - 1)
            P.regs["xb"] = eng.alloc_register("xbound")
            eng.reg_mov(P.regs["xb"], NSLOT - 1)
        P.q["pool"].append(_mk_regs)
        for i in range(4):
            P.dma("sp", XT[:, :, i * 512:(i + 1) * 512], xT_d[:, :, i * 512:(i + 1) * 512], w=[rXT[i]])
        HTflat = carve(65536, KC * S, BF16)
        rZ = P.res("Z")
        P.op("pool", MS(HTflat, 0.0), w=rHT)
        for j in range(NSLOT // 2048):
            P.dma("sp", xs_d[j * 2048:(j + 1) * 2048, :].rearrange("(p a) n -> p (a n)", p=128), HTflat,
                  r=rHT, w=[], dres=rZ, nowait_w=[rZ])
        P.op("dve", CP(IDB[:], CST[:, 0:128]), r=[rC], w=[rC])
        P.op("dve", CP(TRIB[:], CST[:, 128:256]), r=[rC], w=[rC])
        P.op("dve", MS(ONESB[:], 1.0), w=[rC])
        P.op("act", ACT(CA[:], V("c", 0, 8), AF.Silu), r=[rVEC], w=[rC])

        NAS = 2
        adaw_slots = [v3(carve(M0 + s_ * 16384, KC * 512, F32), KC) for s_ in range(NAS)]
        r_adaw = [P.res("adaw%d" % s_) for s_ in range(NAS)]
        ROW = carve(M0 + 32768, 6 * D, F32, parts=1)
        CAb = CA
        rROW = P.res("ROW")
        P.op("dve", MS(SMALL[:, 0:1], EPS), w=[rC])
        P.op("dve", MS(SMALL[:, 1:2], 1.0), w=[rC])
        if "mods" in phases:
            for i in range(DEPTH):
                for blk in range(12):
                    sl = (i * 12 + blk) % NAS
                    P.dma("sp", adaw_slots[sl][:],
                          adaw_d[i, :, blk * 512:(blk + 1) * 512].rearrange("(k p) n -> p k n", p=128),
                          w=[r_adaw[sl]])
                    ps, pr = psum()
                    P.op("pe", [MM(ps[0:1, :], CAb[:, k:k + 1], adaw_slots[sl][:, k, :], st=(k == 0), sp=(k == KC - 1))
                                for k in range(KC)], r=[r_adaw[sl], rC], w=[pr])
                    P.op("act", ACT(ROW[0:1, blk * 512:(blk + 1) * 512], ps[0:1, :], AF.Copy), r=[pr], w=[rROW])
                ps, pr = psum()
                P.op("pe", [MM(ps[:, j:j + 1], ROW[0:1, j * 128:(j + 1) * 128], SMALL[0:1, 1:2]) for j in range(48)],
                     r=[rROW, rC], w=[pr])
                P.op("dve", TT(MOD[:, i * 48:(i + 1) * 48], ps[:, 0:48], V("adab%d" % i, 0, 48), ALU.add),
                     r=[pr, rVEC], w=[rMOD])
                P.op("dve", STT(DER[:, i * 16:i * 16 + 8], MOD[:, i * 48 + 8:i * 48 + 16], 1.0, V("gmix%d" % i, 0, 8),
                                ALU.add, ALU.mult), r=[rMOD, rVEC], w=[rMOD])
                P.op("dve", STT(DER[:, i * 16 + 8:i * 16 + 16], MOD[:, i * 48 + 32:i * 48 + 40], 1.0,
                                V("gffn%d" % i, 0, 8), ALU.add, ALU.mult), r=[rMOD, rVEC], w=[rMOD])
        P.barrier()

        def modv(i, which, c):
            o = i * 48 + which * 8 + c
            return MOD[:, o:o + 1]

        def norm_phase(A_of, B_of, out_mode, layer=0):
            base = M0
            SQ = v3(carve(base, KC * 256, BF16), KC)
            TMPs = [v3(carve(base + 4096 + s_ * 8192, KC * 256, F32), KC) for s_ in range(2)]
            RSTDs = [carve(base + 4096 + 16384 + s_ * 1024, 256, F32) for s_ in range(2)]
            rSQ = P.res("SQ")
            rTMP = [P.res("TMP0"), P.res("TMP1")]
            rRS = [P.res("RS0"), P.res("RS1")]
            rLOG = P.res("LOG")
            LOG = None
            if out_mode == "ffn":
                LOG = v3(carve(base + 24576, 16 * 36, F32), 16)
                WR = v3(carve(base + 24576 + 4096, KC * 36, F32), KC)
                rWR = P.res("WR")
                P.dma("sp", WR[:], wr_d[:, layer, :, :], w=[rWR])
            rOUT = [P.res("OUT0"), P.res("OUT1")]
            def n1a(tt):
                sl = slice(tt * 256, (tt + 1) * 256)
                xt_r = rXT[tt // 2]
                P.op("act", ACT(SQ[:], XT[:, :, sl], AF.Square), r=[xt_r], w=[rSQ])
                ps, pr = psum()
                P.op("pe", [MM(ps[:, 0:256], ONESB[:], SQ[:, c, :], st=(c == 0), sp=(c == KC - 1)) for c in range(KC)],
                     r=[rSQ, rC], w=[pr])
                return ps, pr

            def n1b(tt, ps, pr):
                sl = slice(tt * 256, (tt + 1) * 256)
                xt_r = rXT[tt // 2]
                s_ = tt % 2
                P.op("act", ACT(RSTDs[s_], ps[:, 0:256], AF.Sqrt, bias=SMALL[:, 0:1], scale=1.0 / D), r=[pr, rC], w=[rRS[s_]])
                P.op("dve", lambda e, o=RSTDs[s_]: e.reciprocal(out=o, in_=o), r=[rRS[s_]], w=[rRS[s_]])
                for c in range(KC):
                    P.op("dve", STT(TMPs[s_][:, c, :], XT[:, c, sl], A_of(c), RSTDs[s_], ALU.mult, ALU.mult),
                         r=[xt_r, rRS[s_], rMOD], w=[rTMP[s_]])

            def n2(tt):
                sl = slice(tt * 256, (tt + 1) * 256)
                s_ = tt % 2
                if out_mode == "mix":
                    for c in range(KC):
                        P.op("act", ACT(HT[:, c, sl], TMPs[s_][:, c, :], AF.Identity, bias=B_of(c), scale=1.0),
                             r=[rTMP[s_], rMOD], w=[rHT[tt // 2]])
                elif out_mode == "ffn":
                    for c in range(KC):
                        P.op("act", ACT(TMPs[s_][:, c, :], TMPs[s_][:, c, :], AF.Identity, bias=B_of(c), scale=1.0),
                             r=[rMOD], w=[rTMP[s_]])
                    P.op("pool", CP(HT[:, :, sl], TMPs[s_][:]), r=[rTMP[s_]], w=[rHT[tt // 2]])
                    for sub in range(2):
                        ps2, pr2 = psum()
                        P.op("pe", [MM(ps2[:, 0:36], TMPs[s_][:, k, sub * 128:(sub + 1) * 128], WR[:, k, :],
                                       st=(k == 0), sp=(k == KC - 1)) for k in range(KC)],
                             r=[rTMP[s_], rWR], w=[pr2])
                        P.op("dve", TT(LOG[:, tt * 2 + sub, :], ps2[:, 0:36], BRB[:, layer * 36:(layer + 1) * 36], ALU.add),
                             r=[pr2, rC], w=[rLOG])
                else:
                    P.dma("sp", out_d[:, :, sl], TMPs[s_][:], r=[rTMP[s_]], w=[], dres=rOUT[s_])

            pend = n1a(0)
            n1b(0, *pend)
            for tt in range(8):
                if tt + 1 < 8:
                    pend = n1a(tt + 1)
                n2(tt)
                if tt + 1 < 8:
                    n1b(tt + 1, *pend)
            return LOG, rLOG, rOUT

        def moe_phase(layer, LOG, rLOG):
            base = M0
            rb = base + 32768
            nR = [0]

            def rt(n):
                a = carve(rb + nR[0], n, F32)
                nR[0] += n * 4
                return a
            rR = P.res("route")
            GL = LOG[:, :, 0:4]
            EL = LOG[:, :, 4:36]
            GMAX = rt(16)
            GSH = v3(rt(64), 16)
            OHG = v3(rt(64), 16)
            GE = v3(rt(64), 16)
            GSUM = rt(16)
            GP = rt(16)
            PEN = v3(rt(64), 16)
            ELM = v3(rt(512), 16)
            V1 = rt(16)
            D1 = v3(rt(512), 16)
            OH1 = v3(rt(512), 16)
            ELM2 = v3(rt(512), 16)
            V2 = rt(16)
            OH2 = v3(rt(512), 16)
            DV = rt(16)
            E21 = rt(16)
            P1 = rt(16)
            C1 = rt(16)
            C2 = rt(16)

            def bc3(a, n):
                return a.unsqueeze(2).to_broadcast([128, 16, n])

            def dv(fn, r=(), w=()):
                P.op("dve", fn, r=list(r) + [rLOG, rR], w=list(w) + [rR])
            dv(lambda e: e.tensor_reduce(out=GMAX, in_=GL, axis=AX.X, op=ALU.max))
            dv(TT(GSH[:], GL, bc3(GMAX, 4), ALU.subtract))
            dv(TS(OHG[:], GSH[:], 0.0, None, ALU.is_equal))
            P.op("act", ACT(GE[:], GSH[:], AF.Exp), r=[rR], w=[rR])
            dv(lambda e: e.tensor_reduce(out=GSUM, in_=GE[:], axis=AX.X, op=ALU.add))
            dv(lambda e: e.reciprocal(out=GP, in_=GSUM))
            dv(TS(PEN[:], OHG[:], -1.0, BIG, ALU.add, ALU.mult))
            ELM4 = ELM[:].rearrange("p a (g x) -> p a g x", g=4)
            EL4 = EL.rearrange("p a (g x) -> p a g x", g=4)
            dv(TT(ELM4, EL4, PEN[:].unsqueeze(3).to_broadcast([128, 16, 4, 8]), ALU.add))
            dv(lambda e: e.tensor_reduce(out=V1, in_=ELM[:], axis=AX.X, op=ALU.max))
            dv(TT(D1[:], ELM[:], bc3(V1, 32), ALU.subtract))
            dv(TS(OH1[:], D1[:], 0.0, None, ALU.is_equal))
            dv(STT(ELM2[:], OH1[:], -BIG, ELM[:], ALU.mult, ALU.add))
            dv(lambda e: e.tensor_reduce(out=V2, in_=ELM2[:], axis=AX.X, op=ALU.max))
            dv(TT(D1[:], ELM2[:], bc3(V2, 32), ALU.subtract))
            dv(TS(OH2[:], D1[:], 0.0, None, ALU.is_equal))
            dv(TT(DV, V2, V1, ALU.subtract))
            P.op("act", ACT(E21, DV, AF.Exp), r=[rR], w=[rR])
            dv(TS(P1, E21, 1.0, None, ALU.add))
            dv(lambda e: e.reciprocal(out=P1, in_=P1))
            dv(TT(C1, P1, GP, ALU.mult))
            dv(TT(C2, E21, C1, ALU.mult))
            rIDX = P.res("IDX")
            MBo = rb + nR[0]
            nR[0] += 1024
            MB = v3(carve(MBo, 512, BF16), 16)
            CNT = rt(32)
            CMP3 = v3(rt(512), 32)
            NBK = rt(32)
            T3 = v3(rt(1024), 32)
            LE3 = v3(rt(1024), 32)
            PEND = rt(32)
            PST = rt(32)
            DA = v3(rt(512), 16)
            DF = rt(32)
            CMPB3 = v3(rt(NSTEP * 32), NSTEP)
            EBf = rt(NSTEP)
            INV = rt(NSTEP)
            WF = rt(NSTEP)
            BST = BIDX[:, 0:NSTEP]
            dv(TT(MB[:], OH1[:], OH2[:], ALU.add))
            psR, prR = psum()
            fns = []
            for t in range(16):
                fns.append(MM(psR[:, t * 32:(t + 1) * 32], TRIB[:], MB[:, t, :], st=True, sp=(t == 15)))
                for t2 in range(t + 1, 16):
                    fns.append(MM(psR[:, t * 32:(t + 1) * 32], ONESB[:], MB[:, t2, :], st=False, sp=(t2 == 15)))
            P.op("pe", fns, r=[rR, rC], w=[prR])
            psC, prC = psum()
            P.op("pe", [MM(psC[:, 0:32], ONESB[:], MB[:, t, :], st=(t == 0), sp=(t == 15)) for t in range(16)],
                 r=[rR, rC], w=[prC])
            dv(CP(CNT, psC[:, 0:32]), r=[prC])
            dv(TT(CMP3[:], CNT.unsqueeze(2).to_broadcast([128, 32, 16]), TH16.unsqueeze(1).to_broadcast([128, 32, 16]),
                  ALU.is_gt), r=[rC])
            dv(lambda e: e.tensor_reduce(out=NBK, in_=CMP3[:], axis=AX.X, op=ALU.add))
            IO32 = BIDX[:, 0:32]
            dv(TT(LE3[:], IO32.unsqueeze(1).to_broadcast([128, 32, 32]), IO32.unsqueeze(2).to_broadcast([128, 32, 32]),
                  ALU.is_le), r=[rC])
            dv(TT(T3[:], LE3[:], NBK.unsqueeze(1).to_broadcast([128, 32, 32]), ALU.mult))
            dv(lambda e: e.tensor_reduce(out=PEND, in_=T3[:], axis=AX.X, op=ALU.add))
            dv(TT(PST, PEND, NBK, ALU.subtract))
            dv(TS(PST, PST, 256.0, None, ALU.mult))
            dv(TT(DA[:], v3(psR[:], 16), PST.unsqueeze(1).to_broadcast([128, 16, 32]), ALU.add), r=[prR])
            dv(TT(D1[:], OH1[:], DA[:], ALU.mult))
            dv(lambda e: e.tensor_reduce(out=DF[:, 0:16], in_=D1[:], axis=AX.X, op=ALU.add))
            dv(TT(D1[:], OH2[:], DA[:], ALU.mult))
            dv(lambda e: e.tensor_reduce(out=DF[:, 16:32], in_=D1[:], axis=AX.X, op=ALU.add))
            dv(CP(DIDX[:], DF), w=[rIDX])
            dv(CP(GATE[:, 0:16], C1), w=[rIDX])
            dv(CP(GATE[:, 16:32], C2), w=[rIDX])
            dv(TT(CMPB3[:], PEND.unsqueeze(1).to_broadcast([128, NSTEP, 32]), BST.unsqueeze(2).to_broadcast([128, NSTEP, 32]),
                  ALU.is_le), r=[rC])
            dv(lambda e: e.tensor_reduce(out=EBf, in_=CMPB3[:], axis=AX.X, op=ALU.add))
            dv(TS(INV, BST, PEND[:, 31:32], None, ALU.is_ge), r=[rC])
            dv(TS(EBf, EBf, 31.0, None, ALU.min))
            dv(TS(WF, EBf, float(layer * NEXP), 128.0, ALU.add, ALU.mult))
            dv(TS(WF, WF, PIDX, None, ALU.add), r=[rC])
            dv(STT(WF, INV, 1.0e6, WF, ALU.mult, ALU.add))
            dv(CP(WIDX[:], WF), w=[rIDX])
            P.barrier()

            IOA = bass.IndirectOffsetOnAxis
            Wslots = [L0, M0]

            def wviews(slot):
                o = Wslots[slot]
                return carve(o, 4096, BF16), carve(o + 8192, 4096, BF16), carve(o + 16384, 4096, BF16)
            rWGU = [P.res("WGU0"), P.res("WGU1")]
            rWD = [P.res("WD0"), P.res("WD1")]

            def wgather(dst, src, bi):
                return lambda eng: eng.indirect_dma_start(out=dst, out_offset=None, in_=src,
                                                          in_offset=IOA(ap=WIDX[:, bi:bi + 1], axis=0),
                                                          bounds_check=P.regs["wb"], oob_is_err=False)

            def load_wgu(st):
                wg, wu, _ = wviews(st % 2)
                P.dmaf("pool", wgather(wg, wgL_d, st), r=[rIDX], w=[rWGU[st % 2]])
                P.dmaf("pool", wgather(wu, wuL_d, st), r=[rIDX], w=[rWGU[st % 2]])

            def load_wd(st):
                _, _, wdn = wviews(st % 2)
                P.dmaf("pool", wgather(wdn, wdL_d, st), r=[rIDX], w=[rWD[st % 2]])

            eb = M0 + 24576
            HTOK = [carve(eb + s_ * 2048, 1024, BF16) for s_ in range(2)]
            NXS = 4
            XSL = [carve(eb + 49152 + s_ * 2048, 1024, BF16) for s_ in range(NXS)]
            XF = [carve(eb + 8192 + s_ * 2048, 1024, BF16) for s_ in range(2)]
            SS = [carve(eb + 12288 + s_ * 2048, 512, F32) for s_ in range(2)]
            HIDS = [carve(eb + 16384 + s_ * 1024, 512, BF16) for s_ in range(2)]
            HIDT = [carve(eb + 18432 + s_ * 1024, 512, BF16) for s_ in range(2)]
            YS = [carve(eb + 20480 + s_ * 2048, 1024, BF16) for s_ in range(2)]
            NYG = 4
            YG = [[carve(eb + 24576 + (k * NYG + s_) * 2048, 1024, BF16) for s_ in range(NYG)] for k in range(2)]
            YC = [carve(eb + 40960 + s_ * 4096, 1024, F32) for s_ in range(2)]
            two = lambda n: [P.res(n + "0"), P.res(n + "1")]
            rHTOK, rXF, rSS, rHIDS, rHIDT, rYS = two("HTOK"), two("XF"), two("SS"), two("HIDS"), two("HIDT"), two("YS")
            rXSL = [P.res("XSL%d" % i) for i in range(NXS)]
            rXs, rYs = two("Xs"), two("Ys")
            rYG = [[P.res("YG%d_%d" % (k, i)) for i in range(NYG)] for k in range(2)]
            rYC = two("YC")

            load_wgu(0)
            load_wd(0)
            load_wgu(1)
            load_wd(1)
            for t16 in range(16):
                b = t16 % 2
                ps, pr = psum()
                pb_ = ps[:].bitcast(BF16)
                P.op("pe", [TR(pb_[:, k * 128:(k + 1) * 128], HT[:, k, t16 * 128:(t16 + 1) * 128], IDB[:]) for k in range(KC)],
                     r=[rHT[t16 // 4], rC], w=[pr])
                if b == 0:
                    P.op("act", ACT(HTOK[b], pb_[:, 0:1024], AF.Copy), r=[pr], w=[rHTOK[b]])
                else:
                    P.op("dve", CP(HTOK[b], pb_[:, 0:1024]), r=[pr], w=[rHTOK[b]])
                for k in range(2):
                    c = k * 16 + t16
                    P.dmaf("pool", lambda eng, b=b, c=c: eng.indirect_dma_start(
                        out=xs_d, out_offset=IOA(ap=DIDX[:, c:c + 1], axis=0), in_=HTOK[b], in_offset=None,
                        bounds_check=P.regs["xb"], oob_is_err=False),
                        r=[rHTOK[b], rIDX], w=[], dres=rXs[b], nowait_w=[rXs[b]])

            def xload(bi):
                x4 = bi % NXS
                P.dma("sp", XSL[x4], xs_d[bi * 128:(bi + 1) * 128, :], r=[rXs[0], rXs[1]], w=[rXSL[x4]])

            def stage_Tx(bi):
                s2 = bi % 2
                x4 = bi % NXS
                ps, pr = psum()
                pb_ = ps[:].bitcast(BF16)
                P.op("pe", [TR(pb_[:, k * 128:(k + 1) * 128], XSL[x4][:, k * 128:(k + 1) * 128], IDB[:]) for k in range(KC)],
                     r=[rXSL[x4], rC], w=[pr])
                P.op("act", ACT(XF[s2], pb_[:, 0:1024], AF.Copy), r=[pr], w=[rXF[s2]])

            def stage_GU(bi):
                s2 = bi % 2
                ws = (bi // 2) % 2
                wg, wu, _ = wviews(ws)
                psg, prg = psum()
                P.op("pe", [MM(psg[:], XF[s2][:, k * 128:(k + 1) * 128], wg[:, k * 512:(k + 1) * 512], st=(k == 0), sp=(k == KC - 1))
                            for k in range(KC)], r=[rXF[s2], rWGU[ws]], w=[prg])
                psu, pru = psum()
                P.op("pe", [MM(psu[:], XF[s2][:, k * 128:(k + 1) * 128], wu[:, k * 512:(k + 1) * 512], st=(k == 0), sp=(k == KC - 1))
                            for k in range(KC)], r=[rXF[s2], rWGU[ws]], w=[pru])
                P.op("act", ACT(SS[s2], psg[:], AF.Silu), r=[prg], w=[rSS[s2]])
                P.op("dve", TT(HIDS[s2], psu[:], SS[s2], ALU.mult), r=[pru, rSS[s2]], w=[rHIDS[s2]])

            def stage_Th(bi):
                s2 = bi % 2
                ps, pr = psum()
                pb_ = ps[:].bitcast(BF16)
                P.op("pe", [TR(pb_[:, f * 128:(f + 1) * 128], HIDS[s2][:, f * 128:(f + 1) * 128], IDB[:]) for f in range(4)],
                     r=[rHIDS[s2], rC], w=[pr])
                P.op("dve", CP(HIDT[s2], pb_[:, 0:512]), r=[pr], w=[rHIDT[s2]])

            def stage_D(bi):
                s2 = bi % 2
                ws = (bi // 2) % 2
                _, _, wdn = wviews(ws)
                for half in range(2):
                    ps, pr = psum()
                    P.op("pe", [MM(ps[:], HIDT[s2][:, f * 128:(f + 1) * 128],
                                   wdn[:, f * 1024 + half * 512:f * 1024 + (half + 1) * 512], st=(f == 0), sp=(f == 3))
                                for f in range(4)], r=[rHIDT[s2], rWD[ws]], w=[pr])
                    if half == 0:
                        P.op("act", ACT(YS[s2][:, 0:512], ps[:], AF.Copy), r=[pr], w=[rYS[s2]])
                    else:
                        P.op("dve", CP(YS[s2][:, 512:1024], ps[:]), r=[pr], w=[rYS[s2]])
                P.dma("sp", ys_d[bi * 128:(bi + 1) * 128, :], YS[s2], r=[rYS[s2]], w=[], dres=rYs[s2], nowait_w=[rYs[s2]])

            for bi in range(NXS - 1):
                xload(bi)
            stage_Tx(0)
            for i in range(NSUB + 1):
                if i >= 1:
                    stage_Th(i - 1)
                if i + NXS - 1 < NSUB:
                    xload(i + NXS - 1)
                if i + 1 < NSUB:
                    stage_Tx(i + 1)
                if i < NSUB:
                    stage_GU(i)
                    if i % 2 == 1 and i // 2 + 2 < NSTEP:
                        load_wgu(i // 2 + 2)
                if i >= 1:
                    stage_D(i - 1)
                    if (i - 1) % 2 == 1 and (i - 1) // 2 + 2 < NSTEP:
                        load_wd((i - 1) // 2 + 2)
            P.barrier()

            def issue_gather(t16):
                g = t16 % NYG
                for k in range(2):
                    c = k * 16 + t16
                    P.dmaf("pool", lambda eng, g=g, c=c, k=k: eng.indirect_dma_start(
                        out=YG[k][g], out_offset=None, in_=ys_d, in_offset=IOA(ap=DIDX[:, c:c + 1], axis=0),
                        bounds_check=P.regs["xb"], oob_is_err=False),
                        r=[rYs[0], rYs[1], rIDX], w=[rYG[k][g]])
            def k1(t16):
                b = t16 % 2
                g = t16 % NYG
                P.op("act", ACT(YC[b], YG[0][g], AF.Copy, scale=GATE[:, t16:t16 + 1]), r=[rYG[0][g], rIDX], w=[rYC[b]])
                P.op("dve", STT(YC[b], YG[1][g], GATE[:, 16 + t16:17 + t16], YC[b], ALU.mult, ALU.add),
                     r=[rYG[1][g], rIDX], w=[rYC[b]])

            def k2(t16):
                b = t16 % 2
                tok = slice(t16 * 128, (t16 + 1) * 128)
                for half in range(2):
                    ps, pr = psum()
                    P.op("pe", [TR(ps[:, j * 128:(j + 1) * 128], YC[b][:, (half * 4 + j) * 128:(half * 4 + j + 1) * 128], IDF)
                                for j in range(4)], r=[rYC[b], rC], w=[pr])
                    for j in range(4):
                        dc = half * 4 + j
                        P.op("dve", STT(XT[:, dc, tok], ps[:, j * 128:(j + 1) * 128], modv(layer, 5, dc), XT[:, dc, tok],
                                        ALU.mult, ALU.add), r=[pr, rMOD], w=[rXT[t16 // 4]])

            for t16 in range(NYG - 1):
                issue_gather(t16)
            k1(0)
            for t16 in range(16):
                if t16 + NYG - 1 < 16:
                    issue_gather(t16 + NYG - 1)
                if t16 + 1 < 16:
                    k1(t16 + 1)
                k2(t16)
            P.barrier()

        def mixer_conv(layer):
            UT = v3(carve(M0, KC * 2080, BF16), KC)
            o = M0 + 33280
            WIN = [v3(carve(o + s_ * 2048, KC * 128, BF16), KC) for s_ in range(4)]
            o += 8192
            SIG = [carve(o + s_ * 2048, 512, F32) for s_ in range(2)]
            o += 4096
            DG = [v3(carve(o + s_ * 8192, 31 * 128, BF16), 31) for s_ in range(2)]
            WOUT = v3(carve(o, KC * D, BF16), KC)
            o += 16384
            YSQ = v3(carve(o, KC * 512, BF16), KC)
            o += 8192
            T1 = [carve(o + s_ * 2048, 512, F32) for s_ in range(2)]
            o += 4096
            MEAN = carve(o, 512, F32)
            MSQ = carve(o + 2048, 512, F32)
            RSTD = carve(o + 4096, 512, F32)
            o += 6144
            rUT = P.res("UT")
            rWIN = [P.res("WIN%d" % s_) for s_ in range(4)]
            rSIG = [P.res("SIG0"), P.res("SIG1")]
            rDG = [P.res("DG0"), P.res("DG1")]
            Y = HT
            P.op("pool", MS(UT[:, :, 0:32], 0.0), w=[rUT])
            for c in range(KC):
                sa, sg = (2 * c) % 4, (2 * c + 1) % 4
                P.dma("pool", WIN[sa][:], cwin_d[:, c * 128:(c + 1) * 128].rearrange("(k p) n -> p k n", p=128), w=[rWIN[sa]])
                P.dma("pool", WIN[sg][:], cwin_d[:, D + c * 128:D + (c + 1) * 128].rearrange("(k p) n -> p k n", p=128),
                      w=[rWIN[sg]])
                for tt in range(4):
                    sl = slice(tt * 512, (tt + 1) * 512)
                    s2 = (c * 4 + tt) % 2
                    psa, pra = psum()
                    P.op("pe", [MM(psa[:], WIN[sa][:, k, :], HT[:, k, sl], st=(k == 0), sp=(k == KC - 1)) for k in range(KC)],
                         r=[rWIN[sa], rHT[tt]], w=[pra])
                    psg, prg = psum()
                    P.op("pe", [MM(psg[:], WIN[sg][:, k, :], HT[:, k, sl], st=(k == 0), sp=(k == KC - 1)) for k in range(KC)],
                         r=[rWIN[sg], rHT[tt]], w=[prg])
                    P.op("act", ACT(SIG[s2], psg[:], AF.Sigmoid, bias=V("cbin", 8 + c), scale=1.0), r=[prg, rVEC], w=[rSIG[s2]])
                    P.op("dve", STT(UT[:, c, 32 + tt * 512:32 + (tt + 1) * 512], psa[:], V("cbin", c), SIG[s2], ALU.add, ALU.mult),
                         r=[pra, rSIG[s2], rVEC], w=[rUT])
            P.barrier()
            for c in range(KC):
                ds = c % 2
                P.op("dve", [TS(DG[ds][:, j, :], IDB[:], V("wdw", c * 31 + j), None, ALU.mult) for j in range(31)],
                     r=[rVEC, rC], w=[rDG[ds]])
                for tt in range(4):
                    ps, pr = psum()
                    P.op("pe", [MM(ps[:], DG[ds][:, j, :], UT[:, c, tt * 512 + j + 2: tt * 512 + j + 2 + 512], st=(j == 0), sp=(j == 30))
                                for j in range(31)], r=[rDG[ds], rUT], w=[pr])
                    P.op("act", ACT(Y[:, c, tt * 512:(tt + 1) * 512], ps[:], AF.Identity, bias=V("cbdw", c), scale=1.0),
                         r=[pr, rVEC], w=[rHT[tt]])
            P.barrier()
            rW_ = P.res("WOUT")
            P.dma("pool", WOUT[:], cwout_d.rearrange("(k p) n -> p k n", p=128), w=[rW_])
            rYSQ = P.res("YSQ")
            rST = P.res("stats")
            rT1 = [P.res("T1a"), P.res("T1b")]
            for tt in range(4):
                sl = slice(tt * 512, (tt + 1) * 512)
                P.op("act", ACT(YSQ[:], Y[:, :, sl], AF.Square), r=[rHT[tt]], w=[rYSQ])
                p1, r1 = psum()
                P.op("pe", [MM(p1[:], ONESB[:], Y[:, c, sl], st=(c == 0), sp=(c == KC - 1)) for c in range(KC)],
                     r=[rHT[tt], rC], w=[r1])
                p2, r2 = psum()
                P.op("pe", [MM(p2[:], ONESB[:], YSQ[:, c, :], st=(c == 0), sp=(c == KC - 1)) for c in range(KC)],
                     r=[rYSQ, rC], w=[r2])
                P.op("dve", TS(MEAN, p1[:], 1.0 / D, None, ALU.mult), r=[r1], w=[rST])
                P.op("dve", TT(MSQ, MEAN, MEAN, ALU.mult), r=[rST], w=[rST])
                P.op("dve", STT(RSTD, p2[:], 1.0 / D, MSQ, ALU.mult, ALU.subtract), r=[r2, rST], w=[rST])
                P.op("act", ACT(RSTD, RSTD, AF.Sqrt, bias=SMALL[:, 0:1], scale=1.0), r=[rST, rC], w=[rST])
                P.op("dve", lambda e: e.reciprocal(out=RSTD, in_=RSTD), r=[rST], w=[rST])
                for c in range(KC):
                    s2 = c % 2
                    P.op("dve", TT(T1[s2], Y[:, c, sl], MEAN, ALU.subtract), r=[rHT[tt], rST], w=[rT1[s2]])
                    P.op("pool", TT(T1[s2], T1[s2], RSTD, ALU.mult), r=[rST], w=[rT1[s2]])
                    P.op("act", ACT(Y[:, c, sl], T1[s2], AF.Silu, bias=V("clnb", c), scale=V("clng", c)),
                         r=[rT1[s2], rVEC], w=[rHT[tt]])
            for tt in range(4):
                sl = slice(tt * 512, (tt + 1) * 512)
                for dc in range(KC):
                    ps, pr = psum()
                    P.op("pe", [MM(ps[:], WOUT[:, c, dc * 128:(dc + 1) * 128], Y[:, c, sl], st=(c == 0), sp=(c == KC - 1))
                                for c in range(KC)], r=[rW_, rHT[tt]], w=[pr])
                    P.op("dve", STT(XT[:, dc, sl], ps[:], modv(layer, 2, dc), XT[:, dc, sl], ALU.mult, ALU.add),
                         r=[pr, rMOD], w=[rXT[tt]])
            P.barrier()

        def mixer_pool(layer):
            o = M0
            SA = [carve(o + s_ * 8192, S, F32) for s_ in range(2)]
            SB = [carve(o + 16384 + s_ * 8192, S, F32) for s_ in range(2)]
            PL = v3(carve(o + 32768, KC * S, BF16), KC)
            o2 = o + 32768 + 32768
            PW = carve(o2, 4 * 2 * 256, BF16).rearrange("p (g k n) -> p g k n", g=4, k=2)
            INVC = carve(o2 + 4096, 64, F32)
            T16 = [carve(o2 + 4096 + 256 + s_ * 64, 16, F32) for s_ in range(2)]
            YT = [carve(o2 + 8192 + s_ * 2048, 512, F32) for s_ in range(2)]
            GL_ = carve(o2 + 8192 + 4096, 16, F32)
            rPW = P.res("PW")
            rIN = P.res("INVC")
            rS = [P.res("S0"), P.res("S1")]
            rPL = [P.res("PL%d" % c) for c in range(KC)]
            rYT = [P.res("YT0"), P.res("YT1")]
            rGL = P.res("GL")
            rT16 = [P.res("T16a"), P.res("T16b")]
            P.dma("pool", PW, pw_d.rearrange("g (k p) n -> p g k n", p=128), w=[rPW])
            P.dma("sp", INVC, invc_d, w=[rIN])
            for c in range(KC):
                P.op("dve", TT(GL_[:, c:c + 1], modv(layer, 2, c), V("psc", c), ALU.mult), r=[rMOD, rVEC], w=[rGL])
                P.op("dve", TT(GL_[:, 8 + c:9 + c], GL_[:, c:c + 1], V("pb", c), ALU.mult), r=[rVEC], w=[rGL])
            for c in range(KC):
                gi = c // 2
                wnd = 2 << gi
                en = "pool" if c % 4 == 3 else "dve"
                s_ = 1 if en == "pool" else 0
                bufs = [SA[s_], SB[s_]]
                cur = HT[:, c, :]
                sh = 1
                bi = 0
                while sh < wnd:
                    nxt = bufs[bi]
                    P.op(en, [TT(nxt[:, sh:], cur[:, sh:], cur[:, 0:S - sh], ALU.add), CP(nxt[:, 0:sh], cur[:, 0:sh])],
                         r=[rHT[0], rHT[1], rHT[2], rHT[3]], w=[rS[s_]])
                    cur = nxt
                    bi ^= 1
                    sh *= 2
                oth = bufs[bi]
                P.op(en, TS(oth, cur, 1.0 / wnd, None, ALU.mult), r=[], w=[rS[s_]])
                P.op(en, TT(PL[:, c, :], oth, HT[:, c, :], ALU.subtract), r=[rHT[0], rHT[1], rHT[2], rHT[3], rS[s_]], w=[rPL[c]])
                P.op(en, TT(T16[s_], cur[:, 0:16], INVC[:, gi * 16:(gi + 1) * 16], ALU.mult), r=[rIN, rS[s_]], w=[rT16[s_]])
                P.op(en, TT(PL[:, c, 0:16], T16[s_], HT[:, c, 0:16], ALU.subtract), r=[rT16[s_]], w=[rPL[c]])
            i_ = 0
            for gi in range(4):
                for dn in range(2):
                    dc = gi * 2 + dn
                    for tt in range(4):
                        sl = slice(tt * 512, (tt + 1) * 512)
                        ps, pr = psum()
                        P.op("pe", [MM(ps[:], PW[:, gi, k, dn * 128:(dn + 1) * 128], PL[:, gi * 2 + k, sl], st=(k == 0), sp=(k == 1))
                                    for k in range(2)], r=[rPW, rPL[gi * 2], rPL[gi * 2 + 1]], w=[pr])
                        s2 = i_ % 2
                        i_ += 1
                        P.op("act", ACT(YT[s2], ps[:], AF.Identity, bias=GL_[:, 8 + dc:9 + dc], scale=GL_[:, dc:dc + 1]),
                             r=[pr, rGL], w=[rYT[s2]])
                        P.op("dve", TT(XT[:, dc, sl], XT[:, dc, sl], YT[s2], ALU.add), r=[rYT[s2]], w=[rXT[tt]])
            P.barrier()

        def mixer_hgrn(layer):
            o = M0
            A_ = carve(o, S, F32); o += 8192
            B_ = carve(o, S, F32); o += 8192
            C_ = carve(o, S, F32); o += 8192
            D_ = carve(o, S, F32); o += 8192
            QTb = carve(o, S, BF16); o += 4096
            KTb = carve(o, S, BF16); o += 4096
            KHT = carve(o, S, BF16); o += 4096
            KHtok = v3(carve(o, 16 * 128, BF16), 16); o += 4096
            Vt = v3(carve(o, 16 * 128, BF16), 16); o += 4096
            Gb = carve(o, S, BF16); o += 4096
            WQ = v3(carve(o, KC * 512, BF16), KC); o += 8192
            SCM = carve(o, S, F32); o += 8192
            WO = [carve(o + s_ * 2048, D, BF16) for s_ in range(2)]; o += 4096
            SF = carve(o, 128, F32); o += 512
            SBF = [carve(o + s_ * 256, 128, BF16) for s_ in range(2)]; o += 512
            SC = [carve(o + s_ * 256, 128, BF16) for s_ in range(2)]; o += 512
            LB = carve(o, 64, F32); o += 256
            RS = carve(o, 512, F32); o += 2048
            OSQ = KHT
            OG = KTb
            rA, rB, rCc, rD = P.res("A"), P.res("B"), P.res("C"), P.res("D")
            rQT, rKT, rKHT, rKHtok, rV, rG = P.res("QT"), P.res("KT"), P.res("KHT"), P.res("KHtok"), P.res("V"), P.res("G")
            rWQ, rSCM, rLB = P.res("WQ"), P.res("SCM"), P.res("LB")
            rWO = [P.res("WO0"), P.res("WO1")]
            rSF = P.res("SF")
            rSBF = [P.res("SBF0"), P.res("SBF1")]
            rSC = [P.res("SC0"), P.res("SC1")]
            rRS = P.res("RS")
            P.dma("sp", SCM, scm_d, w=[rSCM])
            EX = carve(o, 64, F32); o += 256
            for i in range(DEPTH):
                P.op("act", ACT(EX[:, i * 8:(i + 1) * 8], V("lbl%d" % i, 0, 8), AF.Exp), r=[rVEC], w=[rLB])
            P.op("dve", TT(LB[:, 24:32], EX[:, 0:8], EX[:, 8:16], ALU.add), r=[rLB], w=[rLB])
            P.op("dve", TT(LB[:, 24:32], LB[:, 24:32], EX[:, 16:24], ALU.add), r=[rLB], w=[rLB])
            P.op("dve", TT(LB[:, 24:32], LB[:, 24:32], EX[:, 24:32], ALU.add), r=[rLB], w=[rLB])
            P.op("dve", lambda e: e.reciprocal(out=LB[:, 24:32], in_=LB[:, 24:32]), r=[rLB], w=[rLB])
            P.op("dve", MS(LB[:, 0:8], 0.0), w=[rLB])
            for i in range(1, layer + 1):
                P.op("dve", TT(LB[:, 0:8], LB[:, 0:8], EX[:, i * 8:(i + 1) * 8], ALU.add), r=[rLB], w=[rLB])
            P.op("dve", TT(LB[:, 0:8], LB[:, 0:8], LB[:, 24:32], ALU.mult), r=[rLB], w=[rLB])
            P.op("dve", TS(LB[:, 8:16], LB[:, 0:8], -1.0, 1.0, ALU.mult, ALU.add), r=[rLB], w=[rLB])
            P.op("dve", TS(LB[:, 16:24], LB[:, 8:16], -1.0, None, ALU.mult), r=[rLB], w=[rLB])
            for h in range(KC):
                for part in range(4):
                    P.dma("pool", WQ[:, :, part * 128:(part + 1) * 128],
                          hwin_d[:, part * D + h * 128: part * D + (h + 1) * 128].rearrange("(k p) n -> p k n", p=128),
                          w=[rWQ])
                P.dma("pool", WO[h % 2], hwout_d[h * 128:(h + 1) * 128, :], w=[rWO[h % 2]])
                for tt in range(4):
                    sl = slice(tt * 512, (tt + 1) * 512)
                    for part, dst in ((0, "q"), (1, "f"), (3, "g")):
                        ps, pr = psum()
                        P.op("pe", [MM(ps[:], WQ[:, k, part * 128:(part + 1) * 128], HT[:, k, sl], st=(k == 0), sp=(k == KC - 1))
                                    for k in range(KC)], r=[rWQ, rHT[tt]], w=[pr])
                        if dst == "q":
                            P.op("act", ACT(D_[:, sl], ps[:], AF.Silu), r=[pr], w=[rD])
                        elif dst == "f":
                            P.op("act", ACT(A_[:, sl], ps[:], AF.Sigmoid), r=[pr], w=[rA])
                        else:
                            P.op("act", ACT(Gb[:, sl], ps[:], AF.Silu), r=[pr], w=[rG])
                for t16 in range(16):
                    ps, pr = psum()
                    P.op("pe", [MM(ps[:, 0:128], HT[:, k, t16 * 128:(t16 + 1) * 128], WQ[:, k, 256:384], st=(k == 0), sp=(k == KC - 1))
                                for k in range(KC)], r=[rWQ, rHT[t16 // 4]], w=[pr])
                    P.op("act", ACT(Vt[:, t16, :], ps[:, 0:128], AF.Copy), r=[pr], w=[rV])
                lb, oml, noml = LB[:, h:h + 1], LB[:, 8 + h:9 + h], LB[:, 16 + h:17 + h]
                P.op("dve", TS(B_, A_, oml, lb, ALU.mult, ALU.add), r=[rA, rLB], w=[rB])
                P.op("act", ACT(B_, B_, AF.Ln), r=[], w=[rB])
                P.op("act", ACT(A_, A_, AF.Identity, bias=oml, scale=noml), r=[rLB, rB], w=[rA])
                P.op("dve", lambda e: e.tensor_tensor_scan(out=C_, data0=SCM, data1=B_, initial=0.0, op0=ALU.mult, op1=ALU.add),
                     r=[rB, rSCM], w=[rCc])
                P.op("act", ACT(B_, C_, AF.Exp), r=[rCc], w=[rB])
                P.op("dve", TS(C_, C_, -1.0, 80.0, ALU.mult, ALU.min), r=[rB], w=[rCc])
                P.op("act", ACT(C_, C_, AF.Exp), r=[], w=[rCc])
                P.op("dve", TT(QTb, D_, B_, ALU.mult), r=[rD, rB], w=[rQT])
                P.op("dve", TT(A_, A_, C_, ALU.mult), r=[rCc], w=[rA])
                P.op("act", ACT(KTb, A_, AF.Copy), r=[rA], w=[rKT])
                A3 = A_.rearrange("p (n c) -> p n c", c=64)
                B3 = B_.rearrange("p (n c) -> p n c", c=64)
                K3 = KHT.rearrange("p (n c) -> p n c", c=64)
                P.op("dve", TT(K3, A3, B3[:, :, 63:64].to_broadcast([128, 32, 64]), ALU.mult), r=[rA, rB], w=[rKHT])
                for t16 in range(16):
                    ps, pr = psum()
                    pb_ = ps[:].bitcast(BF16)
                    P.op("pe", TR(pb_[:, 0:128], KHT[:, t16 * 128:(t16 + 1) * 128], IDB[:]), r=[rKHT, rC], w=[pr])
                    P.op("act", ACT(KHtok[:, t16, :], pb_[:, 0:128], AF.Copy), r=[pr], w=[rKHtok])
                P.op("dve", MS(SF, 0.0), w=[rSF])
                P.op("dve", MS(SBF[0], 0.0), w=[rSBF[0]])
                sv = 0
                for t16 in range(16):
                    tsl = slice(t16 * 128, (t16 + 1) * 128)
                    sc_i = t16 % 2
                    ps, pr = psum()
                    P.op("pe", MM(ps[:, 0:128], KTb[:, tsl], QTb[:, tsl]), r=[rKT, rQT], w=[pr])
                    P.op("dve", TT(SC[sc_i], ps[:, 0:128], MASK2, ALU.mult), r=[pr, rC], w=[rSC[sc_i]])
                    pa, pra = psum()
                    P.op("pe", [MM(pa[:, 0:64], SBF[sv], QTb[:, t16 * 128:t16 * 128 + 64], st=True, sp=False),
                                MM(pa[:, 0:64], Vt[0:64, t16, :], SC[sc_i][0:64, 0:64], st=False, sp=True)],
                         r=[rSBF[sv], rQT, rV, rSC[sc_i]], w=[pra])
                    pu, pru = psum()
                    P.op("pe", MM(pu[:, 0:128], KHtok[0:64, t16, :], Vt[0:64, t16, :]), r=[rKHtok, rV], w=[pru])
                    P.op("dve", STT(SF, SF, B_[:, t16 * 128 + 63:t16 * 128 + 64], pu[:, 0:128], ALU.mult, ALU.add),
                         r=[pru, rB], w=[rSF])
                    P.op("act", ACT(SBF[1 - sv], SF, AF.Copy), r=[rSF], w=[rSBF[1 - sv]])
                    sv = 1 - sv
                    pb2, prb = psum()
                    P.op("pe", [MM(pb2[:, 0:64], SBF[sv], QTb[:, t16 * 128 + 64:t16 * 128 + 128], st=True, sp=False),
                                MM(pb2[:, 0:64], Vt[:, t16, :], SC[sc_i][:, 64:128], st=False, sp=True)],
                         r=[rSBF[sv], rQT, rV, rSC[sc_i]], w=[prb])
                    pu2, pru2 = psum()
                    P.op("pe", MM(pu2[:, 0:128], KHtok[64:128, t16, :], Vt[64:128, t16, :]), r=[rKHtok, rV], w=[pru2])
                    P.op("dve", STT(SF, SF, B_[:, t16 * 128 + 127:t16 * 128 + 128], pu2[:, 0:128], ALU.mult, ALU.add),
                         r=[pru2, rB], w=[rSF])
                    P.op("act", ACT(SBF[1 - sv], SF, AF.Copy), r=[rSF], w=[rSBF[1 - sv]])
                    sv = 1 - sv
                    P.op("act", ACT(D_[:, t16 * 128:t16 * 128 + 64], pa[:, 0:64], AF.Copy), r=[pra, rQT], w=[rD])
                    P.op("act", ACT(D_[:, t16 * 128 + 64:t16 * 128 + 128], pb2[:, 0:64], AF.Copy), r=[prb], w=[rD])
                for tt in range(4):
                    sl = slice(tt * 512, (tt + 1) * 512)
                    P.op("act", ACT(OSQ[:, sl], D_[:, sl], AF.Square), r=[rD, rKHtok], w=[rKHT])
                    ps, pr = psum()
                    P.op("pe", MM(ps[:], ONESB[:], OSQ[:, sl]), r=[rKHT, rC], w=[pr])
                    P.op("act", ACT(RS, ps[:], AF.Sqrt, bias=SMALL[:, 0:1], scale=1.0 / 128), r=[pr, rC], w=[rRS])
                    P.op("dve", lambda e: e.reciprocal(out=RS, in_=RS), r=[rRS], w=[rRS])
                    P.op("dve", STT(D_[:, sl], D_[:, sl], V("hng", h), RS, ALU.mult, ALU.mult), r=[rRS, rVEC], w=[rD])
                    P.op("pool", TT(OG[:, sl], D_[:, sl], Gb[:, sl], ALU.mult), r=[rD, rG, rSC[0], rSC[1]], w=[rKT])
                    for dc in range(KC):
                        ps2, pr2 = psum()
                        P.op("pe", MM(ps2[:], WO[h % 2][:, dc * 128:(dc + 1) * 128], OG[:, sl]), r=[rWO[h % 2], rKT], w=[pr2])
                        P.op("dve", STT(XT[:, dc, sl], ps2[:], modv(layer, 2, dc), XT[:, dc, sl], ALU.mult, ALU.add),
                             r=[pr2, rMOD], w=[rXT[tt]])
            P.barrier()

        def mixer_sb(layer):
            scale = 64 ** -0.5
            o = L0
            QT = [carve(o + s_ * 4096, S, BF16) for s_ in range(2)]; o += 8192
            KT = [carve(o + s_ * 4096, S, BF16) for s_ in range(2)]; o += 8192
            VP = [v3(carve(o + hp * 4096, 16 * 128, BF16), 16) for hp in range(2)]; o += 8192
            OT = [carve(o + s_ * 4096, S, BF16) for s_ in range(2)]; o += 8192
            WQ = [v3(carve(o + s_ * 6144, KC * 384, BF16), KC) for s_ in range(2)]; o += 12288
            WO = [carve(o + s_ * 2048, D, BF16) for s_ in range(2)]; o += 4096
            SP = [carve(o + s_ * 8192, S, F32) for s_ in range(2)]; o += 16384
            A_ = [carve(o + s_ * 8192, S, F32) for s_ in range(2)]; o += 16384
            C_ = carve(o, S, F32); o += 8192
            W_ = [carve(o + s_ * 4096, S, BF16) for s_ in range(2)]; o += 8192
            WT = [v3(carve(o + s_ * 4096, 16 * 128, BF16), 16) for s_ in range(2)]; o += 8192
            assert o <= ARENA_BYTES, o
            NT = [SMALL[:, 8:9], SMALL[:, 9:10]]
            two = lambda n: [P.res(n + "0"), P.res(n + "1")]
            rQT, rKT, rOT, rWQ, rWO = two("QT"), two("KT"), two("OT"), two("WQ"), two("WO")
            rVP = P.res("VP")
            rSP = two("SP")
            rCc = P.res("C")
            rA, rW, rWT, rNT = two("A"), two("W"), two("WT"), two("NT")
            rNEG = P.res("NEGM")
            P.op("dve", TS(NEGM[:], CST[:, 128:256], -1.0, 30000.0, ALU.add, ALU.mult), r=[rC], w=[rNEG])
            po, pro = banks[7], bres[7]
            P.op("pool", MS(VP[0][:, :, 64:128], 0.0), w=[rVP])
            P.op("pool", MS(VP[1][:, :, 0:64], 0.0), w=[rVP])
            zb = [0]
            tb = [0]

            def zbank():
                i = zb[0]
                zb[0] = (i + 1) % 4
                return banks[i], bres[i]

            def tbank():
                i = 4 + tb[0]
                tb[0] = (tb[0] + 1) % 3
                return banks[i], bres[i]

            def load_weights(c):
                s_ = c % 2
                for part in range(3):
                    P.dma("pool", WQ[s_][:, :, part * 128:(part + 1) * 128],
                          sqkv_d[:, part * D + c * 128: part * D + (c + 1) * 128].rearrange("(k p) n -> p k n", p=128),
                          w=[rWQ[s_]])
                P.dma("pool", WO[s_], swout_d[c * 128:(c + 1) * 128, :], w=[rWO[s_]])

            def qk_piece(c, m):
                s_ = c % 2
                tt, part = m // 2, m % 2
                sl = slice(tt * 512, (tt + 1) * 512)
                dst, rd = ((QT[s_], rQT[s_]), (KT[s_], rKT[s_]))[part]
                ps, pr = tbank()
                P.op("pe", [MM(ps[:], WQ[s_][:, k, part * 128:(part + 1) * 128], HT[:, k, sl], st=(k == 0), sp=(k == KC - 1))
                            for k in range(KC)], r=[rWQ[s_], rHT[tt]], w=[pr])
                P.op("dve", CP(dst[:, sl], ps[:]), r=[pr], w=[rd])

            def qk_proj(c):
                for m in range(8):
                    qk_piece(c, m)

            def v_proj(c):
                s_ = c % 2
                for t16 in range(16):
                    ps, pr = tbank()
                    P.op("pe", [MM(ps[:, 0:128], HT[:, k, t16 * 128:(t16 + 1) * 128], WQ[s_][:, k, 256:384], st=(k == 0), sp=(k == KC - 1))
                                for k in range(KC)], r=[rWQ[s_], rHT[t16 // 4]], w=[pr])
                    P.op("dve", [CP(VP[0][:, t16, 0:64], ps[:, 0:64]), CP(VP[1][:, t16, 64:128], ps[:, 64:128])], r=[pr], w=[rVP])

            def stageA(u, c, qb, hp):
                s_ = c % 2
                b = u % 2
                hs = slice(hp * 64, (hp + 1) * 64)
                tq = slice(qb * 128, (qb + 1) * 128)
                nk = (qb + 1) * 128
                nch = (nk + 511) // 512
                zs = []
                for ch in range(nch):
                    w = min(512, nk - ch * 512)
                    cs = slice(ch * 512, ch * 512 + w)
                    pz, prz = zbank()
                    fns = [MM(pz[:, 0:w], QT[s_][hs, tq], KT[s_][hs, cs], st=True, sp=(ch != nch - 1))]
                    if ch == nch - 1:
                        fns.append(MM(pz[:, w - 128:w], IDB[:], NEGM[:], st=False, sp=True))
                    P.op("pe", fns, r=[rQT[s_], rKT[s_], rNEG, rC], w=[prz])
                    P.op("act", ACT(SP[b][:, cs], pz[:, 0:w], AF.Exp, scale=scale), r=[prz], w=[rSP[b]])
                    zs.append((pz, prz, w, cs))
                P.op("act", ACT(SP[b][:, 0:nk], SP[b][:, 0:nk], AF.Ln, bias=SMALL[:, 1:2], scale=1.0), r=[rC], w=[rSP[b]])
                for pz, prz, w, cs in zs:
                    P.op("dve", STT(A_[b][:, cs], pz[:, 0:w], scale, SP[b][:, cs], ALU.mult, ALU.subtract), r=[prz, rSP[b]], w=[rA[b]])

            def stageB(u, c, qb, hp):
                b = u % 2
                nk = (qb + 1) * 128
                e_scan, e_add = ("dve", "pool")
                P.op(e_scan, lambda e, b=b, nk=nk: e.tensor_tensor_scan(
                    out=C_[:, 0:nk], data0=SMALL[:, 1:2].to_broadcast([128, nk]), data1=SP[b][:, 0:nk], initial=0.0,
                    op0=ALU.mult, op1=ALU.add), r=[rSP[b], rC], w=[rCc])
                P.op(e_scan, TS(NT[b], C_[:, nk - 1:nk], -1.0, None, ALU.mult), r=[rCc], w=[rNT[b]])
                P.op(e_add, TT(A_[b][:, 0:nk], A_[b][:, 0:nk], C_[:, 0:nk], ALU.add), r=[rCc], w=[rA[b]])

            def stageC1(u, c, qb, hp):
                b = u % 2
                nk = (qb + 1) * 128
                P.op("act", ACT(W_[b][:, 0:nk], A_[b][:, 0:nk], AF.Exp, bias=NT[b], scale=1.0), r=[rA[b], rNT[b]], w=[rW[b]])

            def stageC(u, c, qb, hp):
                s_ = c % 2
                b = u % 2
                tq = slice(qb * 128, (qb + 1) * 128)
                nk = (qb + 1) * 128
                nb_ = qb + 1
                for g0 in range(0, nb_, 8):
                    n = min(8, nb_ - g0)
                    pt, prt = tbank()
                    ptb = pt[:].bitcast(BF16)
                    P.op("pe", [TR(ptb[:, j * 128:(j + 1) * 128], W_[b][:, (g0 + j) * 128:(g0 + j + 1) * 128], IDB[:]) for j in range(n)],
                         r=[rW[b], rC], w=[prt])
                    dst = WT[b][:, g0:g0 + n, :].rearrange("p a b -> p (a b)")
                    if g0 == 0:
                        P.op("act", ACT(dst, ptb[:, 0:n * 128], AF.Copy), r=[prt], w=[rWT[b]])
                    else:
                        P.op("dve", CP(dst, ptb[:, 0:n * 128]), r=[prt], w=[rWT[b]])
                pcol = slice((qb % 4) * 128, (qb % 4 + 1) * 128)
                P.op("pe", [MM(po[:, pcol], VP[hp][:, jb, :], WT[b][:, jb, :], st=(hp == 0 and jb == 0), sp=(hp == 1 and jb == qb))
                            for jb in range(nb_)], r=[rVP, rWT[b]], w=[pro])
                if hp == 1:
                    P.op("dve", CP(OT[s_][:, tq], po[:, pcol]), r=[pro], w=[rOT[s_]])
                    if qb % 4 == 3:
                        I = qb // 4
                        qsl = slice(I * 512, (I + 1) * 512)
                        for dc in range(KC):
                            ps2, pr2 = tbank()
                            P.op("pe", MM(ps2[:], WO[s_][:, dc * 128:(dc + 1) * 128], OT[s_][:, qsl]), r=[rWO[s_], rOT[s_]], w=[pr2])
                            P.op("dve", STT(XT[:, dc, qsl], ps2[:], modv(layer, 2, dc), XT[:, dc, qsl], ALU.mult, ALU.add),
                                 r=[pr2, rMOD], w=[rXT[I]])

            units = [(c, qb, hp) for c in range(KC) for qb in range(16) for hp in range(2)]
            load_weights(0)
            load_weights(1)
            nu = len(units)
            for i in range(nu + 2):
                if 2 <= i:
                    stageC1(i - 2, *units[i - 2])
                if 1 <= i <= nu:
                    stageB(i - 1, *units[i - 1])
                if i < nu:
                    c, qb, hp = units[i]
                    if qb == 0 and hp == 0 and c == 0:
                        qk_proj(c)
                    stageA(i, c, qb, hp)
                    j = qb * 2 + hp
                    if c + 1 < KC and j % 4 == 3:
                        qk_piece(c + 1, j // 4)
                if 2 <= i:
                    c, qb, hp = units[i - 2]
                    if qb == 0 and hp == 0:
                        v_proj(c)
                    stageC(i - 2, c, qb, hp)
                    if qb == 15 and hp == 1 and c + 2 < KC:
                        load_weights(c + 2)
            P.barrier()

        b7 = [0]

        def psum7():
            i = b7[0]
            b7[0] = (i + 1) % 7
            return banks[i], bres[i]

        mixers = [mixer_conv, mixer_hgrn, mixer_pool, mixer_sb]
        for i in range(DEPTH):
            if ("mix%d" % i) in phases:
                norm_phase(lambda c, i=i: DER[:, i * 16 + c:i * 16 + c + 1], lambda c, i=i: modv(i, 0, c), "mix")
                P.barrier()
                mixers[i](i)
            if ("hdump%d" % i) in phases:
                norm_phase(lambda c, i=i: DER[:, i * 16 + c:i * 16 + c + 1], lambda c, i=i: modv(i, 0, c), "mix")
                P.barrier()
                for tt in range(4):
                    P.op("act", ACT(XT[:, :, tt * 512:(tt + 1) * 512], HT[:, :, tt * 512:(tt + 1) * 512], AF.Copy),
                         r=[rHT[tt]], w=[rXT[tt]])
                P.barrier()
            if ("fdump%d" % i) in phases:
                LOG, rLOG, _ = norm_phase(lambda c, i=i: DER[:, i * 16 + 8 + c:i * 16 + 9 + c], lambda c, i=i: modv(i, 3, c),
                                          "ffn", layer=i)
                P.barrier()
                for tt in range(4):
                    P.op("act", ACT(XT[:, :, tt * 512:(tt + 1) * 512], HT[:, :, tt * 512:(tt + 1) * 512], AF.Copy),
                         r=[rHT[tt]], w=[rXT[tt]])
                P.op("act", ACT(XT[:, 0, 0:576], LOG[:].rearrange("p a b -> p (a b)"), AF.Copy), r=[rLOG], w=[rXT[0]])
                P.barrier()
            if ("ffn%d" % i) in phases:
                LOG, rLOG, _ = norm_phase(lambda c, i=i: DER[:, i * 16 + 8 + c:i * 16 + 9 + c], lambda c, i=i: modv(i, 3, c),
                                          "ffn", layer=i)
                moe_phase(i, LOG, rLOG)
        if dbg:
            rOUT = P.res("OUT")
            for i in range(4):
                P.dma("sp", out_d[:, :, i * 512:(i + 1) * 512], XT[:, :, i * 512:(i + 1) * 512], r=[rXT[i]], w=[], dres=rOUT)
        else:
            norm_phase(lambda c: V("fin", c), None, "final")
        P.barrier()
        block = es.enter_context(nc.Block())
        P.replay(block)
    return nc


ALL_PHASES = ["mods"] + [p for i in range(DEPTH) for p in ("mix%d" % i, "ffn%d" % i)]


def _consts():
    ident = np.eye(128, dtype=np.float32)
    j = np.arange(128)[:, None]
    s = np.arange(128)[None, :]
    tri = (j > s).astype(np.float32)
    mask2 = ((j <= s) & ((j // 64) == (s // 64))).astype(np.float32)
    cA = np.concatenate([ident, tri, mask2], axis=1)
    t = np.arange(512)[None, :]
    sbm = np.concatenate([((r * 128 + j) < t).astype(np.float32) for r in range(4)], axis=1)
    scm = np.broadcast_to((np.arange(S) % 64 != 0).astype(np.float32)[None, :], (128, S)).copy()
    invc = np.zeros((128, 64), np.float32)
    for gi, w in enumerate((2, 4, 8, 16)):
        invc[:, gi * 16:(gi + 1) * 16] = 1.0 / np.minimum(np.arange(16) + 1, w)
    cB = np.zeros((128, NCB), np.float32)
    cB[:, 0:16] = 256.0 * np.arange(16)[None, :]
    cB[:, 16:80] = np.arange(64)[None, :]
    cB[:, 80] = np.arange(128)
    return cA, sbm, scm, invc, cB


_CACHE = {}


def kernel(**inp):
    return run(inp, ALL_PHASES, False)


def run(inp, phases, dbg, cores=8):
    f = lambda a: np.ascontiguousarray(np.asarray(a, np.float32))
    key = (tuple(phases), dbg)
    if key not in _CACHE:
        _CACHE[key] = build_program(phases, dbg)
    nc = _CACHE[key]
    cA, sbm, scm, invc, cB = _consts()
    x = f(inp["x"])
    shared = {
        "cA": cA, "sbmask": sbm, "scanmask": scm, "invc": invc, "cB": cB,
        "ada_w": f(inp["ada_w"]),
        "conv_w_in": f(inp["conv_w_in"][0]), "conv_w_out": f(inp["conv_w_out"][0]),
        "hgrn_w_in": f(inp["hgrn_w_in"][0]), "hgrn_w_out": f(inp["hgrn_w_out"][0]),
        "pool_w": f(inp["pool_w"][0]),
        "sb_w_qkv": f(inp["sb_w_qkv"][0]), "sb_w_out": f(inp["sb_w_out"][0]),
    }
    shared["wgL"] = np.ascontiguousarray(
        f(inp["moe_w_gate"]).reshape(DEPTH, NEXP, KC, 128, FH).transpose(0, 1, 3, 2, 4)).reshape(DEPTH * NEXP * 128, KC * FH)
    shared["wuL"] = np.ascontiguousarray(
        f(inp["moe_w_up"]).reshape(DEPTH, NEXP, KC, 128, FH).transpose(0, 1, 3, 2, 4)).reshape(DEPTH * NEXP * 128, KC * FH)
    shared["wdL"] = np.ascontiguousarray(
        f(inp["moe_w_down"]).reshape(DEPTH, NEXP, 4, 128, D).transpose(0, 1, 3, 2, 4)).reshape(DEPTH * NEXP * 128, 4 * D)
    wr = np.concatenate([f(inp["moe_w_rg"]), f(inp["moe_w_re"])], axis=2)
    shared["wr"] = np.ascontiguousarray(wr.reshape(DEPTH, KC, 128, 36).transpose(2, 0, 1, 3))
    br = np.concatenate([f(inp["moe_b_rg"]), f(inp["moe_b_re"])], axis=1).reshape(1, DEPTH * 36)
    shared["br"] = np.ascontiguousarray(np.broadcast_to(br, (128, DEPTH * 36)))
    in_maps = []
    for b in range(cores):
        vec = np.zeros((128, NVEC), np.float32)

        def put(name, arr):
            a = _fm(arr)
            vec[:, VOFF[name]:VOFF[name] + a.shape[1]] = a
        for i in range(DEPTH):
            put("gmix%d" % i, inp["norm_mix_g"][i])
            put("gffn%d" % i, inp["norm_ffn_g"][i])
            put("adab%d" % i, inp["ada_b"][i])
            put("lbl%d" % i, inp["hgrn_lb_logits"][i])
        put("fin", inp["final_g"])
        put("cbin", inp["conv_b_in"][0])
        put("cbdw", inp["conv_b_dw"][0])
        put("clng", inp["conv_ln_g"][0])
        put("clnb", inp["conv_ln_b"][0])
        put("hng", inp["hgrn_norm_g"][0])
        put("pb", inp["pool_b"][0])
        put("psc", inp["pool_scale"][0])
        wdw = f(inp["conv_w_dw"][0])
        vec[:, VOFF["wdw"]:VOFF["wdw"] + 248] = wdw.reshape(31, KC, 128).transpose(2, 1, 0).reshape(128, 248)
        put("c", inp["c"][b])
        m = dict(shared)
        m["vecs"] = vec
        m["xT"] = np.ascontiguousarray(x[b].T.reshape(KC, 128, S).transpose(1, 0, 2))
        in_maps.append(m)
    res = run_bass_kernel_spmd(nc, in_maps, core_ids=list(range(cores)))
    outs = []
    for b in range(cores):
        oT = res.results[b]["outT"]
        outs.append(np.ascontiguousarray(oT.transpose(1, 0, 2).reshape(D, S).T))
    return np.stack(outs, axis=0).astype(np.float32)
```

```python
import numpy as np
from contextlib import ExitStack
import concourse.bass as bass
import concourse.mybir as mybir
from concourse.bass_utils import run_bass_kernel_spmd

F32 = mybir.dt.float32
BF16 = mybir.dt.bfloat16
AF = mybir.ActivationFunctionType
ALU = mybir.AluOpType
AX = mybir.AxisListType

D = 1024
S = 2048
KC = 8
DEPTH = 4
EPS = 1e-6
NEXP = 32
FH = 512
L0 = 98304
M0 = L0 + 24576
ARENA_BYTES = 204800
BIG = 1.0e30
NSTEP = 48
NSUB = 2 * NSTEP
NSLOT = NSTEP * 256
NCB = 84
I32 = mybir.dt.int32

ENGS = ("pe", "act", "dve", "pool", "sp")


class Slot:
    __slots__ = ("sem", "cnt", "eng")

    def __init__(self, sem):
        self.sem = sem
        self.cnt = 0
        self.eng = None


class Res:
    __slots__ = ("name", "w", "r", "slot")

    def __init__(self, name):
        self.name = name
        self.w = None
        self.r = {}
        self.slot = None


class Prog:
    def __init__(self, nc, es):
        self.nc = nc
        self.es = es
        self.q = {e: [] for e in ENGS}
        self.cnt = {e: 0 for e in ENGS}
        self.seen = {e: {} for e in ENGS}
        self.esem = {e: es.enter_context(nc.semaphore("sem_" + e)) for e in ("pe", "act", "dve", "pool")}
        self.slots = []
        self.free = {}
        self.live = []
        self.regs = {}

    def res(self, name):
        return Res(name)

    def _sem_of(self, key):
        return self.esem[key] if isinstance(key, str) else key.sem

    def _wait(self, e, toks, strict=False):
        need = {}
        for t in toks:
            if t is None:
                continue
            k, v = t
            if (not strict) and k == e and e == "pe":
                continue
            if need.get(k, 0) < v:
                need[k] = v
        for k, v in need.items():
            if self.seen[e].get(k, 0) >= v:
                continue
            self.seen[e][k] = v
            sem = self._sem_of(k)
            self.q[e].append(lambda eng, sem=sem, v=v: eng.wait_ge(sem, v))

    def _collect(self, r, w):
        toks = []
        for x in r:
            toks.append(x.w)
        for x in w:
            toks.append(x.w)
            toks.extend(x.r.items())
        return toks

    def op(self, e, fns, r=(), w=()):
        if callable(fns):
            fns = [fns]
        self._wait(e, self._collect(r, w))
        self.cnt[e] += 1
        n = self.cnt[e]
        sem = self.esem[e]
        last = len(fns) - 1
        for i, fn in enumerate(fns):
            if i == last:
                self.q[e].append(lambda eng, fn=fn, sem=sem: fn(eng).then_inc(sem, 1))
            else:
                self.q[e].append(fn)
        tok = (e, n)
        for x in r:
            if x.r.get(e, 0) < n:
                x.r[e] = n
        for x in w:
            x.w = tok
            x.r = {}
        return tok

    def dma(self, e, out, in_, r=(), w=(), dres=None, nowait_w=()):
        return self.dmaf(e, lambda eng, out=out, in_=in_: eng.dma_start(out=out, in_=in_), r, w, dres, nowait_w)

    def dmaf(self, e, fn, r=(), w=(), dres=None, nowait_w=()):
        if dres is None:
            dres = w[0]
        if dres.slot is None:
            fl = self.free.setdefault(e, [])
            if fl:
                dres.slot = fl.pop()
            else:
                dres.slot = Slot(self.es.enter_context(self.nc.semaphore("dsem_%s_%d" % (e, len(self.slots)))))
                dres.slot.eng = e
                self.slots.append(dres.slot)
            self.live.append(dres)
        slot = dres.slot
        assert slot.eng == e, (dres.name, slot.eng, e)
        self._wait(e, self._collect(r, w), strict=True)
        slot.cnt += 16
        v = slot.cnt
        sem = slot.sem
        self.q[e].append(lambda eng, fn=fn, sem=sem: fn(eng).then_inc(sem, 16))
        tok = (slot, v)
        for x in r:
            x.r[slot] = v
        for x in w:
            x.w = tok
            x.r = {}
        for x in nowait_w:
            x.w = tok
        return tok

    def barrier(self):
        toks = [(e, self.cnt[e]) for e in ("pe", "act", "dve", "pool") if self.cnt[e] > 0]
        toks += [(d, d.cnt) for d in self.slots if d.cnt > 0]
        for e in ENGS:
            self._wait(e, toks)
        for d in self.live:
            self.free.setdefault(d.slot.eng, []).append(d.slot)
            d.slot = None
        self.live = []

    def replay(self, block):
        def mk(name):
            def f(eng):
                for fn in self.q[name]:
                    fn(eng)
            return f
        block.tensor(mk("pe"))
        block.scalar(mk("act"))
        block.vector(mk("dve"))
        block.gpsimd(mk("pool"))
        block.sync(mk("sp"))


def MM(o, l, r, st=True, sp=True):
    return lambda e: e.matmul(o, lhsT=l, rhs=r, start=st, stop=sp)


def TR(o, i, ident):
    return lambda e: e.transpose(o, i, ident)


def ACT(o, i, f, bias=None, scale=None):
    kw = {}
    if bias is not None:
        kw["bias"] = bias
    if scale is not None:
        kw["scale"] = scale
    return lambda e: e.activation(out=o, in_=i, func=f, **kw)


def TS(o, i, s1, s2, op0, op1=None):
    if op1 is None:
        return lambda e: e.tensor_scalar(out=o, in0=i, scalar1=s1, scalar2=None, op0=op0)
    return lambda e: e.tensor_scalar(out=o, in0=i, scalar1=s1, scalar2=s2, op0=op0, op1=op1)


def TT(o, a, b, op):
    return lambda e: e.tensor_tensor(out=o, in0=a, in1=b, op=op)


def STT(o, a, s, b, op0, op1):
    return lambda e: e.scalar_tensor_tensor(out=o, in0=a, scalar=s, in1=b, op0=op0, op1=op1)


def CP(o, i):
    return lambda e: e.tensor_copy(out=o, in_=i)


def MS(o, v):
    return lambda e: e.memset(o, v)


def _vec_layout():
    off = {}
    n = 0

    def add(name, cols):
        nonlocal n
        off[name] = n
        n += cols
    for i in range(DEPTH):
        add("gmix%d" % i, 8)
        add("gffn%d" % i, 8)
        add("adab%d" % i, 48)
        add("lbl%d" % i, 8)
    add("fin", 8)
    add("cbin", 16)
    add("cbdw", 8)
    add("clng", 8)
    add("clnb", 8)
    add("hng", 8)
    add("pb", 8)
    add("psc", 8)
    add("wdw", 248)
    add("c", 8)
    return off, n


VOFF, NVEC = _vec_layout()


def _fm(v):
    v = np.asarray(v, np.float32).reshape(-1, 128)
    return np.ascontiguousarray(v.T)


def build_program(phases, dbg=False):
    nc = bass.Bass("TRN2", target_bir_lowering=False)
    dt_ = nc.dram_tensor
    xT_d = dt_("xT", [128, KC, S], F32, kind="ExternalInput").ap()
    vec_d = dt_("vecs", [128, NVEC], F32, kind="ExternalInput").ap()
    br_d = dt_("br", [128, DEPTH * 36], F32, kind="ExternalInput").ap()
    wr_d = dt_("wr", [128, DEPTH, KC, 36], F32, kind="ExternalInput").ap()
    cA_d = dt_("cA", [128, 384], F32, kind="ExternalInput").ap()
    sbm_d = dt_("sbmask", [128, 2048], F32, kind="ExternalInput").ap()
    scm_d = dt_("scanmask", [128, 2048], F32, kind="ExternalInput").ap()
    invc_d = dt_("invc", [128, 64], F32, kind="ExternalInput").ap()
    cB_d = dt_("cB", [128, NCB], F32, kind="ExternalInput").ap()
    adaw_d = dt_("ada_w", [DEPTH, D, 6 * D], F32, kind="ExternalInput").ap()
    cwin_d = dt_("conv_w_in", [D, 2 * D], F32, kind="ExternalInput").ap()
    cwout_d = dt_("conv_w_out", [D, D], F32, kind="ExternalInput").ap()
    hwin_d = dt_("hgrn_w_in", [D, 4 * D], F32, kind="ExternalInput").ap()
    hwout_d = dt_("hgrn_w_out", [D, D], F32, kind="ExternalInput").ap()
    pw_d = dt_("pool_w", [4, 256, 256], F32, kind="ExternalInput").ap()
    sqkv_d = dt_("sb_w_qkv", [D, 3 * D], F32, kind="ExternalInput").ap()
    swout_d = dt_("sb_w_out", [D, D], F32, kind="ExternalInput").ap()
    wgL_d = dt_("wgL", [DEPTH * NEXP * 128, 4096], F32, kind="ExternalInput").ap()
    wuL_d = dt_("wuL", [DEPTH * NEXP * 128, 4096], F32, kind="ExternalInput").ap()
    wdL_d = dt_("wdL", [DEPTH * NEXP * 128, 4096], F32, kind="ExternalInput").ap()
    xs_d = dt_("xs_scr", [NSLOT, D], BF16, kind="Internal").ap()
    ys_d = dt_("ys_scr", [NSLOT, D], BF16, kind="Internal").ap()
    out_d = dt_("outT", [128, KC, S], F32, kind="ExternalOutput").ap()

    es = ExitStack()
    with es:
        arena = es.enter_context(nc.sbuf_tensor("arena", [128, ARENA_BYTES // 2], BF16))

        def carve(off, n, dt=BF16, parts=128):
            if dt == BF16:
                a = arena[0:parts, off // 2: off // 2 + n]
            else:
                a = arena[0:parts, off // 2: off // 2 + 2 * n].bitcast(F32)
            return a

        def v3(ap, a):
            return ap.rearrange("p (a b) -> p a b", a=a)

        sb = lambda name, shape, dt: es.enter_context(nc.sbuf_tensor(name, shape, dt))
        VEC = sb("VEC", [128, NVEC], F32)
        BRB = sb("BRB", [128, DEPTH * 36], F32)
        MOD = sb("MOD", [128, DEPTH * 48], F32)
        DER = sb("DER", [128, DEPTH * 16 + 32], F32)
        CST = sb("CST", [128, 384], F32)
        IDB = sb("IDB", [128, 128], BF16)
        ONESB = sb("ONESB", [128, 128], BF16)
        TRIB = sb("TRIB", [128, 128], BF16)
        CA = sb("CA", [128, 8], F32)
        SMALL = sb("SMALL", [128, 64], F32)
        CSTB = sb("CSTB", [128, NCB], F32)
        DIDX = sb("DIDX", [128, 32], I32)
        WIDX = sb("WIDX", [128, NSTEP], I32)
        GATE = sb("GATE", [128, 32], F32)
        NEGM = sb("NEGM", [128, 128], BF16)
        TH16 = CSTB[:, 0:16]
        BIDX = CSTB[:, 16:80]
        PIDX = CSTB[:, 80:81]
        banks = [es.enter_context(nc.psum_tensor("bank%d" % i, [128, 512], F32)) for i in range(8)]
        IDF = CST[:, 0:128]
        MASK2 = CST[:, 256:384]

        P = Prog(nc, es)
        bres = [P.res("bank%d" % i) for i in range(8)]
        bptr = [0]

        def psum():
            i = bptr[0]
            bptr[0] = (i + 1) % 8
            return banks[i], bres[i]

        XT = v3(carve(0, KC * S, F32), KC)
        HT = v3(carve(65536, KC * S, BF16), KC)
        rXT = [P.res("XT%d" % i) for i in range(4)]
        rHT = [P.res("HT%d" % i) for i in range(4)]
        rC = P.res("consts")
        rVEC = P.res("VEC")
        rMOD = P.res("MOD")

        def V(name, c0=0, n=1):
            o = VOFF[name] + c0
            return VEC[:, o:o + n]

        P.dma("sp", VEC[:], vec_d, w=[rVEC])
        P.dma("sp", BRB[:], br_d, w=[rC])
        P.dma("sp", CST[:], cA_d, w=[rC])
        P.dma("sp", CSTB[:], cB_d, w=[rC])

        def _mk_regs(eng):
            P.regs["wb"] = eng.alloc_register("wbound")
            eng.reg_mov(P.regs["wb"], DEPTH * NEXP * 128 - 1)
            P.regs["xb"] = eng.alloc_register("xbound")
            eng.reg_mov(P.regs["xb"], NSLOT - 1)
        P.q["pool"].append(_mk_regs)
        for i in range(4):
            P.dma("sp", XT[:, :, i * 512:(i + 1) * 512], xT_d[:, :, i * 512:(i + 1) * 512], w=[rXT[i]])
        HTflat = carve(65536, KC * S, BF16)
        rZ = P.res("Z")
        P.op("pool", MS(HTflat, 0.0), w=rHT)
        for j in range(NSLOT // 2048):
            P.dma("sp", xs_d[j * 2048:(j + 1) * 2048, :].rearrange("(p a) n -> p (a n)", p=128), HTflat,
                  r=rHT, w=[], dres=rZ, nowait_w=[rZ])
        P.op("dve", CP(IDB[:], CST[:, 0:128]), r=[rC], w=[rC])
        P.op("dve", CP(TRIB[:], CST[:, 128:256]), r=[rC], w=[rC])
        P.op("dve", MS(ONESB[:], 1.0), w=[rC])
        P.op("act", ACT(CA[:], V("c", 0, 8), AF.Silu), r=[rVEC], w=[rC])

        NAS = 2
        adaw_slots = [v3(carve(M0 + s_ * 16384, KC * 512, F32), KC) for s_ in range(NAS)]
        r_adaw = [P.res("adaw%d" % s_) for s_ in range(NAS)]
        ROW = carve(M0 + 32768, 6 * D, F32, parts=1)
        CAb = CA
        rROW = P.res("ROW")
        P.op("dve", MS(SMALL[:, 0:1], EPS), w=[rC])
        P.op("dve", MS(SMALL[:, 1:2], 1.0), w=[rC])
        if "mods" in phases:
            for i in range(DEPTH):
                for blk in range(12):
                    sl = (i * 12 + blk) % NAS
                    P.dma("sp", adaw_slots[sl][:],
                          adaw_d[i, :, blk * 512:(blk + 1) * 512].rearrange("(k p) n -> p k n", p=128),
                          w=[r_adaw[sl]])
                    ps, pr = psum()
                    P.op("pe", [MM(ps[0:1, :], CAb[:, k:k + 1], adaw_slots[sl][:, k, :], st=(k == 0), sp=(k == KC - 1))
                                for k in range(KC)], r=[r_adaw[sl], rC], w=[pr])
                    P.op("act", ACT(ROW[0:1, blk * 512:(blk + 1) * 512], ps[0:1, :], AF.Copy), r=[pr], w=[rROW])
                ps, pr = psum()
                P.op("pe", [MM(ps[:, j:j + 1], ROW[0:1, j * 128:(j + 1) * 128], SMALL[0:1, 1:2]) for j in range(48)],
                     r=[rROW, rC], w=[pr])
                P.op("dve", TT(MOD[:, i * 48:(i + 1) * 48], ps[:, 0:48], V("adab%d" % i, 0, 48), ALU.add),
                     r=[pr, rVEC], w=[rMOD])
                P.op("dve", STT(DER[:, i * 16:i * 16 + 8], MOD[:, i * 48 + 8:i * 48 + 16], 1.0, V("gmix%d" % i, 0, 8),
                                ALU.add, ALU.mult), r=[rMOD, rVEC], w=[rMOD])
                P.op("dve", STT(DER[:, i * 16 + 8:i * 16 + 16], MOD[:, i * 48 + 32:i * 48 + 40], 1.0,
                                V("gffn%d" % i, 0, 8), ALU.add, ALU.mult), r=[rMOD, rVEC], w=[rMOD])
        P.barrier()

        def modv(i, which, c):
            o = i * 48 + which * 8 + c
            return MOD[:, o:o + 1]

        def norm_phase(A_of, B_of, out_mode, layer=0):
            base = M0
            SQ = v3(carve(base, KC * 256, BF16), KC)
            TMPs = [v3(carve(base + 4096 + s_ * 8192, KC * 256, F32), KC) for s_ in range(2)]
            RSTDs = [carve(base + 4096 + 16384 + s_ * 1024, 256, F32) for s_ in range(2)]
            rSQ = P.res("SQ")
            rTMP = [P.res("TMP0"), P.res("TMP1")]
            rRS = [P.res("RS0"), P.res("RS1")]
            rLOG = P.res("LOG")
            LOG = None
            if out_mode == "ffn":
                LOG = v3(carve(base + 24576, 16 * 36, F32), 16)
                WR = v3(carve(base + 24576 + 4096, KC * 36, F32), KC)
                rWR = P.res("WR")
                P.dma("sp", WR[:], wr_d[:, layer, :, :], w=[rWR])
            rOUT = [P.res("OUT0"), P.res("OUT1")]
            def n1a(tt):
                sl = slice(tt * 256, (tt + 1) * 256)
                xt_r = rXT[tt // 2]
                P.op("act", ACT(SQ[:], XT[:, :, sl], AF.Square), r=[xt_r], w=[rSQ])
                ps, pr = psum()
                P.op("pe", [MM(ps[:, 0:256], ONESB[:], SQ[:, c, :], st=(c == 0), sp=(c == KC - 1)) for c in range(KC)],
                     r=[rSQ, rC], w=[pr])
                return ps, pr

            def n1b(tt, ps, pr):
                sl = slice(tt * 256, (tt + 1) * 256)
                xt_r = rXT[tt // 2]
                s_ = tt % 2
                P.op("act", ACT(RSTDs[s_], ps[:, 0:256], AF.Sqrt, bias=SMALL[:, 0:1], scale=1.0 / D), r=[pr, rC], w=[rRS[s_]])
                P.op("dve", lambda e, o=RSTDs[s_]: e.reciprocal(out=o, in_=o), r=[rRS[s_]], w=[rRS[s_]])
                for c in range(KC):
                    P.op("dve", STT(TMPs[s_][:, c, :], XT[:, c, sl], A_of(c), RSTDs[s_], ALU.mult, ALU.mult),
                         r=[xt_r, rRS[s_], rMOD], w=[rTMP[s_]])

            def n2(tt):
                sl = slice(tt * 256, (tt + 1) * 256)
                s_ = tt % 2
                if out_mode == "mix":
                    for c in range(KC):
                        P.op("act", ACT(HT[:, c, sl], TMPs[s_][:, c, :], AF.Identity, bias=B_of(c), scale=1.0),
                             r=[rTMP[s_], rMOD], w=[rHT[tt // 2]])
                elif out_mode == "ffn":
                    for c in range(KC):
                        P.op("act", ACT(TMPs[s_][:, c, :], TMPs[s_][:, c, :], AF.Identity, bias=B_of(c), scale=1.0),
                             r=[rMOD], w=[rTMP[s_]])
                    P.op("pool", CP(HT[:, :, sl], TMPs[s_][:]), r=[rTMP[s_]], w=[rHT[tt // 2]])
                    for sub in range(2):
                        ps2, pr2 = psum()
                        P.op("pe", [MM(ps2[:, 0:36], TMPs[s_][:, k, sub * 128:(sub + 1) * 128], WR[:, k, :],
                                       st=(k == 0), sp=(k == KC - 1)) for k in range(KC)],
                             r=[rTMP[s_], rWR], w=[pr2])
                        P.op("dve", TT(LOG[:, tt * 2 + sub, :], ps2[:, 0:36], BRB[:, layer * 36:(layer + 1) * 36], ALU.add),
                             r=[pr2, rC], w=[rLOG])
                else:
                    P.dma("sp", out_d[:, :, sl], TMPs[s_][:], r=[rTMP[s_]], w=[], dres=rOUT[s_])

            pend = n1a(0)
            n1b(0, *pend)
            for tt in range(8):
                if tt + 1 < 8:
                    pend = n1a(tt + 1)
                n2(tt)
                if tt + 1 < 8:
                    n1b(tt + 1, *pend)
            return LOG, rLOG, rOUT

        def moe_phase(layer, LOG, rLOG):
            base = M0
            rb = base + 32768
            nR = [0]

            def rt(n):
                a = carve(rb + nR[0], n, F32)
                nR[0] += n * 4
                return a
            rR = P.res("route")
            GL = LOG[:, :, 0:4]
            EL = LOG[:, :, 4:36]
            GMAX = rt(16)
            GSH = v3(rt(64), 16)
            OHG = v3(rt(64), 16)
            GE = v3(rt(64), 16)
            GSUM = rt(16)
            GP = rt(16)
            PEN = v3(rt(64), 16)
            ELM = v3(rt(512), 16)
            V1 = rt(16)
            D1 = v3(rt(512), 16)
            OH1 = v3(rt(512), 16)
            ELM2 = v3(rt(512), 16)
            V2 = rt(16)
            OH2 = v3(rt(512), 16)
            DV = rt(16)
            E21 = rt(16)
            P1 = rt(16)
            C1 = rt(16)
            C2 = rt(16)

            def bc3(a, n):
                return a.unsqueeze(2).to_broadcast([128, 16, n])

            def dv(fn, r=(), w=()):
                P.op("dve", fn, r=list(r) + [rLOG, rR], w=list(w) + [rR])
            dv(lambda e: e.tensor_reduce(out=GMAX, in_=GL, axis=AX.X, op=ALU.max))
            dv(TT(GSH[:], GL, bc3(GMAX, 4), ALU.subtract))
            dv(TS(OHG[:], GSH[:], 0.0, None, ALU.is_equal))
            P.op("act", ACT(GE[:], GSH[:], AF.Exp), r=[rR], w=[rR])
            dv(lambda e: e.tensor_reduce(out=GSUM, in_=GE[:], axis=AX.X, op=ALU.add))
            dv(lambda e: e.reciprocal(out=GP, in_=GSUM))
            dv(TS(PEN[:], OHG[:], -1.0, BIG, ALU.add, ALU.mult))
            ELM4 = ELM[:].rearrange("p a (g x) -> p a g x", g=4)
            EL4 = EL.rearrange("p a (g x) -> p a g x", g=4)
            dv(TT(ELM4, EL4, PEN[:].unsqueeze(3).to_broadcast([128, 16, 4, 8]), ALU.add))
            dv(lambda e: e.tensor_reduce(out=V1, in_=ELM[:], axis=AX.X, op=ALU.max))
            dv(TT(D1[:], ELM[:], bc3(V1, 32), ALU.subtract))
            dv(TS(OH1[:], D1[:], 0.0, None, ALU.is_equal))
            dv(STT(ELM2[:], OH1[:], -BIG, ELM[:], ALU.mult, ALU.add))
            dv(lambda e: e.tensor_reduce(out=V2, in_=ELM2[:], axis=AX.X, op=ALU.max))
            dv(TT(D1[:], ELM2[:], bc3(V2, 32), ALU.subtract))
            dv(TS(OH2[:], D1[:], 0.0, None, ALU.is_equal))
            dv(TT(DV, V2, V1, ALU.subtract))
            P.op("act", ACT(E21, DV, AF.Exp), r=[rR], w=[rR])
            dv(TS(P1, E21, 1.0, None, ALU.add))
            dv(lambda e: e.reciprocal(out=P1, in_=P1))
            dv(TT(C1, P1, GP, ALU.mult))
            dv(TT(C2, E21, C1, ALU.mult))
            rIDX = P.res("IDX")
            MBo = rb + nR[0]
            nR[0] += 1024
            MB = v3(carve(MBo, 512, BF16), 16)
            CNT = rt(32)
            CMP3 = v3(rt(512), 32)
            NBK = rt(32)
            T3 = v3(rt(1024), 32)
            LE3 = v3(rt(1024), 32)
            PEND = rt(32)
            PST = rt(32)
            DA = v3(rt(512), 16)
            DF = rt(32)
            CMPB3 = v3(rt(NSTEP * 32), NSTEP)
            EBf = rt(NSTEP)
            INV = rt(NSTEP)
            WF = rt(NSTEP)
            BST = BIDX[:, 0:NSTEP]
            dv(TT(MB[:], OH1[:], OH2[:], ALU.add))
            psR, prR = psum()
            fns = []
            for t in range(16):
                fns.append(MM(psR[:, t * 32:(t + 1) * 32], TRIB[:], MB[:, t, :], st=True, sp=(t == 15)))
                for t2 in range(t + 1, 16):
                    fns.append(MM(psR[:, t * 32:(t + 1) * 32], ONESB[:], MB[:, t2, :], st=False, sp=(t2 == 15)))
            P.op("pe", fns, r=[rR, rC], w=[prR])
            psC, prC = psum()
            P.op("pe", [MM(psC[:, 0:32], ONESB[:], MB[:, t, :], st=(t == 0), sp=(t == 15)) for t in range(16)],
                 r=[rR, rC], w=[prC])
            dv(CP(CNT, psC[:, 0:32]), r=[prC])
            dv(TT(CMP3[:], CNT.unsqueeze(2).to_broadcast([128, 32, 16]), TH16.unsqueeze(1).to_broadcast([128, 32, 16]),
                  ALU.is_gt), r=[rC])
            dv(lambda e: e.tensor_reduce(out=NBK, in_=CMP3[:], axis=AX.X, op=ALU.add))
            IO32 = BIDX[:, 0:32]
            dv(TT(LE3[:], IO32.unsqueeze(1).to_broadcast([128, 32, 32]), IO32.unsqueeze(2).to_broadcast([128, 32, 32]),
                  ALU.is_le), r=[rC])
            dv(TT(T3[:], LE3[:], NBK.unsqueeze(1).to_broadcast([128, 32, 32]), ALU.mult))
            dv(lambda e: e.tensor_reduce(out=PEND, in_=T3[:], axis=AX.X, op=ALU.add))
            dv(TT(PST, PEND, NBK, ALU.subtract))
            dv(TS(PST, PST, 256.0, None, ALU.mult))
            dv(TT(DA[:], v3(psR[:], 16), PST.unsqueeze(1).to_broadcast([128, 16, 32]), ALU.add), r=[prR])
            dv(TT(D1[:], OH1[:], DA[:], ALU.mult))
            dv(lambda e: e.tensor_reduce(out=DF[:, 0:16], in_=D1[:], axis=AX.X, op=ALU.add))
            dv(TT(D1[:], OH2[:], DA[:], ALU.mult))
            dv(lambda e: e.tensor_reduce(out=DF[:, 16:32], in_=D1[:], axis=AX.X, op=ALU.add))
            dv(CP(DIDX[:], DF), w=[rIDX])
            dv(CP(GATE[:, 0:16], C1), w=[rIDX])
            dv(CP(GATE[:, 16:32], C2), w=[rIDX])
            dv(TT(CMPB3[:], PEND.unsqueeze(1).to_broadcast([128, NSTEP, 32]), BST.unsqueeze(2).to_broadcast([128, NSTEP, 32]),
                  ALU.is_le), r=[rC])
            dv(lambda e: e.tensor_reduce(out=EBf, in_=CMPB3[:], axis=AX.X, op=ALU.add))
            dv(TS(INV, BST, PEND[:, 31:32], None, ALU.is_ge), r=[rC])
            dv(TS(EBf, EBf, 31.0, None, ALU.min))
            dv(TS(WF, EBf, float(layer * NEXP), 128.0, ALU.add, ALU.mult))
            dv(TS(WF, WF, PIDX, None, ALU.add), r=[rC])
            dv(STT(WF, INV, 1.0e6, WF, ALU.mult, ALU.add))
            dv(CP(WIDX[:], WF), w=[rIDX])
            P.barrier()

            IOA = bass.IndirectOffsetOnAxis
            Wslots = [L0, M0]

            def wviews(slot):
                o = Wslots[slot]
                return carve(o, 4096, BF16), carve(o + 8192, 4096, BF16), carve(o + 16384, 4096, BF16)
            rWGU = [P.res("WGU0"), P.res("WGU1")]
            rWD = [P.res("WD0"), P.res("WD1")]

            perm = []
            for s_i in range(NSTEP // 2):
                perm += [s_i, NSTEP - 1 - s_i]

            def bid(n):
                return 2 * perm[n // 2] + (n % 2)

            def wgather(dst, src, pos):
                col = perm[pos]
                return lambda eng: eng.indirect_dma_start(out=dst, out_offset=None, in_=src,
                                                          in_offset=IOA(ap=WIDX[:, col:col + 1], axis=0),
                                                          bounds_check=P.regs["wb"], oob_is_err=False)

            def load_wgu(st):
                wg, wu, _ = wviews(st % 2)
                P.dmaf("pool", wgather(wg, wgL_d, st), r=[rIDX], w=[rWGU[st % 2]])
                P.dmaf("pool", wgather(wu, wuL_d, st), r=[rIDX], w=[rWGU[st % 2]])

            def load_wd(st):
                _, _, wdn = wviews(st % 2)
                P.dmaf("pool", wgather(wdn, wdL_d, st), r=[rIDX], w=[rWD[st % 2]])

            eb = M0 + 24576
            HTOK = [carve(eb + s_ * 2048, 1024, BF16) for s_ in range(2)]
            NXS = 4
            XSL = [carve(eb + 49152 + s_ * 2048, 1024, BF16) for s_ in range(NXS)]
            XF = [carve(eb + 8192 + s_ * 2048, 1024, BF16) for s_ in range(2)]
            SS = [carve(eb + 12288 + s_ * 2048, 512, F32) for s_ in range(2)]
            HIDS = [carve(eb + 16384 + s_ * 1024, 512, BF16) for s_ in range(2)]
            HIDT = [carve(eb + 18432 + s_ * 1024, 512, BF16) for s_ in range(2)]
            YS = [carve(eb + 20480 + s_ * 2048, 1024, BF16) for s_ in range(2)]
            NYG = 4
            YG = [[carve(eb + 24576 + (k * NYG + s_) * 2048, 1024, BF16) for s_ in range(NYG)] for k in range(2)]
            YC = [carve(eb + 40960 + s_ * 4096, 1024, F32) for s_ in range(2)]
            two = lambda n: [P.res(n + "0"), P.res(n + "1")]
            rHTOK, rXF, rSS, rHIDS, rHIDT, rYS = two("HTOK"), two("XF"), two("SS"), two("HIDS"), two("HIDT"), two("YS")
            rXSL = [P.res("XSL%d" % i) for i in range(NXS)]
            rXs, rYs = two("Xs"), two("Ys")
            rYG = [[P.res("YG%d_%d" % (k, i)) for i in range(NYG)] for k in range(2)]
            rYC = two("YC")

            load_wgu(0)
            load_wd(0)
            load_wgu(1)
            load_wd(1)
            for t16 in range(16):
                b = t16 % 2
                ps, pr = psum()
                pb_ = ps[:].bitcast(BF16)
                P.op("pe", [TR(pb_[:, k * 128:(k + 1) * 128], HT[:, k, t16 * 128:(t16 + 1) * 128], IDB[:]) for k in range(KC)],
                     r=[rHT[t16 // 4], rC], w=[pr])
                if b == 0:
                    P.op("act", ACT(HTOK[b], pb_[:, 0:1024], AF.Copy), r=[pr], w=[rHTOK[b]])
                else:
                    P.op("dve", CP(HTOK[b], pb_[:, 0:1024]), r=[pr], w=[rHTOK[b]])
                for k in range(2):
                    c = k * 16 + t16
                    P.dmaf("pool", lambda eng, b=b, c=c: eng.indirect_dma_start(
                        out=xs_d, out_offset=IOA(ap=DIDX[:, c:c + 1], axis=0), in_=HTOK[b], in_offset=None,
                        bounds_check=P.regs["xb"], oob_is_err=False),
                        r=[rHTOK[b], rIDX], w=[], dres=rXs[b], nowait_w=[rXs[b]])

            def xload(bi):
                x4 = bi % NXS
                P.dma("sp", XSL[x4], xs_d[bid(bi) * 128:(bid(bi) + 1) * 128, :], r=[rXs[0], rXs[1]], w=[rXSL[x4]])

            def stage_Tx(bi):
                s2 = bi % 2
                x4 = bi % NXS
                ps, pr = psum()
                pb_ = ps[:].bitcast(BF16)
                P.op("pe", [TR(pb_[:, k * 128:(k + 1) * 128], XSL[x4][:, k * 128:(k + 1) * 128], IDB[:]) for k in range(KC)],
                     r=[rXSL[x4], rC], w=[pr])
                P.op("act", ACT(XF[s2], pb_[:, 0:1024], AF.Copy), r=[pr], w=[rXF[s2]])

            def stage_GU(bi):
                s2 = bi % 2
                ws = (bi // 2) % 2
                wg, wu, _ = wviews(ws)
                psg, prg = psum()
                P.op("pe", [MM(psg[:], XF[s2][:, k * 128:(k + 1) * 128], wg[:, k * 512:(k + 1) * 512], st=(k == 0), sp=(k == KC - 1))
                            for k in range(KC)], r=[rXF[s2], rWGU[ws]], w=[prg])
                psu, pru = psum()
                P.op("pe", [MM(psu[:], XF[s2][:, k * 128:(k + 1) * 128], wu[:, k * 512:(k + 1) * 512], st=(k == 0), sp=(k == KC - 1))
                            for k in range(KC)], r=[rXF[s2], rWGU[ws]], w=[pru])
                P.op("act", ACT(SS[s2], psg[:], AF.Silu), r=[prg], w=[rSS[s2]])
                P.op("dve", TT(HIDS[s2], psu[:], SS[s2], ALU.mult), r=[pru, rSS[s2]], w=[rHIDS[s2]])

            def stage_Th(bi):
                s2 = bi % 2
                ps, pr = psum()
                pb_ = ps[:].bitcast(BF16)
                P.op("pe", [TR(pb_[:, f * 128:(f + 1) * 128], HIDS[s2][:, f * 128:(f + 1) * 128], IDB[:]) for f in range(4)],
                     r=[rHIDS[s2], rC], w=[pr])
                P.op("dve", CP(HIDT[s2], pb_[:, 0:512]), r=[pr], w=[rHIDT[s2]])

            def stage_D(bi):
                s2 = bi % 2
                ws = (bi // 2) % 2
                _, _, wdn = wviews(ws)
                for half in range(2):
                    ps, pr = psum()
                    P.op("pe", [MM(ps[:], HIDT[s2][:, f * 128:(f + 1) * 128],
                                   wdn[:, f * 1024 + half * 512:f * 1024 + (half + 1) * 512], st=(f == 0), sp=(f == 3))
                                for f in range(4)], r=[rHIDT[s2], rWD[ws]], w=[pr])
                    if half == 0:
                        P.op("act", ACT(YS[s2][:, 0:512], ps[:], AF.Copy), r=[pr], w=[rYS[s2]])
                    else:
                        P.op("dve", CP(YS[s2][:, 512:1024], ps[:]), r=[pr], w=[rYS[s2]])
                P.dma("sp", ys_d[bid(bi) * 128:(bid(bi) + 1) * 128, :], YS[s2], r=[rYS[s2]], w=[], dres=rYs[s2], nowait_w=[rYs[s2]])

            for bi in range(NXS - 1):
                xload(bi)
            stage_Tx(0)
            for i in range(NSUB + 1):
                if i >= 1:
                    stage_Th(i - 1)
                if i + NXS - 1 < NSUB:
                    xload(i + NXS - 1)
                if i + 1 < NSUB:
                    stage_Tx(i + 1)
                if i < NSUB:
                    stage_GU(i)
                    if i % 2 == 1 and i // 2 + 2 < NSTEP:
                        load_wgu(i // 2 + 2)
                if i >= 1:
                    stage_D(i - 1)
                    if (i - 1) % 2 == 1 and (i - 1) // 2 + 2 < NSTEP:
                        load_wd((i - 1) // 2 + 2)
            P.barrier()

            def issue_gather(t16):
                g = t16 % NYG
                for k in range(2):
                    c = k * 16 + t16
                    P.dmaf("pool", lambda eng, g=g, c=c, k=k: eng.indirect_dma_start(
                        out=YG[k][g], out_offset=None, in_=ys_d, in_offset=IOA(ap=DIDX[:, c:c + 1], axis=0),
                        bounds_check=P.regs["xb"], oob_is_err=False),
                        r=[rYs[0], rYs[1], rIDX], w=[rYG[k][g]])
            def k1(t16):
                b = t16 % 2
                g = t16 % NYG
                P.op("act", ACT(YC[b], YG[0][g], AF.Copy, scale=GATE[:, t16:t16 + 1]), r=[rYG[0][g], rIDX], w=[rYC[b]])
                P.op("dve", STT(YC[b], YG[1][g], GATE[:, 16 + t16:17 + t16], YC[b], ALU.mult, ALU.add),
                     r=[rYG[1][g], rIDX], w=[rYC[b]])

            def k2(t16):
                b = t16 % 2
                tok = slice(t16 * 128, (t16 + 1) * 128)
                for half in range(2):
                    ps, pr = psum()
                    P.op("pe", [TR(ps[:, j * 128:(j + 1) * 128], YC[b][:, (half * 4 + j) * 128:(half * 4 + j + 1) * 128], IDF)
                                for j in range(4)], r=[rYC[b], rC], w=[pr])
                    for j in range(4):
                        dc = half * 4 + j
                        P.op("dve", STT(XT[:, dc, tok], ps[:, j * 128:(j + 1) * 128], modv(layer, 5, dc), XT[:, dc, tok],
                                        ALU.mult, ALU.add), r=[pr, rMOD], w=[rXT[t16 // 4]])

            for t16 in range(NYG - 1):
                issue_gather(t16)
            k1(0)
            for t16 in range(16):
                if t16 + NYG - 1 < 16:
                    issue_gather(t16 + NYG - 1)
                if t16 + 1 < 16:
                    k1(t16 + 1)
                k2(t16)
            P.barrier()

        def mixer_conv(layer):
            UT = v3(carve(M0, KC * 2080, BF16), KC)
            o = M0 + 33280
            WIN = [v3(carve(o + s_ * 2048, KC * 128, BF16), KC) for s_ in range(4)]
            o += 8192
            SIG = [carve(o + s_ * 2048, 512, F32) for s_ in range(2)]
            o += 4096
            DG = [v3(carve(o + s_ * 8192, 31 * 128, BF16), 31) for s_ in range(2)]
            WOUT = v3(carve(o, KC * D, BF16), KC)
            o += 16384
            YSQ = v3(carve(o, KC * 512, BF16), KC)
            o += 8192
            T1 = [carve(o + s_ * 2048, 512, F32) for s_ in range(2)]
            o += 4096
            MEAN = carve(o, 512, F32)
            MSQ = carve(o + 2048, 512, F32)
            RSTD = carve(o + 4096, 512, F32)
            o += 6144
            rUT = P.res("UT")
            rWIN = [P.res("WIN%d" % s_) for s_ in range(4)]
            rSIG = [P.res("SIG0"), P.res("SIG1")]
            rDG = [P.res("DG0"), P.res("DG1")]
            Y = HT
            P.op("pool", MS(UT[:, :, 0:32], 0.0), w=[rUT])
            for c in range(KC):
                sa, sg = (2 * c) % 4, (2 * c + 1) % 4
                P.dma("pool", WIN[sa][:], cwin_d[:, c * 128:(c + 1) * 128].rearrange("(k p) n -> p k n", p=128), w=[rWIN[sa]])
                P.dma("pool", WIN[sg][:], cwin_d[:, D + c * 128:D + (c + 1) * 128].rearrange("(k p) n -> p k n", p=128),
                      w=[rWIN[sg]])
                for tt in range(4):
                    sl = slice(tt * 512, (tt + 1) * 512)
                    s2 = (c * 4 + tt) % 2
                    psa, pra = psum()
                    P.op("pe", [MM(psa[:], WIN[sa][:, k, :], HT[:, k, sl], st=(k == 0), sp=(k == KC - 1)) for k in range(KC)],
                         r=[rWIN[sa], rHT[tt]], w=[pra])
                    psg, prg = psum()
                    P.op("pe", [MM(psg[:], WIN[sg][:, k, :], HT[:, k, sl], st=(k == 0), sp=(k == KC - 1)) for k in range(KC)],
                         r=[rWIN[sg], rHT[tt]], w=[prg])
                    P.op("act", ACT(SIG[s2], psg[:], AF.Sigmoid, bias=V("cbin", 8 + c), scale=1.0), r=[prg, rVEC], w=[rSIG[s2]])
                    P.op("dve", STT(UT[:, c, 32 + tt * 512:32 + (tt + 1) * 512], psa[:], V("cbin", c), SIG[s2], ALU.add, ALU.mult),
                         r=[pra, rSIG[s2], rVEC], w=[rUT])
            P.barrier()
            for c in range(KC):
                ds = c % 2
                P.op("dve", [TS(DG[ds][:, j, :], IDB[:], V("wdw", c * 31 + j), None, ALU.mult) for j in range(31)],
                     r=[rVEC, rC], w=[rDG[ds]])
                for tt in range(4):
                    ps, pr = psum()
                    P.op("pe", [MM(ps[:], DG[ds][:, j, :], UT[:, c, tt * 512 + j + 2: tt * 512 + j + 2 + 512], st=(j == 0), sp=(j == 30))
                                for j in range(31)], r=[rDG[ds], rUT], w=[pr])
                    P.op("act", ACT(Y[:, c, tt * 512:(tt + 1) * 512], ps[:], AF.Identity, bias=V("cbdw", c), scale=1.0),
                         r=[pr, rVEC], w=[rHT[tt]])
            P.barrier()
            rW_ = P.res("WOUT")
            P.dma("pool", WOUT[:], cwout_d.rearrange("(k p) n -> p k n", p=128), w=[rW_])
            rYSQ = P.res("YSQ")
            rST = P.res("stats")
            rT1 = [P.res("T1a"), P.res("T1b")]
            for tt in range(4):
                sl = slice(tt * 512, (tt + 1) * 512)
                P.op("act", ACT(YSQ[:], Y[:, :, sl], AF.Square), r=[rHT[tt]], w=[rYSQ])
                p1, r1 = psum()
                P.op("pe", [MM(p1[:], ONESB[:], Y[:, c, sl], st=(c == 0), sp=(c == KC - 1)) for c in range(KC)],
                     r=[rHT[tt], rC], w=[r1])
                p2, r2 = psum()
                P.op("pe", [MM(p2[:], ONESB[:], YSQ[:, c, :], st=(c == 0), sp=(c == KC - 1)) for c in range(KC)],
                     r=[rYSQ, rC], w=[r2])
                P.op("dve", TS(MEAN, p1[:], 1.0 / D, None, ALU.mult), r=[r1], w=[rST])
                P.op("dve", TT(MSQ, MEAN, MEAN, ALU.mult), r=[rST], w=[rST])
                P.op("dve", STT(RSTD, p2[:], 1.0 / D, MSQ, ALU.mult, ALU.subtract), r=[r2, rST], w=[rST])
                P.op("act", ACT(RSTD, RSTD, AF.Sqrt, bias=SMALL[:, 0:1], scale=1.0), r=[rST, rC], w=[rST])
                P.op("dve", lambda e: e.reciprocal(out=RSTD, in_=RSTD), r=[rST], w=[rST])
                for c in range(KC):
                    s2 = c % 2
                    P.op("dve", TT(T1[s2], Y[:, c, sl], MEAN, ALU.subtract), r=[rHT[tt], rST], w=[rT1[s2]])
                    P.op("pool", TT(T1[s2], T1[s2], RSTD, ALU.mult), r=[rST], w=[rT1[s2]])
                    P.op("act", ACT(Y[:, c, sl], T1[s2], AF.Silu, bias=V("clnb", c), scale=V("clng", c)),
                         r=[rT1[s2], rVEC], w=[rHT[tt]])
            for tt in range(4):
                sl = slice(tt * 512, (tt + 1) * 512)
                for dc in range(KC):
                    ps, pr = psum()
                    P.op("pe", [MM(ps[:], WOUT[:, c, dc * 128:(dc + 1) * 128], Y[:, c, sl], st=(c == 0), sp=(c == KC - 1))
                                for c in range(KC)], r=[rW_, rHT[tt]], w=[pr])
                    P.op("dve", STT(XT[:, dc, sl], ps[:], modv(layer, 2, dc), XT[:, dc, sl], ALU.mult, ALU.add),
                         r=[pr, rMOD], w=[rXT[tt]])
            P.barrier()

        def mixer_pool(layer):
            o = M0
            SA = [carve(o + s_ * 8192, S, F32) for s_ in range(2)]
            SB = [carve(o + 16384 + s_ * 8192, S, F32) for s_ in range(2)]
            PL = v3(carve(o + 32768, KC * S, BF16), KC)
            o2 = o + 32768 + 32768
            PW = carve(o2, 4 * 2 * 256, BF16).rearrange("p (g k n) -> p g k n", g=4, k=2)
            INVC = carve(o2 + 4096, 64, F32)
            T16 = [carve(o2 + 4096 + 256 + s_ * 64, 16, F32) for s_ in range(2)]
            YT = [carve(o2 + 8192 + s_ * 2048, 512, F32) for s_ in range(2)]
            GL_ = carve(o2 + 8192 + 4096, 16, F32)
            rPW = P.res("PW")
            rIN = P.res("INVC")
            rS = [P.res("S0"), P.res("S1")]
            rPL = [P.res("PL%d" % c) for c in range(KC)]
            rYT = [P.res("YT0"), P.res("YT1")]
            rGL = P.res("GL")
            rT16 = [P.res("T16a"), P.res("T16b")]
            P.dma("pool", PW, pw_d.rearrange("g (k p) n -> p g k n", p=128), w=[rPW])
            P.dma("sp", INVC, invc_d, w=[rIN])
            for c in range(KC):
                P.op("dve", TT(GL_[:, c:c + 1], modv(layer, 2, c), V("psc", c), ALU.mult), r=[rMOD, rVEC], w=[rGL])
                P.op("dve", TT(GL_[:, 8 + c:9 + c], GL_[:, c:c + 1], V("pb", c), ALU.mult), r=[rVEC], w=[rGL])
            for c in range(KC):
                gi = c // 2
                wnd = 2 << gi
                en = "pool" if c % 4 == 3 else "dve"
                s_ = 1 if en == "pool" else 0
                bufs = [SA[s_], SB[s_]]
                cur = HT[:, c, :]
                sh = 1
                bi = 0
                while sh < wnd:
                    nxt = bufs[bi]
                    P.op(en, [TT(nxt[:, sh:], cur[:, sh:], cur[:, 0:S - sh], ALU.add), CP(nxt[:, 0:sh], cur[:, 0:sh])],
                         r=[rHT[0], rHT[1], rHT[2], rHT[3]], w=[rS[s_]])
                    cur = nxt
                    bi ^= 1
                    sh *= 2
                oth = bufs[bi]
                P.op(en, TS(oth, cur, 1.0 / wnd, None, ALU.mult), r=[], w=[rS[s_]])
                P.op(en, TT(PL[:, c, :], oth, HT[:, c, :], ALU.subtract), r=[rHT[0], rHT[1], rHT[2], rHT[3], rS[s_]], w=[rPL[c]])
                P.op(en, TT(T16[s_], cur[:, 0:16], INVC[:, gi * 16:(gi + 1) * 16], ALU.mult), r=[rIN, rS[s_]], w=[rT16[s_]])
                P.op(en, TT(PL[:, c, 0:16], T16[s_], HT[:, c, 0:16], ALU.subtract), r=[rT16[s_]], w=[rPL[c]])
            i_ = 0
            for gi in range(4):
                for dn in range(2):
                    dc = gi * 2 + dn
                    for tt in range(4):
                        sl = slice(tt * 512, (tt + 1) * 512)
                        ps, pr = psum()
                        P.op("pe", [MM(ps[:], PW[:, gi, k, dn * 128:(dn + 1) * 128], PL[:, gi * 2 + k, sl], st=(k == 0), sp=(k == 1))
                                    for k in range(2)], r=[rPW, rPL[gi * 2], rPL[gi * 2 + 1]], w=[pr])
                        s2 = i_ % 2
                        i_ += 1
                        P.op("act", ACT(YT[s2], ps[:], AF.Identity, bias=GL_[:, 8 + dc:9 + dc], scale=GL_[:, dc:dc + 1]),
                             r=[pr, rGL], w=[rYT[s2]])
                        P.op("dve", TT(XT[:, dc, sl], XT[:, dc, sl], YT[s2], ALU.add), r=[rYT[s2]], w=[rXT[tt]])
            P.barrier()

        def mixer_hgrn(layer):
            o = M0
            A_ = carve(o, S, F32); o += 8192
            B_ = carve(o, S, F32); o += 8192
            C_ = carve(o, S, F32); o += 8192
            D_ = carve(o, S, F32); o += 8192
            QTb = carve(o, S, BF16); o += 4096
            KTb = carve(o, S, BF16); o += 4096
            KHT = carve(o, S, BF16); o += 4096
            KHtok = v3(carve(o, 16 * 128, BF16), 16); o += 4096
            Vt = v3(carve(o, 16 * 128, BF16), 16); o += 4096
            Gb = carve(o, S, BF16); o += 4096
            WQ = v3(carve(o, KC * 512, BF16), KC); o += 8192
            SCM = carve(o, S, F32); o += 8192
            WO = [carve(o + s_ * 2048, D, BF16) for s_ in range(2)]; o += 4096
            SF = carve(o, 128, F32); o += 512
            SBF = [carve(o + s_ * 256, 128, BF16) for s_ in range(2)]; o += 512
            SC = [carve(o + s_ * 256, 128, BF16) for s_ in range(2)]; o += 512
            LB = carve(o, 64, F32); o += 256
            RS = carve(o, 512, F32); o += 2048
            OSQ = KHT
            OG = KTb
            rA, rB, rCc, rD = P.res("A"), P.res("B"), P.res("C"), P.res("D")
            rQT, rKT, rKHT, rKHtok, rV, rG = P.res("QT"), P.res("KT"), P.res("KHT"), P.res("KHtok"), P.res("V"), P.res("G")
            rWQ, rSCM, rLB = P.res("WQ"), P.res("SCM"), P.res("LB")
            rWO = [P.res("WO0"), P.res("WO1")]
            rSF = P.res("SF")
            rSBF = [P.res("SBF0"), P.res("SBF1")]
            rSC = [P.res("SC0"), P.res("SC1")]
            rRS = P.res("RS")
            P.dma("sp", SCM, scm_d, w=[rSCM])
            EX = carve(o, 64, F32); o += 256
            for i in range(DEPTH):
                P.op("act", ACT(EX[:, i * 8:(i + 1) * 8], V("lbl%d" % i, 0, 8), AF.Exp), r=[rVEC], w=[rLB])
            P.op("dve", TT(LB[:, 24:32], EX[:, 0:8], EX[:, 8:16], ALU.add), r=[rLB], w=[rLB])
            P.op("dve", TT(LB[:, 24:32], LB[:, 24:32], EX[:, 16:24], ALU.add), r=[rLB], w=[rLB])
            P.op("dve", TT(LB[:, 24:32], LB[:, 24:32], EX[:, 24:32], ALU.add), r=[rLB], w=[rLB])
            P.op("dve", lambda e: e.reciprocal(out=LB[:, 24:32], in_=LB[:, 24:32]), r=[rLB], w=[rLB])
            P.op("dve", MS(LB[:, 0:8], 0.0), w=[rLB])
            for i in range(1, layer + 1):
                P.op("dve", TT(LB[:, 0:8], LB[:, 0:8], EX[:, i * 8:(i + 1) * 8], ALU.add), r=[rLB], w=[rLB])
            P.op("dve", TT(LB[:, 0:8], LB[:, 0:8], LB[:, 24:32], ALU.mult), r=[rLB], w=[rLB])
            P.op("dve", TS(LB[:, 8:16], LB[:, 0:8], -1.0, 1.0, ALU.mult, ALU.add), r=[rLB], w=[rLB])
            P.op("dve", TS(LB[:, 16:24], LB[:, 8:16], -1.0, None, ALU.mult), r=[rLB], w=[rLB])
            for h in range(KC):
                for part in range(4):
                    P.dma("pool", WQ[:, :, part * 128:(part + 1) * 128],
                          hwin_d[:, part * D + h * 128: part * D + (h + 1) * 128].rearrange("(k p) n -> p k n", p=128),
                          w=[rWQ])
                P.dma("pool", WO[h % 2], hwout_d[h * 128:(h + 1) * 128, :], w=[rWO[h % 2]])
                for tt in range(4):
                    sl = slice(tt * 512, (tt + 1) * 512)
                    for part, dst in ((0, "q"), (1, "f"), (3, "g")):
                        ps, pr = psum()
                        P.op("pe", [MM(ps[:], WQ[:, k, part * 128:(part + 1) * 128], HT[:, k, sl], st=(k == 0), sp=(k == KC - 1))
                                    for k in range(KC)], r=[rWQ, rHT[tt]], w=[pr])
                        if dst == "q":
                            P.op("act", ACT(D_[:, sl], ps[:], AF.Silu), r=[pr], w=[rD])
                        elif dst == "f":
                            P.op("act", ACT(A_[:, sl], ps[:], AF.Sigmoid), r=[pr], w=[rA])
                        else:
                            P.op("act", ACT(Gb[:, sl], ps[:], AF.Silu), r=[pr], w=[rG])
                for t16 in range(16):
                    ps, pr = psum()
                    P.op("pe", [MM(ps[:, 0:128], HT[:, k, t16 * 128:(t16 + 1) * 128], WQ[:, k, 256:384], st=(k == 0), sp=(k == KC - 1))
                                for k in range(KC)], r=[rWQ, rHT[t16 // 4]], w=[pr])
                    P.op("act", ACT(Vt[:, t16, :], ps[:, 0:128], AF.Copy), r=[pr], w=[rV])
                lb, oml, noml = LB[:, h:h + 1], LB[:, 8 + h:9 + h], LB[:, 16 + h:17 + h]
                P.op("dve", TS(B_, A_, oml, lb, ALU.mult, ALU.add), r=[rA, rLB], w=[rB])
                P.op("act", ACT(B_, B_, AF.Ln), r=[], w=[rB])
                P.op("act", ACT(A_, A_, AF.Identity, bias=oml, scale=noml), r=[rLB, rB], w=[rA])
                P.op("dve", lambda e: e.tensor_tensor_scan(out=C_, data0=SCM, data1=B_, initial=0.0, op0=ALU.mult, op1=ALU.add),
                     r=[rB, rSCM], w=[rCc])
                P.op("act", ACT(B_, C_, AF.Exp), r=[rCc], w=[rB])
                P.op("dve", TS(C_, C_, -1.0, 80.0, ALU.mult, ALU.min), r=[rB], w=[rCc])
                P.op("act", ACT(C_, C_, AF.Exp), r=[], w=[rCc])
                P.op("dve", TT(QTb, D_, B_, ALU.mult), r=[rD, rB], w=[rQT])
                P.op("dve", TT(A_, A_, C_, ALU.mult), r=[rCc], w=[rA])
                P.op("act", ACT(KTb, A_, AF.Copy), r=[rA], w=[rKT])
                A3 = A_.rearrange("p (n c) -> p n c", c=64)
                B3 = B_.rearrange("p (n c) -> p n c", c=64)
                K3 = KHT.rearrange("p (n c) -> p n c", c=64)
                P.op("dve", TT(K3, A3, B3[:, :, 63:64].to_broadcast([128, 32, 64]), ALU.mult), r=[rA, rB], w=[rKHT])
                for t16 in range(16):
                    ps, pr = psum()
                    pb_ = ps[:].bitcast(BF16)
                    P.op("pe", TR(pb_[:, 0:128], KHT[:, t16 * 128:(t16 + 1) * 128], IDB[:]), r=[rKHT, rC], w=[pr])
                    P.op("act", ACT(KHtok[:, t16, :], pb_[:, 0:128], AF.Copy), r=[pr], w=[rKHtok])
                P.op("dve", MS(SF, 0.0), w=[rSF])
                P.op("dve", MS(SBF[0], 0.0), w=[rSBF[0]])
                sv = 0
                for t16 in range(16):
                    tsl = slice(t16 * 128, (t16 + 1) * 128)
                    sc_i = t16 % 2
                    ps, pr = psum()
                    P.op("pe", MM(ps[:, 0:128], KTb[:, tsl], QTb[:, tsl]), r=[rKT, rQT], w=[pr])
                    P.op("dve", TT(SC[sc_i], ps[:, 0:128], MASK2, ALU.mult), r=[pr, rC], w=[rSC[sc_i]])
                    pa, pra = psum()
                    P.op("pe", [MM(pa[:, 0:64], SBF[sv], QTb[:, t16 * 128:t16 * 128 + 64], st=True, sp=False),
                                MM(pa[:, 0:64], Vt[0:64, t16, :], SC[sc_i][0:64, 0:64], st=False, sp=True)],
                         r=[rSBF[sv], rQT, rV, rSC[sc_i]], w=[pra])
                    pu, pru = psum()
                    P.op("pe", MM(pu[:, 0:128], KHtok[0:64, t16, :], Vt[0:64, t16, :]), r=[rKHtok, rV], w=[pru])
                    P.op("dve", STT(SF, SF, B_[:, t16 * 128 + 63:t16 * 128 + 64], pu[:, 0:128], ALU.mult, ALU.add),
                         r=[pru, rB], w=[rSF])
                    P.op("act", ACT(SBF[1 - sv], SF, AF.Copy), r=[rSF], w=[rSBF[1 - sv]])
                    sv = 1 - sv
                    pb2, prb = psum()
                    P.op("pe", [MM(pb2[:, 0:64], SBF[sv], QTb[:, t16 * 128 + 64:t16 * 128 + 128], st=True, sp=False),
                                MM(pb2[:, 0:64], Vt[:, t16, :], SC[sc_i][:, 64:128], st=False, sp=True)],
                         r=[rSBF[sv], rQT, rV, rSC[sc_i]], w=[prb])
                    pu2, pru2 = psum()
                    P.op("pe", MM(pu2[:, 0:128], KHtok[64:128, t16, :], Vt[64:128, t16, :]), r=[rKHtok, rV], w=[pru2])
                    P.op("dve", STT(SF, SF, B_[:, t16 * 128 + 127:t16 * 128 + 128], pu2[:, 0:128], ALU.mult, ALU.add),
                         r=[pru2, rB], w=[rSF])
                    P.op("act", ACT(SBF[1 - sv], SF, AF.Copy), r=[rSF], w=[rSBF[1 - sv]])
                    sv = 1 - sv
                    P.op("act", ACT(D_[:, t16 * 128:t16 * 128 + 64], pa[:, 0:64], AF.Copy), r=[pra, rQT], w=[rD])
                    P.op("act", ACT(D_[:, t16 * 128 + 64:t16 * 128 + 128], pb2[:, 0:64], AF.Copy), r=[prb], w=[rD])
                for tt in range(4):
                    sl = slice(tt * 512, (tt + 1) * 512)
                    P.op("act", ACT(OSQ[:, sl], D_[:, sl], AF.Square), r=[rD, rKHtok], w=[rKHT])
                    ps, pr = psum()
                    P.op("pe", MM(ps[:], ONESB[:], OSQ[:, sl]), r=[rKHT, rC], w=[pr])
                    P.op("act", ACT(RS, ps[:], AF.Sqrt, bias=SMALL[:, 0:1], scale=1.0 / 128), r=[pr, rC], w=[rRS])
                    P.op("dve", lambda e: e.reciprocal(out=RS, in_=RS), r=[rRS], w=[rRS])
                    P.op("dve", STT(D_[:, sl], D_[:, sl], V("hng", h), RS, ALU.mult, ALU.mult), r=[rRS, rVEC], w=[rD])
                    P.op("pool", TT(OG[:, sl], D_[:, sl], Gb[:, sl], ALU.mult), r=[rD, rG, rSC[0], rSC[1]], w=[rKT])
                    for dc in range(KC):
                        ps2, pr2 = psum()
                        P.op("pe", MM(ps2[:], WO[h % 2][:, dc * 128:(dc + 1) * 128], OG[:, sl]), r=[rWO[h % 2], rKT], w=[pr2])
                        P.op("dve", STT(XT[:, dc, sl], ps2[:], modv(layer, 2, dc), XT[:, dc, sl], ALU.mult, ALU.add),
                             r=[pr2, rMOD], w=[rXT[tt]])
            P.barrier()

        def mixer_sb(layer):
            scale = 64 ** -0.5
            o = L0
            QT = [carve(o + s_ * 4096, S, BF16) for s_ in range(2)]; o += 8192
            KT = [carve(o + s_ * 4096, S, BF16) for s_ in range(2)]; o += 8192
            VP = [v3(carve(o + hp * 4096, 16 * 128, BF16), 16) for hp in range(2)]; o += 8192
            OT = [carve(o + s_ * 4096, S, BF16) for s_ in range(2)]; o += 8192
            WQ = [v3(carve(o + s_ * 6144, KC * 384, BF16), KC) for s_ in range(2)]; o += 12288
            WO = [carve(o + s_ * 2048, D, BF16) for s_ in range(2)]; o += 4096
            SP = [carve(o + s_ * 8192, S, F32) for s_ in range(2)]; o += 16384
            A_ = [carve(o + s_ * 8192, S, F32) for s_ in range(2)]; o += 16384
            C_ = carve(o, S, F32); o += 8192
            W_ = [carve(o + s_ * 4096, S, BF16) for s_ in range(2)]; o += 8192
            WT = [v3(carve(o + s_ * 4096, 16 * 128, BF16), 16) for s_ in range(2)]; o += 8192
            assert o <= ARENA_BYTES, o
            NT = [SMALL[:, 8:9], SMALL[:, 9:10]]
            two = lambda n: [P.res(n + "0"), P.res(n + "1")]
            rQT, rKT, rOT, rWQ, rWO = two("QT"), two("KT"), two("OT"), two("WQ"), two("WO")
            rVP = P.res("VP")
            rSP = two("SP")
            rCc = P.res("C")
            rA, rW, rWT, rNT = two("A"), two("W"), two("WT"), two("NT")
            rNEG = P.res("NEGM")
            P.op("dve", TS(NEGM[:], CST[:, 128:256], -1.0, 30000.0, ALU.add, ALU.mult), r=[rC], w=[rNEG])
            po, pro = banks[7], bres[7]
            P.op("pool", MS(VP[0][:, :, 64:128], 0.0), w=[rVP])
            P.op("pool", MS(VP[1][:, :, 0:64], 0.0), w=[rVP])
            zb = [0]
            tb = [0]

            def zbank():
                i = zb[0]
                zb[0] = (i + 1) % 4
                return banks[i], bres[i]

            def tbank():
                i = 4 + tb[0]
                tb[0] = (tb[0] + 1) % 3
                return banks[i], bres[i]

            def load_weights(c):
                s_ = c % 2
                for part in range(3):
                    P.dma("pool", WQ[s_][:, :, part * 128:(part + 1) * 128],
                          sqkv_d[:, part * D + c * 128: part * D + (c + 1) * 128].rearrange("(k p) n -> p k n", p=128),
                          w=[rWQ[s_]])
                P.dma("pool", WO[s_], swout_d[c * 128:(c + 1) * 128, :], w=[rWO[s_]])

            def qk_piece(c, m):
                s_ = c % 2
                tt, part = m // 2, m % 2
                sl = slice(tt * 512, (tt + 1) * 512)
                dst, rd = ((QT[s_], rQT[s_]), (KT[s_], rKT[s_]))[part]
                ps, pr = tbank()
                P.op("pe", [MM(ps[:], WQ[s_][:, k, part * 128:(part + 1) * 128], HT[:, k, sl], st=(k == 0), sp=(k == KC - 1))
                            for k in range(KC)], r=[rWQ[s_], rHT[tt]], w=[pr])
                P.op("dve", CP(dst[:, sl], ps[:]), r=[pr], w=[rd])

            def qk_proj(c):
                for m in range(8):
                    qk_piece(c, m)

            def v_proj(c):
                s_ = c % 2
                for t16 in range(16):
                    ps, pr = tbank()
                    P.op("pe", [MM(ps[:, 0:128], HT[:, k, t16 * 128:(t16 + 1) * 128], WQ[s_][:, k, 256:384], st=(k == 0), sp=(k == KC - 1))
                                for k in range(KC)], r=[rWQ[s_], rHT[t16 // 4]], w=[pr])
                    P.op("dve", [CP(VP[0][:, t16, 0:64], ps[:, 0:64]), CP(VP[1][:, t16, 64:128], ps[:, 64:128])], r=[pr], w=[rVP])

            def stageA(u, c, qb, hp):
                s_ = c % 2
                b = u % 2
                hs = slice(hp * 64, (hp + 1) * 64)
                tq = slice(qb * 128, (qb + 1) * 128)
                nk = (qb + 1) * 128
                nch = (nk + 511) // 512
                zs = []
                for ch in range(nch):
                    w = min(512, nk - ch * 512)
                    cs = slice(ch * 512, ch * 512 + w)
                    pz, prz = zbank()
                    fns = [MM(pz[:, 0:w], QT[s_][hs, tq], KT[s_][hs, cs], st=True, sp=(ch != nch - 1))]
                    if ch == nch - 1:
                        fns.append(MM(pz[:, w - 128:w], IDB[:], NEGM[:], st=False, sp=True))
                    P.op("pe", fns, r=[rQT[s_], rKT[s_], rNEG, rC], w=[prz])
                    P.op("act", ACT(SP[b][:, cs], pz[:, 0:w], AF.Exp, scale=scale), r=[prz], w=[rSP[b]])
                    zs.append((pz, prz, w, cs))
                P.op("act", ACT(SP[b][:, 0:nk], SP[b][:, 0:nk], AF.Ln, bias=SMALL[:, 1:2], scale=1.0), r=[rC], w=[rSP[b]])
                for pz, prz, w, cs in zs:
                    P.op("dve", STT(A_[b][:, cs], pz[:, 0:w], scale, SP[b][:, cs], ALU.mult, ALU.subtract), r=[prz, rSP[b]], w=[rA[b]])

            def stageB(u, c, qb, hp):
                b = u % 2
                nk = (qb + 1) * 128
                e_scan, e_add = ("dve", "pool")
                P.op(e_scan, lambda e, b=b, nk=nk: e.tensor_tensor_scan(
                    out=C_[:, 0:nk], data0=SMALL[:, 1:2].to_broadcast([128, nk]), data1=SP[b][:, 0:nk], initial=0.0,
                    op0=ALU.mult, op1=ALU.add), r=[rSP[b], rC], w=[rCc])
                P.op(e_scan, TS(NT[b], C_[:, nk - 1:nk], -1.0, None, ALU.mult), r=[rCc], w=[rNT[b]])
                P.op(e_add, TT(A_[b][:, 0:nk], A_[b][:, 0:nk], C_[:, 0:nk], ALU.add), r=[rCc], w=[rA[b]])

            def stageC1(u, c, qb, hp):
                b = u % 2
                nk = (qb + 1) * 128
                P.op("act", ACT(W_[b][:, 0:nk], A_[b][:, 0:nk], AF.Exp, bias=NT[b], scale=1.0), r=[rA[b], rNT[b]], w=[rW[b]])

            def stageC(u, c, qb, hp):
                s_ = c % 2
                b = u % 2
                tq = slice(qb * 128, (qb + 1) * 128)
                nk = (qb + 1) * 128
                nb_ = qb + 1
                for g0 in range(0, nb_, 8):
                    n = min(8, nb_ - g0)
                    pt, prt = tbank()
                    ptb = pt[:].bitcast(BF16)
                    P.op("pe", [TR(ptb[:, j * 128:(j + 1) * 128], W_[b][:, (g0 + j) * 128:(g0 + j + 1) * 128], IDB[:]) for j in range(n)],
                         r=[rW[b], rC], w=[prt])
                    dst = WT[b][:, g0:g0 + n, :].rearrange("p a b -> p (a b)")
                    if g0 == 0:
                        P.op("act", ACT(dst, ptb[:, 0:n * 128], AF.Copy), r=[prt], w=[rWT[b]])
                    else:
                        P.op("dve", CP(dst, ptb[:, 0:n * 128]), r=[prt], w=[rWT[b]])
                pcol = slice((qb % 4) * 128, (qb % 4 + 1) * 128)
                P.op("pe", [MM(po[:, pcol], VP[hp][:, jb, :], WT[b][:, jb, :], st=(hp == 0 and jb == 0), sp=(hp == 1 and jb == qb))
                            for jb in range(nb_)], r=[rVP, rWT[b]], w=[pro])
                if hp == 1:
                    P.op("dve", CP(OT[s_][:, tq], po[:, pcol]), r=[pro], w=[rOT[s_]])
                    if qb % 4 == 3:
                        I = qb // 4
                        qsl = slice(I * 512, (I + 1) * 512)
                        for dc in range(KC):
                            ps2, pr2 = tbank()
                            P.op("pe", MM(ps2[:], WO[s_][:, dc * 128:(dc + 1) * 128], OT[s_][:, qsl]), r=[rWO[s_], rOT[s_]], w=[pr2])
                            P.op("dve", STT(XT[:, dc, qsl], ps2[:], modv(layer, 2, dc), XT[:, dc, qsl], ALU.mult, ALU.add),
                                 r=[pr2, rMOD], w=[rXT[I]])

            units = [(c, qb, hp) for c in range(KC) for qb in range(16) for hp in range(2)]
            load_weights(0)
            load_weights(1)
            nu = len(units)
            for i in range(nu + 2):
                if 2 <= i:
                    stageC1(i - 2, *units[i - 2])
                if 1 <= i <= nu:
                    stageB(i - 1, *units[i - 1])
                if i < nu:
                    c, qb, hp = units[i]
                    if qb == 0 and hp == 0 and c == 0:
                        qk_proj(c)
                    stageA(i, c, qb, hp)
                    j = qb * 2 + hp
                    if c + 1 < KC and j % 4 == 3:
                        qk_piece(c + 1, j // 4)
                if 2 <= i:
                    c, qb, hp = units[i - 2]
                    if qb == 0 and hp == 0:
                        v_proj(c)
                    stageC(i - 2, c, qb, hp)
                    if qb == 15 and hp == 1 and c + 2 < KC:
                        load_weights(c + 2)
            P.barrier()

        b7 = [0]

        def psum7():
            i = b7[0]
            b7[0] = (i + 1) % 7
            return banks[i], bres[i]

        mixers = [mixer_conv, mixer_hgrn, mixer_pool, mixer_sb]
        for i in range(DEPTH):
            if ("mix%d" % i) in phases:
                norm_phase(lambda c, i=i: DER[:, i * 16 + c:i * 16 + c + 1], lambda c, i=i: modv(i, 0, c), "mix")
                P.barrier()
                mixers[i](i)
            if ("hdump%d" % i) in phases:
                norm_phase(lambda c, i=i: DER[:, i * 16 + c:i * 16 + c + 1], lambda c, i=i: modv(i, 0, c), "mix")
                P.barrier()
                for tt in range(4):
                    P.op("act", ACT(XT[:, :, tt * 512:(tt + 1) * 512], HT[:, :, tt * 512:(tt + 1) * 512], AF.Copy),
                         r=[rHT[tt]], w=[rXT[tt]])
                P.barrier()
            if ("fdump%d" % i) in phases:
                LOG, rLOG, _ = norm_phase(lambda c, i=i: DER[:, i * 16 + 8 + c:i * 16 + 9 + c], lambda c, i=i: modv(i, 3, c),
                                          "ffn", layer=i)
                P.barrier()
                for tt in range(4):
                    P.op("act", ACT(XT[:, :, tt * 512:(tt + 1) * 512], HT[:, :, tt * 512:(tt + 1) * 512], AF.Copy),
                         r=[rHT[tt]], w=[rXT[tt]])
                P.op("act", ACT(XT[:, 0, 0:576], LOG[:].rearrange("p a b -> p (a b)"), AF.Copy), r=[rLOG], w=[rXT[0]])
                P.barrier()
            if ("ffn%d" % i) in phases:
                LOG, rLOG, _ = norm_phase(lambda c, i=i: DER[:, i * 16 + 8 + c:i * 16 + 9 + c], lambda c, i=i: modv(i, 3, c),
                                          "ffn", layer=i)
                moe_phase(i, LOG, rLOG)
        if dbg:
            rOUT = P.res("OUT")
            for i in range(4):
                P.dma("sp", out_d[:, :, i * 512:(i + 1) * 512], XT[:, :, i * 512:(i + 1) * 512], r=[rXT[i]], w=[], dres=rOUT)
        else:
            norm_phase(lambda c: V("fin", c), None, "final")
        P.barrier()
        block = es.enter_context(nc.Block())
        P.replay(block)
    return nc


ALL_PHASES = ["mods"] + [p for i in range(DEPTH) for p in ("mix%d" % i, "ffn%d" % i)]


def _consts():
    ident = np.eye(128, dtype=np.float32)
    j = np.arange(128)[:, None]
    s = np.arange(128)[None, :]
    tri = (j > s).astype(np.float32)
    mask2 = ((j <= s) & ((j // 64) == (s // 64))).astype(np.float32)
    cA = np.concatenate([ident, tri, mask2], axis=1)
    t = np.arange(512)[None, :]
    sbm = np.concatenate([((r * 128 + j) < t).astype(np.float32) for r in range(4)], axis=1)
    scm = np.broadcast_to((np.arange(S) % 64 != 0).astype(np.float32)[None, :], (128, S)).copy()
    invc = np.zeros((128, 64), np.float32)
    for gi, w in enumerate((2, 4, 8, 16)):
        invc[:, gi * 16:(gi + 1) * 16] = 1.0 / np.minimum(np.arange(16) + 1, w)
    cB = np.zeros((128, NCB), np.float32)
    cB[:, 0:16] = 256.0 * np.arange(16)[None, :]
    cB[:, 16:80] = np.arange(64)[None, :]
    cB[:, 80] = np.arange(128)
    return cA, sbm, scm, invc, cB


_CACHE = {}


def kernel(**inp):
    return run(inp, ALL_PHASES, False)


def run(inp, phases, dbg, cores=8):
    f = lambda a: np.ascontiguousarray(np.asarray(a, np.float32))
    key = (tuple(phases), dbg)
    if key not in _CACHE:
        _CACHE[key] = build_program(phases, dbg)
    nc = _CACHE[key]
    cA, sbm, scm, invc, cB = _consts()
    x = f(inp["x"])
    shared = {
        "cA": cA, "sbmask": sbm, "scanmask": scm, "invc": invc, "cB": cB,
        "ada_w": f(inp["ada_w"]),
        "conv_w_in": f(inp["conv_w_in"][0]), "conv_w_out": f(inp["conv_w_out"][0]),
        "hgrn_w_in": f(inp["hgrn_w_in"][0]), "hgrn_w_out": f(inp["hgrn_w_out"][0]),
        "pool_w": f(inp["pool_w"][0]),
        "sb_w_qkv": f(inp["sb_w_qkv"][0]), "sb_w_out": f(inp["sb_w_out"][0]),
    }
    shared["wgL"] = np.ascontiguousarray(
        f(inp["moe_w_gate"]).reshape(DEPTH, NEXP, KC, 128, FH).transpose(0, 1, 3, 2, 4)).reshape(DEPTH * NEXP * 128, KC * FH)
    shared["wuL"] = np.ascontiguousarray(
        f(inp["moe_w_up"]).reshape(DEPTH, NEXP, KC, 128, FH).transpose(0, 1, 3, 2, 4)).reshape(DEPTH * NEXP * 128, KC * FH)
    shared["wdL"] = np.ascontiguousarray(
        f(inp["moe_w_down"]).reshape(DEPTH, NEXP, 4, 128, D).transpose(0, 1, 3, 2, 4)).reshape(DEPTH * NEXP * 128, 4 * D)
    wr = np.concatenate([f(inp["moe_w_rg"]), f(inp["moe_w_re"])], axis=2)
    shared["wr"] = np.ascontiguousarray(wr.reshape(DEPTH, KC, 128, 36).transpose(2, 0, 1, 3))
    br = np.concatenate([f(inp["moe_b_rg"]), f(inp["moe_b_re"])], axis=1).reshape(1, DEPTH * 36)
    shared["br"] = np.ascontiguousarray(np.broadcast_to(br, (128, DEPTH * 36)))
    in_maps = []
    for b in range(cores):
        vec = np.zeros((128, NVEC), np.float32)

        def put(name, arr):
            a = _fm(arr)
            vec[:, VOFF[name]:VOFF[name] + a.shape[1]] = a
        for i in range(DEPTH):
            put("gmix%d" % i, inp["norm_mix_g"][i])
            put("gffn%d" % i, inp["norm_ffn_g"][i])
            put("adab%d" % i, inp["ada_b"][i])
            put("lbl%d" % i, inp["hgrn_lb_logits"][i])
        put("fin", inp["final_g"])
        put("cbin", inp["conv_b_in"][0])
        put("cbdw", inp["conv_b_dw"][0])
        put("clng", inp["conv_ln_g"][0])
        put("clnb", inp["conv_ln_b"][0])
        put("hng", inp["hgrn_norm_g"][0])
        put("pb", inp["pool_b"][0])
        put("psc", inp["pool_scale"][0])
        wdw = f(inp["conv_w_dw"][0])
        vec[:, VOFF["wdw"]:VOFF["wdw"] + 248] = wdw.reshape(31, KC, 128).transpose(2, 1, 0).reshape(128, 248)
        put("c", inp["c"][b])
        m = dict(shared)
        m["vecs"] = vec
        m["xT"] = np.ascontiguousarray(x[b].T.reshape(KC, 128, S).transpose(1, 0, 2))
        in_maps.append(m)
    res = run_bass_kernel_spmd(nc, in_maps, core_ids=list(range(cores)))
    outs = []
    for b in range(cores):
        oT = res.results[b]["outT"]
        outs.append(np.ascontiguousarray(oT.transpose(1, 0, 2).reshape(D, S).T))
    return np.stack(outs, axis=0).astype(np.float32)
```

```python
import numpy as np
from contextlib import ExitStack
import concourse.bass as bass
import concourse.mybir as mybir
from concourse.bass_utils import run_bass_kernel_spmd

F32 = mybir.dt.float32
BF16 = mybir.dt.bfloat16
AF = mybir.ActivationFunctionType
ALU = mybir.AluOpType
AX = mybir.AxisListType

D = 1024
S = 2048
KC = 8
DEPTH = 4
EPS = 1e-6
NEXP = 32
FH = 512
L0 = 98304
M0 = L0 + 24576
ARENA_BYTES = 204800
BIG = 1.0e30
NSTEP = 48
NSUB = 2 * NSTEP
NSLOT = NSTEP * 256
NCB = 84
I32 = mybir.dt.int32

ENGS = ("pe", "act", "dve", "pool", "sp")


class Slot:
    __slots__ = ("sem", "cnt", "eng")

    def __init__(self, sem):
        self.sem = sem
        self.cnt = 0
        self.eng = None


class Res:
    __slots__ = ("name", "w", "r", "slot")

    def __init__(self, name):
        self.name = name
        self.w = None
        self.r = {}
        self.slot = None


class Prog:
    def __init__(self, nc, es):
        self.nc = nc
        self.es = es
        self.q = {e: [] for e in ENGS}
        self.cnt = {e: 0 for e in ENGS}
        self.seen = {e: {} for e in ENGS}
        self.esem = {e: es.enter_context(nc.semaphore("sem_" + e)) for e in ("pe", "act", "dve", "pool")}
        self.slots = []
        self.free = {}
        self.live = []
        self.regs = {}

    def res(self, name):
        return Res(name)

    def _sem_of(self, key):
        return self.esem[key] if isinstance(key, str) else key.sem

    def _wait(self, e, toks, strict=False):
        need = {}
        for t in toks:
            if t is None:
                continue
            k, v = t
            if (not strict) and k == e and e == "pe":
                continue
            if need.get(k, 0) < v:
                need[k] = v
        for k, v in need.items():
            if self.seen[e].get(k, 0) >= v:
                continue
            self.seen[e][k] = v
            sem = self._sem_of(k)
            self.q[e].append(lambda eng, sem=sem, v=v: eng.wait_ge(sem, v))

    def _collect(self, r, w):
        toks = []
        for x in r:
            toks.append(x.w)
        for x in w:
            toks.append(x.w)
            toks.extend(x.r.items())
        return toks

    def op(self, e, fns, r=(), w=()):
        if callable(fns):
            fns = [fns]
        self._wait(e, self._collect(r, w))
        self.cnt[e] += 1
        n = self.cnt[e]
        sem = self.esem[e]
        last = len(fns) - 1
        for i, fn in enumerate(fns):
            if i == last:
                self.q[e].append(lambda eng, fn=fn, sem=sem: fn(eng).then_inc(sem, 1))
            else:
                self.q[e].append(fn)
        tok = (e, n)
        for x in r:
            if x.r.get(e, 0) < n:
                x.r[e] = n
        for x in w:
            x.w = tok
            x.r = {}
        return tok

    def dma(self, e, out, in_, r=(), w=(), dres=None, nowait_w=()):
        return self.dmaf(e, lambda eng, out=out, in_=in_: eng.dma_start(out=out, in_=in_), r, w, dres, nowait_w)

    def dmaf(self, e, fn, r=(), w=(), dres=None, nowait_w=()):
        if dres is None:
            dres = w[0]
        if dres.slot is None:
            fl = self.free.setdefault(e, [])
            if fl:
                dres.slot = fl.pop()
            else:
                dres.slot = Slot(self.es.enter_context(self.nc.semaphore("dsem_%s_%d" % (e, len(self.slots)))))
                dres.slot.eng = e
                self.slots.append(dres.slot)
            self.live.append(dres)
        slot = dres.slot
        assert slot.eng == e, (dres.name, slot.eng, e)
        self._wait(e, self._collect(r, w), strict=True)
        slot.cnt += 16
        v = slot.cnt
        sem = slot.sem
        self.q[e].append(lambda eng, fn=fn, sem=sem: fn(eng).then_inc(sem, 16))
        tok = (slot, v)
        for x in r:
            x.r[slot] = v
        for x in w:
            x.w = tok
            x.r = {}
        for x in nowait_w:
            x.w = tok
        return tok

    def barrier(self):
        toks = [(e, self.cnt[e]) for e in ("pe", "act", "dve", "pool") if self.cnt[e] > 0]
        toks += [(d, d.cnt) for d in self.slots if d.cnt > 0]
        for e in ENGS:
            self._wait(e, toks)
        for d in self.live:
            self.free.setdefault(d.slot.eng, []).append(d.slot)
            d.slot = None
        self.live = []

    def replay(self, block):
        def mk(name):
            def f(eng):
                for fn in self.q[name]:
                    fn(eng)
            return f
        block.tensor(mk("pe"))
        block.scalar(mk("act"))
        block.vector(mk("dve"))
        block.gpsimd(mk("pool"))
        block.sync(mk("sp"))


def MM(o, l, r, st=True, sp=True):
    return lambda e: e.matmul(o, lhsT=l, rhs=r, start=st, stop=sp)


def TR(o, i, ident):
    return lambda e: e.transpose(o, i, ident)


def ACT(o, i, f, bias=None, scale=None):
    kw = {}
    if bias is not None:
        kw["bias"] = bias
    if scale is not None:
        kw["scale"] = scale
    return lambda e: e.activation(out=o, in_=i, func=f, **kw)


def TS(o, i, s1, s2, op0, op1=None):
    if op1 is None:
        return lambda e: e.tensor_scalar(out=o, in0=i, scalar1=s1, scalar2=None, op0=op0)
    return lambda e: e.tensor_scalar(out=o, in0=i, scalar1=s1, scalar2=s2, op0=op0, op1=op1)


def TT(o, a, b, op):
    return lambda e: e.tensor_tensor(out=o, in0=a, in1=b, op=op)


def STT(o, a, s, b, op0, op1):
    return lambda e: e.scalar_tensor_tensor(out=o, in0=a, scalar=s, in1=b, op0=op0, op1=op1)


def CP(o, i):
    return lambda e: e.tensor_copy(out=o, in_=i)


def MS(o, v):
    return lambda e: e.memset(o, v)


def _vec_layout():
    off = {}
    n = 0

    def add(name, cols):
        nonlocal n
        off[name] = n
        n += cols
    for i in range(DEPTH):
        add("gmix%d" % i, 8)
        add("gffn%d" % i, 8)
        add("adab%d" % i, 48)
        add("lbl%d" % i, 8)
    add("fin", 8)
    add("cbin", 16)
    add("cbdw", 8)
    add("clng", 8)
    add("clnb", 8)
    add("hng", 8)
    add("pb", 8)
    add("psc", 8)
    add("wdw", 248)
    add("c", 8)
    return off, n


VOFF, NVEC = _vec_layout()


def _fm(v):
    v = np.asarray(v, np.float32).reshape(-1, 128)
    return np.ascontiguousarray(v.T)


def build_program(phases, dbg=False):
    nc = bass.Bass("TRN2", target_bir_lowering=False)
    dt_ = nc.dram_tensor
    xT_d = dt_("xT", [128, KC, S], F32, kind="ExternalInput").ap()
    vec_d = dt_("vecs", [128, NVEC], F32, kind="ExternalInput").ap()
    br_d = dt_("br", [128, DEPTH * 36], F32, kind="ExternalInput").ap()
    wr_d = dt_("wr", [128, DEPTH, KC, 36], F32, kind="ExternalInput").ap()
    cA_d = dt_("cA", [128, 384], F32, kind="ExternalInput").ap()
    sbm_d = dt_("sbmask", [128, 2048], F32, kind="ExternalInput").ap()
    scm_d = dt_("scanmask", [128, 2048], F32, kind="ExternalInput").ap()
    invc_d = dt_("invc", [128, 64], F32, kind="ExternalInput").ap()
    cB_d = dt_("cB", [128, NCB], F32, kind="ExternalInput").ap()
    adaw_d = dt_("ada_w", [DEPTH, D, 6 * D], F32, kind="ExternalInput").ap()
    cwin_d = dt_("conv_w_in", [D, 2 * D], F32, kind="ExternalInput").ap()
    cwout_d = dt_("conv_w_out", [D, D], F32, kind="ExternalInput").ap()
    hwin_d = dt_("hgrn_w_in", [D, 4 * D], F32, kind="ExternalInput").ap()
    hwout_d = dt_("hgrn_w_out", [D, D], F32, kind="ExternalInput").ap()
    pw_d = dt_("pool_w", [4, 256, 256], F32, kind="ExternalInput").ap()
    sqkv_d = dt_("sb_w_qkv", [D, 3 * D], F32, kind="ExternalInput").ap()
    swout_d = dt_("sb_w_out", [D, D], F32, kind="ExternalInput").ap()
    wgL_d = dt_("wgL", [DEPTH * NEXP * 128, 4096], F32, kind="ExternalInput").ap()
    wuL_d = dt_("wuL", [DEPTH * NEXP * 128, 4096], F32, kind="ExternalInput").ap()
    wdL_d = dt_("wdL", [DEPTH * NEXP * 128, 4096], F32, kind="ExternalInput").ap()
    xs_d = dt_("xs_scr", [NSLOT, D], BF16, kind="Internal").ap()
    ys_d = dt_("ys_scr", [NSLOT, D], BF16, kind="Internal").ap()
    out_d = dt_("outT", [128, KC, S], F32, kind="ExternalOutput").ap()

    es = ExitStack()
    with es:
        arena = es.enter_context(nc.sbuf_tensor("arena", [128, ARENA_BYTES // 2], BF16))

        def carve(off, n, dt=BF16, parts=128):
            if dt == BF16:
                a = arena[0:parts, off // 2: off // 2 + n]
            else:
                a = arena[0:parts, off // 2: off // 2 + 2 * n].bitcast(F32)
            return a

        def v3(ap, a):
            return ap.rearrange("p (a b) -> p a b", a=a)

        sb = lambda name, shape, dt: es.enter_context(nc.sbuf_tensor(name, shape, dt))
        VEC = sb("VEC", [128, NVEC], F32)
        BRB = sb("BRB", [128, DEPTH * 36], F32)
        MOD = sb("MOD", [128, DEPTH * 48], F32)
        DER = sb("DER", [128, DEPTH * 16 + 32], F32)
        CST = sb("CST", [128, 384], F32)
        IDB = sb("IDB", [128, 128], BF16)
        ONESB = sb("ONESB", [128, 128], BF16)
        TRIB = sb("TRIB", [128, 128], BF16)
        CA = sb("CA", [128, 8], F32)
        SMALL = sb("SMALL", [128, 64], F32)
        CSTB = sb("CSTB", [128, NCB], F32)
        DIDX = sb("DIDX", [128, 32], I32)
        WIDX = sb("WIDX", [128, NSTEP], I32)
        GATE = sb("GATE", [128, 32], F32)
        NEGM = sb("NEGM", [128, 128], BF16)
        TH16 = CSTB[:, 0:16]
        BIDX = CSTB[:, 16:80]
        PIDX = CSTB[:, 80:81]
        banks = [es.enter_context(nc.psum_tensor("bank%d" % i, [128, 512], F32)) for i in range(8)]
        IDF = CST[:, 0:128]
        MASK2 = CST[:, 256:384]

        P = Prog(nc, es)
        bres = [P.res("bank%d" % i) for i in range(8)]
        bptr = [0]

        def psum():
            i = bptr[0]
            bptr[0] = (i + 1) % 8
            return banks[i], bres[i]

        XT = v3(carve(0, KC * S, F32), KC)
        HT = v3(carve(65536, KC * S, BF16), KC)
        rXT = [P.res("XT%d" % i) for i in range(4)]
        rHT = [P.res("HT%d" % i) for i in range(4)]
        rC = P.res("consts")
        rVEC = P.res("VEC")
        rMOD = P.res("MOD")

        def V(name, c0=0, n=1):
            o = VOFF[name] + c0
            return VEC[:, o:o + n]

        P.dma("sp", VEC[:], vec_d, w=[rVEC])
        P.dma("sp", BRB[:], br_d, w=[rC])
        P.dma("sp", CST[:], cA_d, w=[rC])
        P.dma("sp", CSTB[:], cB_d, w=[rC])

        def _mk_regs(eng):
            P.regs["wb"] = eng.alloc_register("wbound")
            eng.reg_mov(P.regs["wb"], DEPTH * NEXP * 128 - 1)
            P.regs["xb"] = eng.alloc_register("xbound")
            eng.reg_mov(P.regs["xb"], NSLOT - 1)
        P.q["pool"].append(_mk_regs)
        for i in range(4):
            P.dma("sp", XT[:, :, i * 512:(i + 1) * 512], xT_d[:, :, i * 512:(i + 1) * 512], w=[rXT[i]])
        HTflat = carve(65536, KC * S, BF16)
        rZ = P.res("Z")
        P.op("pool", MS(HTflat, 0.0), w=rHT)
        for j in range(NSLOT // 2048):
            P.dma("sp", xs_d[j * 2048:(j + 1) * 2048, :].rearrange("(p a) n -> p (a n)", p=128), HTflat,
                  r=rHT, w=[], dres=rZ, nowait_w=[rZ])
        P.op("dve", CP(IDB[:], CST[:, 0:128]), r=[rC], w=[rC])
        P.op("dve", CP(TRIB[:], CST[:, 128:256]), r=[rC], w=[rC])
        P.op("dve", MS(ONESB[:], 1.0), w=[rC])
        P.op("act", ACT(CA[:], V("c", 0, 8), AF.Silu), r=[rVEC], w=[rC])

        NAS = 4
        adaw_slots = [v3(carve(M0 + s_ * 16384, KC * 512, F32), KC) for s_ in range(NAS)]
        r_adaw = [P.res("adaw%d" % s_) for s_ in range(NAS)]
        ROW = carve(L0, 6 * D, F32, parts=1)
        CAb = CA
        rROW = P.res("ROW")
        P.op("dve", MS(SMALL[:, 0:1], EPS), w=[rC])
        P.op("dve", MS(SMALL[:, 1:2], 1.0), w=[rC])
        if "mods" in phases:
            for i in range(DEPTH):
                for blk in range(12):
                    sl = (i * 12 + blk) % NAS
                    P.dma("sp", adaw_slots[sl][:],
                          adaw_d[i, :, blk * 512:(blk + 1) * 512].rearrange("(k p) n -> p k n", p=128),
                          w=[r_adaw[sl]])
                    ps, pr = psum()
                    P.op("pe", [MM(ps[0:1, :], CAb[:, k:k + 1], adaw_slots[sl][:, k, :], st=(k == 0), sp=(k == KC - 1))
                                for k in range(KC)], r=[r_adaw[sl], rC], w=[pr])
                    P.op("act", ACT(ROW[0:1, blk * 512:(blk + 1) * 512], ps[0:1, :], AF.Copy), r=[pr], w=[rROW])
                ps, pr = psum()
                P.op("pe", [MM(ps[:, j:j + 1], ROW[0:1, j * 128:(j + 1) * 128], SMALL[0:1, 1:2]) for j in range(48)],
                     r=[rROW, rC], w=[pr])
                P.op("dve", TT(MOD[:, i * 48:(i + 1) * 48], ps[:, 0:48], V("adab%d" % i, 0, 48), ALU.add),
                     r=[pr, rVEC], w=[rMOD])
                P.op("dve", STT(DER[:, i * 16:i * 16 + 8], MOD[:, i * 48 + 8:i * 48 + 16], 1.0, V("gmix%d" % i, 0, 8),
                                ALU.add, ALU.mult), r=[rMOD, rVEC], w=[rMOD])
                P.op("dve", STT(DER[:, i * 16 + 8:i * 16 + 16], MOD[:, i * 48 + 32:i * 48 + 40], 1.0,
                                V("gffn%d" % i, 0, 8), ALU.add, ALU.mult), r=[rMOD, rVEC], w=[rMOD])
        P.barrier()

        def modv(i, which, c):
            o = i * 48 + which * 8 + c
            return MOD[:, o:o + 1]

        def norm_phase(A_of, B_of, out_mode, layer=0):
            base = M0
            SQ = v3(carve(base, KC * 256, BF16), KC)
            TMPs = [v3(carve(base + 4096 + s_ * 8192, KC * 256, F32), KC) for s_ in range(2)]
            RSTDs = [carve(base + 4096 + 16384 + s_ * 1024, 256, F32) for s_ in range(2)]
            rSQ = P.res("SQ")
            rTMP = [P.res("TMP0"), P.res("TMP1")]
            rRS = [P.res("RS0"), P.res("RS1")]
            rLOG = P.res("LOG")
            LOG = None
            if out_mode == "ffn":
                LOG = v3(carve(base + 24576, 16 * 36, F32), 16)
                WR = v3(carve(base + 24576 + 4096, KC * 36, F32), KC)
                rWR = P.res("WR")
                P.dma("sp", WR[:], wr_d[:, layer, :, :], w=[rWR])
            rOUT = [P.res("OUT0"), P.res("OUT1")]
            def n1a(tt):
                sl = slice(tt * 256, (tt + 1) * 256)
                xt_r = rXT[tt // 2]
                P.op("act", ACT(SQ[:], XT[:, :, sl], AF.Square), r=[xt_r], w=[rSQ])
                ps, pr = psum()
                P.op("pe", [MM(ps[:, 0:256], ONESB[:], SQ[:, c, :], st=(c == 0), sp=(c == KC - 1)) for c in range(KC)],
                     r=[rSQ, rC], w=[pr])
                return ps, pr

            def n1b(tt, ps, pr):
                sl = slice(tt * 256, (tt + 1) * 256)
                xt_r = rXT[tt // 2]
                s_ = tt % 2
                P.op("act", ACT(RSTDs[s_], ps[:, 0:256], AF.Sqrt, bias=SMALL[:, 0:1], scale=1.0 / D), r=[pr, rC], w=[rRS[s_]])
                P.op("dve", lambda e, o=RSTDs[s_]: e.reciprocal(out=o, in_=o), r=[rRS[s_]], w=[rRS[s_]])
                for c in range(KC):
                    P.op("dve", STT(TMPs[s_][:, c, :], XT[:, c, sl], A_of(c), RSTDs[s_], ALU.mult, ALU.mult),
                         r=[xt_r, rRS[s_], rMOD], w=[rTMP[s_]])

            def n2(tt):
                sl = slice(tt * 256, (tt + 1) * 256)
                s_ = tt % 2
                if out_mode == "mix":
                    for c in range(KC):
                        P.op("act", ACT(HT[:, c, sl], TMPs[s_][:, c, :], AF.Identity, bias=B_of(c), scale=1.0),
                             r=[rTMP[s_], rMOD], w=[rHT[tt // 2]])
                elif out_mode == "ffn":
                    for c in range(KC):
                        P.op("act", ACT(TMPs[s_][:, c, :], TMPs[s_][:, c, :], AF.Identity, bias=B_of(c), scale=1.0),
                             r=[rMOD], w=[rTMP[s_]])
                    P.op("dve", CP(HT[:, :, sl], TMPs[s_][:]), r=[rTMP[s_]], w=[rHT[tt // 2]])
                    for sub in range(2):
                        ps2, pr2 = psum()
                        P.op("pe", [MM(ps2[:, 0:36], TMPs[s_][:, k, sub * 128:(sub + 1) * 128], WR[:, k, :],
                                       st=(k == 0), sp=(k == KC - 1)) for k in range(KC)],
                             r=[rTMP[s_], rWR], w=[pr2])
                        P.op("dve", TT(LOG[:, tt * 2 + sub, :], ps2[:, 0:36], BRB[:, layer * 36:(layer + 1) * 36], ALU.add),
                             r=[pr2, rC], w=[rLOG])
                else:
                    P.dma("sp", out_d[:, :, sl], TMPs[s_][:], r=[rTMP[s_]], w=[], dres=rOUT[s_])

            pend = n1a(0)
            n1b(0, *pend)
            for tt in range(8):
                if tt + 1 < 8:
                    pend = n1a(tt + 1)
                n2(tt)
                if tt + 1 < 8:
                    n1b(tt + 1, *pend)
            return LOG, rLOG, rOUT

        def moe_phase(layer, LOG, rLOG):
            base = M0
            rb = base + 32768
            nR = [0]

            def rt(n):
                a = carve(rb + nR[0], n, F32)
                nR[0] += n * 4
                return a
            rR = P.res("route")
            GL = LOG[:, :, 0:4]
            EL = LOG[:, :, 4:36]
            GMAX = rt(16)
            GSH = v3(rt(64), 16)
            OHG = v3(rt(64), 16)
            GE = v3(rt(64), 16)
            GSUM = rt(16)
            GP = rt(16)
            PEN = v3(rt(64), 16)
            ELM = v3(rt(512), 16)
            V1 = rt(16)
            D1 = v3(rt(512), 16)
            OH1 = v3(rt(512), 16)
            ELM2 = v3(rt(512), 16)
            V2 = rt(16)
            OH2 = v3(rt(512), 16)
            DV = rt(16)
            E21 = rt(16)
            P1 = rt(16)
            C1 = rt(16)
            C2 = rt(16)

            def bc3(a, n):
                return a.unsqueeze(2).to_broadcast([128, 16, n])

            def dv(fn, r=(), w=()):
                P.op("dve", fn, r=list(r) + [rLOG, rR], w=list(w) + [rR])
            dv(lambda e: e.tensor_reduce(out=GMAX, in_=GL, axis=AX.X, op=ALU.max))
            dv(TT(GSH[:], GL, bc3(GMAX, 4), ALU.subtract))
            dv(TS(OHG[:], GSH[:], 0.0, None, ALU.is_equal))
            P.op("act", ACT(GE[:], GSH[:], AF.Exp), r=[rR], w=[rR])
            dv(lambda e: e.tensor_reduce(out=GSUM, in_=GE[:], axis=AX.X, op=ALU.add))
            dv(lambda e: e.reciprocal(out=GP, in_=GSUM))
            dv(TS(PEN[:], OHG[:], -1.0, BIG, ALU.add, ALU.mult))
            ELM4 = ELM[:].rearrange("p a (g x) -> p a g x", g=4)
            EL4 = EL.rearrange("p a (g x) -> p a g x", g=4)
            dv(TT(ELM4, EL4, PEN[:].unsqueeze(3).to_broadcast([128, 16, 4, 8]), ALU.add))
            dv(lambda e: e.tensor_reduce(out=V1, in_=ELM[:], axis=AX.X, op=ALU.max))
            dv(TT(D1[:], ELM[:], bc3(V1, 32), ALU.subtract))
            dv(TS(OH1[:], D1[:], 0.0, None, ALU.is_equal))
            dv(STT(ELM2[:], OH1[:], -BIG, ELM[:], ALU.mult, ALU.add))
            dv(lambda e: e.tensor_reduce(out=V2, in_=ELM2[:], axis=AX.X, op=ALU.max))
            dv(TT(D1[:], ELM2[:], bc3(V2, 32), ALU.subtract))
            dv(TS(OH2[:], D1[:], 0.0, None, ALU.is_equal))
            dv(TT(DV, V2, V1, ALU.subtract))
            P.op("act", ACT(E21, DV, AF.Exp), r=[rR], w=[rR])
            dv(TS(P1, E21, 1.0, None, ALU.add))
            dv(lambda e: e.reciprocal(out=P1, in_=P1))
            dv(TT(C1, P1, GP, ALU.mult))
            dv(TT(C2, E21, C1, ALU.mult))
            rIDX = P.res("IDX")
            MBo = rb + nR[0]
            nR[0] += 1024
            MB = v3(carve(MBo, 512, BF16), 16)
            CNT = rt(32)
            CMP3 = v3(rt(512), 32)
            NBK = rt(32)
            T3 = v3(rt(1024), 32)
            LE3 = v3(rt(1024), 32)
            PEND = rt(32)
            PST = rt(32)
            DA = v3(rt(512), 16)
            DF = rt(32)
            CMPB3 = v3(rt(NSTEP * 32), NSTEP)
            EBf = rt(NSTEP)
            INV = rt(NSTEP)
            WF = rt(NSTEP)
            BST = BIDX[:, 0:NSTEP]
            dv(TT(MB[:], OH1[:], OH2[:], ALU.add))
            psR, prR = psum()
            fns = []
            for t in range(16):
                fns.append(MM(psR[:, t * 32:(t + 1) * 32], TRIB[:], MB[:, t, :], st=True, sp=(t == 15)))
                for t2 in range(t + 1, 16):
                    fns.append(MM(psR[:, t * 32:(t + 1) * 32], ONESB[:], MB[:, t2, :], st=False, sp=(t2 == 15)))
            P.op("pe", fns, r=[rR, rC], w=[prR])
            psC, prC = psum()
            P.op("pe", [MM(psC[:, 0:32], ONESB[:], MB[:, t, :], st=(t == 0), sp=(t == 15)) for t in range(16)],
                 r=[rR, rC], w=[prC])
            dv(CP(CNT, psC[:, 0:32]), r=[prC])
            dv(TT(CMP3[:], CNT.unsqueeze(2).to_broadcast([128, 32, 16]), TH16.unsqueeze(1).to_broadcast([128, 32, 16]),
                  ALU.is_gt), r=[rC])
            dv(lambda e: e.tensor_reduce(out=NBK, in_=CMP3[:], axis=AX.X, op=ALU.add))
            IO32 = BIDX[:, 0:32]
            dv(TT(LE3[:], IO32.unsqueeze(1).to_broadcast([128, 32, 32]), IO32.unsqueeze(2).to_broadcast([128, 32, 32]),
                  ALU.is_le), r=[rC])
            dv(TT(T3[:], LE3[:], NBK.unsqueeze(1).to_broadcast([128, 32, 32]), ALU.mult))
            dv(lambda e: e.tensor_reduce(out=PEND, in_=T3[:], axis=AX.X, op=ALU.add))
            dv(TT(PST, PEND, NBK, ALU.subtract))
            dv(TS(PST, PST, 256.0, None, ALU.mult))
            dv(TT(DA[:], v3(psR[:], 16), PST.unsqueeze(1).to_broadcast([128, 16, 32]), ALU.add), r=[prR])
            dv(TT(D1[:], OH1[:], DA[:], ALU.mult))
            dv(lambda e: e.tensor_reduce(out=DF[:, 0:16], in_=D1[:], axis=AX.X, op=ALU.add))
            dv(TT(D1[:], OH2[:], DA[:], ALU.mult))
            dv(lambda e: e.tensor_reduce(out=DF[:, 16:32], in_=D1[:], axis=AX.X, op=ALU.add))
            dv(CP(DIDX[:], DF), w=[rIDX])
            dv(CP(GATE[:, 0:16], C1), w=[rIDX])
            dv(CP(GATE[:, 16:32], C2), w=[rIDX])
            dv(TT(CMPB3[:], PEND.unsqueeze(1).to_broadcast([128, NSTEP, 32]), BST.unsqueeze(2).to_broadcast([128, NSTEP, 32]),
                  ALU.is_le), r=[rC])
            dv(lambda e: e.tensor_reduce(out=EBf, in_=CMPB3[:], axis=AX.X, op=ALU.add))
            dv(TS(INV, BST, PEND[:, 31:32], None, ALU.is_ge), r=[rC])
            dv(TS(EBf, EBf, 31.0, None, ALU.min))
            dv(TS(WF, EBf, float(layer * NEXP), 128.0, ALU.add, ALU.mult))
            dv(TS(WF, WF, PIDX, None, ALU.add), r=[rC])
            dv(STT(WF, INV, 1.0e6, WF, ALU.mult, ALU.add))
            dv(CP(WIDX[:], WF), w=[rIDX])
            P.barrier()

            IOA = bass.IndirectOffsetOnAxis
            Wslots = [L0, M0]

            def wviews(slot):
                o = Wslots[slot]
                return carve(o, 4096, BF16), carve(o + 8192, 4096, BF16), carve(o + 16384, 4096, BF16)
            rWGU = [P.res("WGU0"), P.res("WGU1")]
            rWD = [P.res("WD0"), P.res("WD1")]

            perm = []
            for s_i in range(NSTEP // 2):
                perm += [s_i, NSTEP - 1 - s_i]

            def bid(n):
                return 2 * perm[n // 2] + (n % 2)

            def wgather(dst, src, pos):
                col = perm[pos]
                return lambda eng: eng.indirect_dma_start(out=dst, out_offset=None, in_=src,
                                                          in_offset=IOA(ap=WIDX[:, col:col + 1], axis=0),
                                                          bounds_check=P.regs["wb"], oob_is_err=False)

            def load_wgu(st):
                wg, wu, _ = wviews(st % 2)
                P.dmaf("pool", wgather(wg, wgL_d, st), r=[rIDX], w=[rWGU[st % 2]])
                P.dmaf("pool", wgather(wu, wuL_d, st), r=[rIDX], w=[rWGU[st % 2]])

            def load_wd(st):
                _, _, wdn = wviews(st % 2)
                P.dmaf("pool", wgather(wdn, wdL_d, st), r=[rIDX], w=[rWD[st % 2]])

            eb = M0 + 24576
            HTOK = [carve(eb + s_ * 2048, 1024, BF16) for s_ in range(2)]
            NXS = 4
            XSL = [carve(eb + 49152 + s_ * 2048, 1024, BF16) for s_ in range(NXS)]
            XF = [carve(eb + 8192 + s_ * 2048, 1024, BF16) for s_ in range(2)]
            SS = [carve(eb + 12288 + s_ * 2048, 512, F32) for s_ in range(2)]
            HIDS = [carve(eb + 16384 + s_ * 1024, 512, BF16) for s_ in range(2)]
            HIDT = [carve(eb + 18432 + s_ * 1024, 512, BF16) for s_ in range(2)]
            YS = [carve(eb + 20480 + s_ * 2048, 1024, BF16) for s_ in range(2)]
            NYG = 4
            YG = [[carve(eb + 24576 + (k * NYG + s_) * 2048, 1024, BF16) for s_ in range(NYG)] for k in range(2)]
            YC = [carve(eb + 40960 + s_ * 4096, 1024, F32) for s_ in range(2)]
            two = lambda n: [P.res(n + "0"), P.res(n + "1")]
            rHTOK, rXF, rSS, rHIDS, rHIDT, rYS = two("HTOK"), two("XF"), two("SS"), two("HIDS"), two("HIDT"), two("YS")
            rXSL = [P.res("XSL%d" % i) for i in range(NXS)]
            rXs, rYs = two("Xs"), two("Ys")
            rYG = [[P.res("YG%d_%d" % (k, i)) for i in range(NYG)] for k in range(2)]
            rYC = two("YC")

            load_wgu(0)
            load_wd(0)
            load_wgu(1)
            load_wd(1)
            for t16 in range(16):
                b = t16 % 2
                ps, pr = psum()
                pb_ = ps[:].bitcast(BF16)
                P.op("pe", [TR(pb_[:, k * 128:(k + 1) * 128], HT[:, k, t16 * 128:(t16 + 1) * 128], IDB[:]) for k in range(KC)],
                     r=[rHT[t16 // 4], rC], w=[pr])
                if b == 0:
                    P.op("act", ACT(HTOK[b], pb_[:, 0:1024], AF.Copy), r=[pr], w=[rHTOK[b]])
                else:
                    P.op("dve", CP(HTOK[b], pb_[:, 0:1024]), r=[pr], w=[rHTOK[b]])
                for k in range(2):
                    c = k * 16 + t16
                    P.dmaf("pool", lambda eng, b=b, c=c: eng.indirect_dma_start(
                        out=xs_d, out_offset=IOA(ap=DIDX[:, c:c + 1], axis=0), in_=HTOK[b], in_offset=None,
                        bounds_check=P.regs["xb"], oob_is_err=False),
                        r=[rHTOK[b], rIDX], w=[], dres=rXs[b], nowait_w=[rXs[b]])

            def xload(bi):
                x4 = bi % NXS
                P.dma("sp", XSL[x4], xs_d[bid(bi) * 128:(bid(bi) + 1) * 128, :], r=[rXs[0], rXs[1]], w=[rXSL[x4]])

            def stage_Tx(bi):
                s2 = bi % 2
                x4 = bi % NXS
                ps, pr = psum()
                pb_ = ps[:].bitcast(BF16)
                P.op("pe", [TR(pb_[:, k * 128:(k + 1) * 128], XSL[x4][:, k * 128:(k + 1) * 128], IDB[:]) for k in range(KC)],
                     r=[rXSL[x4], rC], w=[pr])
                P.op("act", ACT(XF[s2], pb_[:, 0:1024], AF.Copy), r=[pr], w=[rXF[s2]])

            def stage_GU(bi):
                s2 = bi % 2
                ws = (bi // 2) % 2
                wg, wu, _ = wviews(ws)
                psg, prg = psum()
                P.op("pe", [MM(psg[:], XF[s2][:, k * 128:(k + 1) * 128], wg[:, k * 512:(k + 1) * 512], st=(k == 0), sp=(k == KC - 1))
                            for k in range(KC)], r=[rXF[s2], rWGU[ws]], w=[prg])
                psu, pru = psum()
                P.op("pe", [MM(psu[:], XF[s2][:, k * 128:(k + 1) * 128], wu[:, k * 512:(k + 1) * 512], st=(k == 0), sp=(k == KC - 1))
                            for k in range(KC)], r=[rXF[s2], rWGU[ws]], w=[pru])
                P.op("act", ACT(SS[s2], psg[:], AF.Silu), r=[prg], w=[rSS[s2]])
                P.op("dve", TT(HIDS[s2], psu[:], SS[s2], ALU.mult), r=[pru, rSS[s2]], w=[rHIDS[s2]])

            def stage_Th(bi):
                s2 = bi % 2
                ps, pr = psum()
                pb_ = ps[:].bitcast(BF16)
                P.op("pe", [TR(pb_[:, f * 128:(f + 1) * 128], HIDS[s2][:, f * 128:(f + 1) * 128], IDB[:]) for f in range(4)],
                     r=[rHIDS[s2], rC], w=[pr])
                P.op("dve", CP(HIDT[s2], pb_[:, 0:512]), r=[pr], w=[rHIDT[s2]])

            def stage_D(bi):
                s2 = bi % 2
                ws = (bi // 2) % 2
                _, _, wdn = wviews(ws)
                for half in range(2):
                    ps, pr = psum()
                    P.op("pe", [MM(ps[:], HIDT[s2][:, f * 128:(f + 1) * 128],
                                   wdn[:, f * 1024 + half * 512:f * 1024 + (half + 1) * 512], st=(f == 0), sp=(f == 3))
                                for f in range(4)], r=[rHIDT[s2], rWD[ws]], w=[pr])
                    if half == 0:
                        P.op("act", ACT(YS[s2][:, 0:512], ps[:], AF.Copy), r=[pr], w=[rYS[s2]])
                    else:
                        P.op("dve", CP(YS[s2][:, 512:1024], ps[:]), r=[pr], w=[rYS[s2]])
                P.dma("sp", ys_d[bid(bi) * 128:(bid(bi) + 1) * 128, :], YS[s2], r=[rYS[s2]], w=[], dres=rYs[s2], nowait_w=[rYs[s2]])

            for bi in range(NXS - 1):
                xload(bi)
            stage_Tx(0)
            for i in range(NSUB + 1):
                if i >= 1:
                    stage_Th(i - 1)
                if i + NXS - 1 < NSUB:
                    xload(i + NXS - 1)
                if i + 1 < NSUB:
                    stage_Tx(i + 1)
                if i < NSUB:
                    stage_GU(i)
                    if i % 2 == 1 and i // 2 + 2 < NSTEP:
                        load_wgu(i // 2 + 2)
                if i >= 1:
                    stage_D(i - 1)
                    if (i - 1) % 2 == 1 and (i - 1) // 2 + 2 < NSTEP:
                        load_wd((i - 1) // 2 + 2)
            P.barrier()

            def issue_gather(t16):
                g = t16 % NYG
                for k in range(2):
                    c = k * 16 + t16
                    P.dmaf("pool", lambda eng, g=g, c=c, k=k: eng.indirect_dma_start(
                        out=YG[k][g], out_offset=None, in_=ys_d, in_offset=IOA(ap=DIDX[:, c:c + 1], axis=0),
                        bounds_check=P.regs["xb"], oob_is_err=False),
                        r=[rYs[0], rYs[1], rIDX], w=[rYG[k][g]])
            def k1(t16):
                b = t16 % 2
                g = t16 % NYG
                P.op("act", ACT(YC[b], YG[0][g], AF.Copy, scale=GATE[:, t16:t16 + 1]), r=[rYG[0][g], rIDX], w=[rYC[b]])
                P.op("dve", STT(YC[b], YG[1][g], GATE[:, 16 + t16:17 + t16], YC[b], ALU.mult, ALU.add),
                     r=[rYG[1][g], rIDX], w=[rYC[b]])

            def k2(t16):
                b = t16 % 2
                tok = slice(t16 * 128, (t16 + 1) * 128)
                for half in range(2):
                    ps, pr = psum()
                    P.op("pe", [TR(ps[:, j * 128:(j + 1) * 128], YC[b][:, (half * 4 + j) * 128:(half * 4 + j + 1) * 128], IDF)
                                for j in range(4)], r=[rYC[b], rC], w=[pr])
                    for j in range(4):
                        dc = half * 4 + j
                        P.op("dve", STT(XT[:, dc, tok], ps[:, j * 128:(j + 1) * 128], modv(layer, 5, dc), XT[:, dc, tok],
                                        ALU.mult, ALU.add), r=[pr, rMOD], w=[rXT[t16 // 4]])

            for t16 in range(NYG - 1):
                issue_gather(t16)
            k1(0)
            for t16 in range(16):
                if t16 + NYG - 1 < 16:
                    issue_gather(t16 + NYG - 1)
                if t16 + 1 < 16:
                    k1(t16 + 1)
                k2(t16)
            P.barrier()

        def mixer_conv(layer):
            UT = v3(carve(M0, KC * 2080, BF16), KC)
            o = M0 + 33280
            WIN = [v3(carve(o + s_ * 2048, KC * 128, BF16), KC) for s_ in range(4)]
            o += 8192
            SIG = [carve(o + s_ * 2048, 512, F32) for s_ in range(2)]
            o += 4096
            DG = [v3(carve(o + s_ * 8192, 31 * 128, BF16), 31) for s_ in range(2)]
            WOUT = v3(carve(o, KC * D, BF16), KC)
            o += 16384
            YSQ = v3(carve(o, KC * 512, BF16), KC)
            o += 8192
            T1 = [carve(o + s_ * 2048, 512, F32) for s_ in range(2)]
            o += 4096
            MEAN = carve(o, 512, F32)
            MSQ = carve(o + 2048, 512, F32)
            RSTD = carve(o + 4096, 512, F32)
            o += 6144
            rUT = P.res("UT")
            rWIN = [P.res("WIN%d" % s_) for s_ in range(4)]
            rSIG = [P.res("SIG0"), P.res("SIG1")]
            rDG = [P.res("DG0"), P.res("DG1")]
            Y = HT
            P.op("pool", MS(UT[:, :, 0:32], 0.0), w=[rUT])
            for c in range(KC):
                sa, sg = (2 * c) % 4, (2 * c + 1) % 4
                P.dma("pool", WIN[sa][:], cwin_d[:, c * 128:(c + 1) * 128].rearrange("(k p) n -> p k n", p=128), w=[rWIN[sa]])
                P.dma("pool", WIN[sg][:], cwin_d[:, D + c * 128:D + (c + 1) * 128].rearrange("(k p) n -> p k n", p=128),
                      w=[rWIN[sg]])
                for tt in range(4):
                    sl = slice(tt * 512, (tt + 1) * 512)
                    s2 = (c * 4 + tt) % 2
                    psa, pra = psum()
                    P.op("pe", [MM(psa[:], WIN[sa][:, k, :], HT[:, k, sl], st=(k == 0), sp=(k == KC - 1)) for k in range(KC)],
                         r=[rWIN[sa], rHT[tt]], w=[pra])
                    psg, prg = psum()
                    P.op("pe", [MM(psg[:], WIN[sg][:, k, :], HT[:, k, sl], st=(k == 0), sp=(k == KC - 1)) for k in range(KC)],
                         r=[rWIN[sg], rHT[tt]], w=[prg])
                    P.op("act", ACT(SIG[s2], psg[:], AF.Sigmoid, bias=V("cbin", 8 + c), scale=1.0), r=[prg, rVEC], w=[rSIG[s2]])
                    P.op("dve", STT(UT[:, c, 32 + tt * 512:32 + (tt + 1) * 512], psa[:], V("cbin", c), SIG[s2], ALU.add, ALU.mult),
                         r=[pra, rSIG[s2], rVEC], w=[rUT])
            P.barrier()
            for c in range(KC):
                ds = c % 2
                P.op("dve", [TS(DG[ds][:, j, :], IDB[:], V("wdw", c * 31 + j), None, ALU.mult) for j in range(31)],
                     r=[rVEC, rC], w=[rDG[ds]])
                for tt in range(4):
                    ps, pr = psum()
                    P.op("pe", [MM(ps[:], DG[ds][:, j, :], UT[:, c, tt * 512 + j + 2: tt * 512 + j + 2 + 512], st=(j == 0), sp=(j == 30))
                                for j in range(31)], r=[rDG[ds], rUT], w=[pr])
                    P.op("act", ACT(Y[:, c, tt * 512:(tt + 1) * 512], ps[:], AF.Identity, bias=V("cbdw", c), scale=1.0),
                         r=[pr, rVEC], w=[rHT[tt]])
            P.barrier()
            rW_ = P.res("WOUT")
            P.dma("pool", WOUT[:], cwout_d.rearrange("(k p) n -> p k n", p=128), w=[rW_])
            rYSQ = P.res("YSQ")
            rST = P.res("stats")
            rT1 = [P.res("T1a"), P.res("T1b")]
            for tt in range(4):
                sl = slice(tt * 512, (tt + 1) * 512)
                P.op("act", ACT(YSQ[:], Y[:, :, sl], AF.Square), r=[rHT[tt]], w=[rYSQ])
                p1, r1 = psum()
                P.op("pe", [MM(p1[:], ONESB[:], Y[:, c, sl], st=(c == 0), sp=(c == KC - 1)) for c in range(KC)],
                     r=[rHT[tt], rC], w=[r1])
                p2, r2 = psum()
                P.op("pe", [MM(p2[:], ONESB[:], YSQ[:, c, :], st=(c == 0), sp=(c == KC - 1)) for c in range(KC)],
                     r=[rYSQ, rC], w=[r2])
                P.op("dve", TS(MEAN, p1[:], 1.0 / D, None, ALU.mult), r=[r1], w=[rST])
                P.op("dve", TT(MSQ, MEAN, MEAN, ALU.mult), r=[rST], w=[rST])
                P.op("dve", STT(RSTD, p2[:], 1.0 / D, MSQ, ALU.mult, ALU.subtract), r=[r2, rST], w=[rST])
                P.op("act", ACT(RSTD, RSTD, AF.Sqrt, bias=SMALL[:, 0:1], scale=1.0), r=[rST, rC], w=[rST])
                P.op("dve", lambda e: e.reciprocal(out=RSTD, in_=RSTD), r=[rST], w=[rST])
                for c in range(KC):
                    s2 = c % 2
                    P.op("dve", TT(T1[s2], Y[:, c, sl], MEAN, ALU.subtract), r=[rHT[tt], rST], w=[rT1[s2]])
                    P.op("pool", TT(T1[s2], T1[s2], RSTD, ALU.mult), r=[rST], w=[rT1[s2]])
                    P.op("act", ACT(Y[:, c, sl], T1[s2], AF.Silu, bias=V("clnb", c), scale=V("clng", c)),
                         r=[rT1[s2], rVEC], w=[rHT[tt]])
            for tt in range(4):
                sl = slice(tt * 512, (tt + 1) * 512)
                for dc in range(KC):
                    ps, pr = psum()
                    P.op("pe", [MM(ps[:], WOUT[:, c, dc * 128:(dc + 1) * 128], Y[:, c, sl], st=(c == 0), sp=(c == KC - 1))
                                for c in range(KC)], r=[rW_, rHT[tt]], w=[pr])
                    P.op("dve", STT(XT[:, dc, sl], ps[:], modv(layer, 2, dc), XT[:, dc, sl], ALU.mult, ALU.add),
                         r=[pr, rMOD], w=[rXT[tt]])
            P.barrier()

        def mixer_pool(layer):
            o = M0
            SA = [carve(o + s_ * 8192, S, F32) for s_ in range(2)]
            SB = [carve(o + 16384 + s_ * 8192, S, F32) for s_ in range(2)]
            PL = v3(carve(o + 32768, KC * S, BF16), KC)
            o2 = o + 32768 + 32768
            PW = carve(o2, 4 * 2 * 256, BF16).rearrange("p (g k n) -> p g k n", g=4, k=2)
            INVC = carve(o2 + 4096, 64, F32)
            T16 = [carve(o2 + 4096 + 256 + s_ * 64, 16, F32) for s_ in range(2)]
            YT = [carve(o2 + 8192 + s_ * 2048, 512, F32) for s_ in range(2)]
            GL_ = carve(o2 + 8192 + 4096, 16, F32)
            rPW = P.res("PW")
            rIN = P.res("INVC")
            rS = [P.res("S0"), P.res("S1")]
            rPL = [P.res("PL%d" % c) for c in range(KC)]
            rYT = [P.res("YT0"), P.res("YT1")]
            rGL = P.res("GL")
            rT16 = [P.res("T16a"), P.res("T16b")]
            P.dma("pool", PW, pw_d.rearrange("g (k p) n -> p g k n", p=128), w=[rPW])
            P.dma("sp", INVC, invc_d, w=[rIN])
            for c in range(KC):
                P.op("dve", TT(GL_[:, c:c + 1], modv(layer, 2, c), V("psc", c), ALU.mult), r=[rMOD, rVEC], w=[rGL])
                P.op("dve", TT(GL_[:, 8 + c:9 + c], GL_[:, c:c + 1], V("pb", c), ALU.mult), r=[rVEC], w=[rGL])
            for c in range(KC):
                gi = c // 2
                wnd = 2 << gi
                en = "pool" if c % 4 == 3 else "dve"
                s_ = 1 if en == "pool" else 0
                bufs = [SA[s_], SB[s_]]
                cur = HT[:, c, :]
                sh = 1
                bi = 0
                while sh < wnd:
                    nxt = bufs[bi]
                    P.op(en, [TT(nxt[:, sh:], cur[:, sh:], cur[:, 0:S - sh], ALU.add), CP(nxt[:, 0:sh], cur[:, 0:sh])],
                         r=[rHT[0], rHT[1], rHT[2], rHT[3]], w=[rS[s_]])
                    cur = nxt
                    bi ^= 1
                    sh *= 2
                oth = bufs[bi]
                P.op(en, TS(oth, cur, 1.0 / wnd, None, ALU.mult), r=[], w=[rS[s_]])
                P.op(en, TT(PL[:, c, :], oth, HT[:, c, :], ALU.subtract), r=[rHT[0], rHT[1], rHT[2], rHT[3], rS[s_]], w=[rPL[c]])
                P.op(en, TT(T16[s_], cur[:, 0:16], INVC[:, gi * 16:(gi + 1) * 16], ALU.mult), r=[rIN, rS[s_]], w=[rT16[s_]])
                P.op(en, TT(PL[:, c, 0:16], T16[s_], HT[:, c, 0:16], ALU.subtract), r=[rT16[s_]], w=[rPL[c]])
            i_ = 0
            for gi in range(4):
                for dn in range(2):
                    dc = gi * 2 + dn
                    for tt in range(4):
                        sl = slice(tt * 512, (tt + 1) * 512)
                        ps, pr = psum()
                        P.op("pe", [MM(ps[:], PW[:, gi, k, dn * 128:(dn + 1) * 128], PL[:, gi * 2 + k, sl], st=(k == 0), sp=(k == 1))
                                    for k in range(2)], r=[rPW, rPL[gi * 2], rPL[gi * 2 + 1]], w=[pr])
                        s2 = i_ % 2
                        i_ += 1
                        P.op("act", ACT(YT[s2], ps[:], AF.Identity, bias=GL_[:, 8 + dc:9 + dc], scale=GL_[:, dc:dc + 1]),
                             r=[pr, rGL], w=[rYT[s2]])
                        P.op("dve", TT(XT[:, dc, sl], XT[:, dc, sl], YT[s2], ALU.add), r=[rYT[s2]], w=[rXT[tt]])
            P.barrier()

        def mixer_hgrn(layer):
            o = M0
            A_ = carve(o, S, F32); o += 8192
            B_ = carve(o, S, F32); o += 8192
            C_ = carve(o, S, F32); o += 8192
            D_ = carve(o, S, F32); o += 8192
            QTb = carve(o, S, BF16); o += 4096
            KTb = carve(o, S, BF16); o += 4096
            KHT = carve(o, S, BF16); o += 4096
            KHtok = v3(carve(o, 16 * 128, BF16), 16); o += 4096
            Vt = v3(carve(o, 16 * 128, BF16), 16); o += 4096
            Gb = carve(o, S, BF16); o += 4096
            WQ = v3(carve(o, KC * 512, BF16), KC); o += 8192
            SCM = carve(o, S, F32); o += 8192
            WO = [carve(o + s_ * 2048, D, BF16) for s_ in range(2)]; o += 4096
            SF = carve(o, 128, F32); o += 512
            SBF = [carve(o + s_ * 256, 128, BF16) for s_ in range(2)]; o += 512
            SC = [carve(o + s_ * 256, 128, BF16) for s_ in range(2)]; o += 512
            LB = carve(o, 64, F32); o += 256
            RS = carve(o, 512, F32); o += 2048
            OSQ = KHT
            OG = KTb
            rA, rB, rCc, rD = P.res("A"), P.res("B"), P.res("C"), P.res("D")
            rQT, rKT, rKHT, rKHtok, rV, rG = P.res("QT"), P.res("KT"), P.res("KHT"), P.res("KHtok"), P.res("V"), P.res("G")
            rWQ, rSCM, rLB = P.res("WQ"), P.res("SCM"), P.res("LB")
            rWO = [P.res("WO0"), P.res("WO1")]
            rSF = P.res("SF")
            rSBF = [P.res("SBF0"), P.res("SBF1")]
            rSC = [P.res("SC0"), P.res("SC1")]
            rRS = P.res("RS")
            P.dma("sp", SCM, scm_d, w=[rSCM])
            EX = carve(o, 64, F32); o += 256
            for i in range(DEPTH):
                P.op("act", ACT(EX[:, i * 8:(i + 1) * 8], V("lbl%d" % i, 0, 8), AF.Exp), r=[rVEC], w=[rLB])
            P.op("dve", TT(LB[:, 24:32], EX[:, 0:8], EX[:, 8:16], ALU.add), r=[rLB], w=[rLB])
            P.op("dve", TT(LB[:, 24:32], LB[:, 24:32], EX[:, 16:24], ALU.add), r=[rLB], w=[rLB])
            P.op("dve", TT(LB[:, 24:32], LB[:, 24:32], EX[:, 24:32], ALU.add), r=[rLB], w=[rLB])
            P.op("dve", lambda e: e.reciprocal(out=LB[:, 24:32], in_=LB[:, 24:32]), r=[rLB], w=[rLB])
            P.op("dve", MS(LB[:, 0:8], 0.0), w=[rLB])
            for i in range(1, layer + 1):
                P.op("dve", TT(LB[:, 0:8], LB[:, 0:8], EX[:, i * 8:(i + 1) * 8], ALU.add), r=[rLB], w=[rLB])
            P.op("dve", TT(LB[:, 0:8], LB[:, 0:8], LB[:, 24:32], ALU.mult), r=[rLB], w=[rLB])
            P.op("dve", TS(LB[:, 8:16], LB[:, 0:8], -1.0, 1.0, ALU.mult, ALU.add), r=[rLB], w=[rLB])
            P.op("dve", TS(LB[:, 16:24], LB[:, 8:16], -1.0, None, ALU.mult), r=[rLB], w=[rLB])
            for h in range(KC):
                for part in range(4):
                    P.dma("pool", WQ[:, :, part * 128:(part + 1) * 128],
                          hwin_d[:, part * D + h * 128: part * D + (h + 1) * 128].rearrange("(k p) n -> p k n", p=128),
                          w=[rWQ])
                P.dma("pool", WO[h % 2], hwout_d[h * 128:(h + 1) * 128, :], w=[rWO[h % 2]])
                for tt in range(4):
                    sl = slice(tt * 512, (tt + 1) * 512)
                    for part, dst in ((0, "q"), (1, "f"), (3, "g")):
                        ps, pr = psum()
                        P.op("pe", [MM(ps[:], WQ[:, k, part * 128:(part + 1) * 128], HT[:, k, sl], st=(k == 0), sp=(k == KC - 1))
                                    for k in range(KC)], r=[rWQ, rHT[tt]], w=[pr])
                        if dst == "q":
                            P.op("act", ACT(D_[:, sl], ps[:], AF.Silu), r=[pr], w=[rD])
                        elif dst == "f":
                            P.op("act", ACT(A_[:, sl], ps[:], AF.Sigmoid), r=[pr], w=[rA])
                        else:
                            P.op("act", ACT(Gb[:, sl], ps[:], AF.Silu), r=[pr], w=[rG])
                for t16 in range(16):
                    ps, pr = psum()
                    P.op("pe", [MM(ps[:, 0:128], HT[:, k, t16 * 128:(t16 + 1) * 128], WQ[:, k, 256:384], st=(k == 0), sp=(k == KC - 1))
                                for k in range(KC)], r=[rWQ, rHT[t16 // 4]], w=[pr])
                    P.op("act", ACT(Vt[:, t16, :], ps[:, 0:128], AF.Copy), r=[pr], w=[rV])
                lb, oml, noml = LB[:, h:h + 1], LB[:, 8 + h:9 + h], LB[:, 16 + h:17 + h]
                P.op("dve", TS(B_, A_, oml, lb, ALU.mult, ALU.add), r=[rA, rLB], w=[rB])
                P.op("act", ACT(B_, B_, AF.Ln), r=[], w=[rB])
                P.op("act", ACT(A_, A_, AF.Identity, bias=oml, scale=noml), r=[rLB, rB], w=[rA])
                P.op("dve", lambda e: e.tensor_tensor_scan(out=C_, data0=SCM, data1=B_, initial=0.0, op0=ALU.mult, op1=ALU.add),
                     r=[rB, rSCM], w=[rCc])
                P.op("act", ACT(B_, C_, AF.Exp), r=[rCc], w=[rB])
                P.op("dve", TS(C_, C_, -1.0, 80.0, ALU.mult, ALU.min), r=[rB], w=[rCc])
                P.op("act", ACT(C_, C_, AF.Exp), r=[], w=[rCc])
                P.op("dve", TT(QTb, D_, B_, ALU.mult), r=[rD, rB], w=[rQT])
                P.op("dve", TT(A_, A_, C_, ALU.mult), r=[rCc], w=[rA])
                P.op("act", ACT(KTb, A_, AF.Copy), r=[rA], w=[rKT])
                A3 = A_.rearrange("p (n c) -> p n c", c=64)
                B3 = B_.rearrange("p (n c) -> p n c", c=64)
                K3 = KHT.rearrange("p (n c) -> p n c", c=64)
                P.op("dve", TT(K3, A3, B3[:, :, 63:64].to_broadcast([128, 32, 64]), ALU.mult), r=[rA, rB], w=[rKHT])
                for t16 in range(16):
                    ps, pr = psum()
                    pb_ = ps[:].bitcast(BF16)
                    P.op("pe", TR(pb_[:, 0:128], KHT[:, t16 * 128:(t16 + 1) * 128], IDB[:]), r=[rKHT, rC], w=[pr])
                    P.op("act", ACT(KHtok[:, t16, :], pb_[:, 0:128], AF.Copy), r=[pr], w=[rKHtok])
                P.op("dve", MS(SF, 0.0), w=[rSF])
                P.op("dve", MS(SBF[0], 0.0), w=[rSBF[0]])
                sv = 0
                for t16 in range(16):
                    tsl = slice(t16 * 128, (t16 + 1) * 128)
                    sc_i = t16 % 2
                    ps, pr = psum()
                    P.op("pe", MM(ps[:, 0:128], KTb[:, tsl], QTb[:, tsl]), r=[rKT, rQT], w=[pr])
                    P.op("dve", TT(SC[sc_i], ps[:, 0:128], MASK2, ALU.mult), r=[pr, rC], w=[rSC[sc_i]])
                    pa, pra = psum()
                    P.op("pe", [MM(pa[:, 0:64], SBF[sv], QTb[:, t16 * 128:t16 * 128 + 64], st=True, sp=False),
                                MM(pa[:, 0:64], Vt[0:64, t16, :], SC[sc_i][0:64, 0:64], st=False, sp=True)],
                         r=[rSBF[sv], rQT, rV, rSC[sc_i]], w=[pra])
                    pu, pru = psum()
                    P.op("pe", MM(pu[:, 0:128], KHtok[0:64, t16, :], Vt[0:64, t16, :]), r=[rKHtok, rV], w=[pru])
                    P.op("dve", STT(SF, SF, B_[:, t16 * 128 + 63:t16 * 128 + 64], pu[:, 0:128], ALU.mult, ALU.add),
                         r=[pru, rB], w=[rSF])
                    P.op("act", ACT(SBF[1 - sv], SF, AF.Copy), r=[rSF], w=[rSBF[1 - sv]])
                    sv = 1 - sv
                    pb2, prb = psum()
                    P.op("pe", [MM(pb2[:, 0:64], SBF[sv], QTb[:, t16 * 128 + 64:t16 * 128 + 128], st=True, sp=False),
                                MM(pb2[:, 0:64], Vt[:, t16, :], SC[sc_i][:, 64:128], st=False, sp=True)],
                         r=[rSBF[sv], rQT, rV, rSC[sc_i]], w=[prb])
                    pu2, pru2 = psum()
                    P.op("pe", MM(pu2[:, 0:128], KHtok[64:128, t16, :], Vt[64:128, t16, :]), r=[rKHtok, rV], w=[pru2])
                    P.op("dve", STT(SF, SF, B_[:, t16 * 128 + 127:t16 * 128 + 128], pu2[:, 0:128], ALU.mult, ALU.add),
                         r=[pru2, rB], w=[rSF])
                    P.op("act", ACT(SBF[1 - sv], SF, AF.Copy), r=[rSF], w=[rSBF[1 - sv]])
                    sv = 1 - sv
                    P.op("act", ACT(D_[:, t16 * 128:t16 * 128 + 64], pa[:, 0:64], AF.Copy), r=[pra, rQT], w=[rD])
                    P.op("act", ACT(D_[:, t16 * 128 + 64:t16 * 128 + 128], pb2[:, 0:64], AF.Copy), r=[prb], w=[rD])
                for tt in range(4):
                    sl = slice(tt * 512, (tt + 1) * 512)
                    P.op("act", ACT(OSQ[:, sl], D_[:, sl], AF.Square), r=[rD, rKHtok], w=[rKHT])
                    ps, pr = psum()
                    P.op("pe", MM(ps[:], ONESB[:], OSQ[:, sl]), r=[rKHT, rC], w=[pr])
                    P.op("act", ACT(RS, ps[:], AF.Sqrt, bias=SMALL[:, 0:1], scale=1.0 / 128), r=[pr, rC], w=[rRS])
                    P.op("dve", lambda e: e.reciprocal(out=RS, in_=RS), r=[rRS], w=[rRS])
                    P.op("dve", STT(D_[:, sl], D_[:, sl], V("hng", h), RS, ALU.mult, ALU.mult), r=[rRS, rVEC], w=[rD])
                    P.op("pool", TT(OG[:, sl], D_[:, sl], Gb[:, sl], ALU.mult), r=[rD, rG, rSC[0], rSC[1]], w=[rKT])
                    for dc in range(KC):
                        ps2, pr2 = psum()
                        P.op("pe", MM(ps2[:], WO[h % 2][:, dc * 128:(dc + 1) * 128], OG[:, sl]), r=[rWO[h % 2], rKT], w=[pr2])
                        P.op("dve", STT(XT[:, dc, sl], ps2[:], modv(layer, 2, dc), XT[:, dc, sl], ALU.mult, ALU.add),
                             r=[pr2, rMOD], w=[rXT[tt]])
            P.barrier()

        def mixer_sb(layer):
            scale = 64 ** -0.5
            o = L0
            QT = [carve(o + s_ * 4096, S, BF16) for s_ in range(2)]; o += 8192
            KT = [carve(o + s_ * 4096, S, BF16) for s_ in range(2)]; o += 8192
            VP = [v3(carve(o + hp * 4096, 16 * 128, BF16), 16) for hp in range(2)]; o += 8192
            OT = [carve(o + s_ * 4096, S, BF16) for s_ in range(2)]; o += 8192
            WQ = [v3(carve(o + s_ * 6144, KC * 384, BF16), KC) for s_ in range(2)]; o += 12288
            WO = [carve(o + s_ * 2048, D, BF16) for s_ in range(2)]; o += 4096
            SP = [carve(o + s_ * 8192, S, F32) for s_ in range(2)]; o += 16384
            A_ = [carve(o + s_ * 8192, S, F32) for s_ in range(2)]; o += 16384
            C_ = carve(o, S, F32); o += 8192
            W_ = [carve(o + s_ * 4096, S, BF16) for s_ in range(2)]; o += 8192
            WT = [v3(carve(o + s_ * 4096, 16 * 128, BF16), 16) for s_ in range(2)]; o += 8192
            assert o <= ARENA_BYTES, o
            NT = [SMALL[:, 8:9], SMALL[:, 9:10]]
            two = lambda n: [P.res(n + "0"), P.res(n + "1")]
            rQT, rKT, rOT, rWQ, rWO = two("QT"), two("KT"), two("OT"), two("WQ"), two("WO")
            rVP = P.res("VP")
            rSP = two("SP")
            rCc = P.res("C")
            rA, rW, rWT, rNT = two("A"), two("W"), two("WT"), two("NT")
            rNEG = P.res("NEGM")
            P.op("dve", TS(NEGM[:], CST[:, 128:256], -1.0, 30000.0, ALU.add, ALU.mult), r=[rC], w=[rNEG])
            po, pro = banks[7], bres[7]
            P.op("pool", MS(VP[0][:, :, 64:128], 0.0), w=[rVP])
            P.op("pool", MS(VP[1][:, :, 0:64], 0.0), w=[rVP])
            zb = [0]
            tb = [0]

            def zbank():
                i = zb[0]
                zb[0] = (i + 1) % 4
                return banks[i], bres[i]

            def tbank():
                i = 4 + tb[0]
                tb[0] = (tb[0] + 1) % 3
                return banks[i], bres[i]

            def load_weights(c):
                s_ = c % 2
                for part in range(3):
                    P.dma("pool", WQ[s_][:, :, part * 128:(part + 1) * 128],
                          sqkv_d[:, part * D + c * 128: part * D + (c + 1) * 128].rearrange("(k p) n -> p k n", p=128),
                          w=[rWQ[s_]])
                P.dma("pool", WO[s_], swout_d[c * 128:(c + 1) * 128, :], w=[rWO[s_]])

            def qk_piece(c, m):
                s_ = c % 2
                tt, part = m // 2, m % 2
                sl = slice(tt * 512, (tt + 1) * 512)
                dst, rd = ((QT[s_], rQT[s_]), (KT[s_], rKT[s_]))[part]
                ps, pr = tbank()
                P.op("pe", [MM(ps[:], WQ[s_][:, k, part * 128:(part + 1) * 128], HT[:, k, sl], st=(k == 0), sp=(k == KC - 1))
                            for k in range(KC)], r=[rWQ[s_], rHT[tt]], w=[pr])
                P.op("dve", CP(dst[:, sl], ps[:]), r=[pr], w=[rd])

            def qk_proj(c):
                for m in range(8):
                    qk_piece(c, m)

            def v_proj(c):
                s_ = c % 2
                for t16 in range(16):
                    ps, pr = tbank()
                    P.op("pe", [MM(ps[:, 0:128], HT[:, k, t16 * 128:(t16 + 1) * 128], WQ[s_][:, k, 256:384], st=(k == 0), sp=(k == KC - 1))
                                for k in range(KC)], r=[rWQ[s_], rHT[t16 // 4]], w=[pr])
                    P.op("dve", [CP(VP[0][:, t16, 0:64], ps[:, 0:64]), CP(VP[1][:, t16, 64:128], ps[:, 64:128])], r=[pr], w=[rVP])

            def stageA(u, c, qb, hp):
                s_ = c % 2
                b = u % 2
                hs = slice(hp * 64, (hp + 1) * 64)
                tq = slice(qb * 128, (qb + 1) * 128)
                nk = (qb + 1) * 128
                nch = (nk + 511) // 512
                zs = []
                for ch in range(nch):
                    w = min(512, nk - ch * 512)
                    cs = slice(ch * 512, ch * 512 + w)
                    pz, prz = zbank()
                    fns = [MM(pz[:, 0:w], QT[s_][hs, tq], KT[s_][hs, cs], st=True, sp=(ch != nch - 1))]
                    if ch == nch - 1:
                        fns.append(MM(pz[:, w - 128:w], IDB[:], NEGM[:], st=False, sp=True))
                    P.op("pe", fns, r=[rQT[s_], rKT[s_], rNEG, rC], w=[prz])
                    P.op("act", ACT(SP[b][:, cs], pz[:, 0:w], AF.Exp, scale=scale), r=[prz], w=[rSP[b]])
                    zs.append((pz, prz, w, cs))
                P.op("act", ACT(SP[b][:, 0:nk], SP[b][:, 0:nk], AF.Ln, bias=SMALL[:, 1:2], scale=1.0), r=[rC], w=[rSP[b]])
                for pz, prz, w, cs in zs:
                    P.op("dve", STT(A_[b][:, cs], pz[:, 0:w], scale, SP[b][:, cs], ALU.mult, ALU.subtract), r=[prz, rSP[b]], w=[rA[b]])

            def stageB(u, c, qb, hp):
                b = u % 2
                nk = (qb + 1) * 128
                e_scan, e_add = ("dve", "pool")
                P.op(e_scan, lambda e, b=b, nk=nk: e.tensor_tensor_scan(
                    out=C_[:, 0:nk], data0=SMALL[:, 1:2].to_broadcast([128, nk]), data1=SP[b][:, 0:nk], initial=0.0,
                    op0=ALU.mult, op1=ALU.add), r=[rSP[b], rC], w=[rCc])
                P.op(e_scan, TS(NT[b], C_[:, nk - 1:nk], -1.0, None, ALU.mult), r=[rCc], w=[rNT[b]])
                P.op(e_add, TT(A_[b][:, 0:nk], A_[b][:, 0:nk], C_[:, 0:nk], ALU.add), r=[rCc], w=[rA[b]])

            def stageC1(u, c, qb, hp):
                b = u % 2
                nk = (qb + 1) * 128
                P.op("act", ACT(W_[b][:, 0:nk], A_[b][:, 0:nk], AF.Exp, bias=NT[b], scale=1.0), r=[rA[b], rNT[b]], w=[rW[b]])

            def stageC(u, c, qb, hp):
                s_ = c % 2
                b = u % 2
                tq = slice(qb * 128, (qb + 1) * 128)
                nk = (qb + 1) * 128
                nb_ = qb + 1
                for g0 in range(0, nb_, 8):
                    n = min(8, nb_ - g0)
                    pt, prt = tbank()
                    ptb = pt[:].bitcast(BF16)
                    P.op("pe", [TR(ptb[:, j * 128:(j + 1) * 128], W_[b][:, (g0 + j) * 128:(g0 + j + 1) * 128], IDB[:]) for j in range(n)],
                         r=[rW[b], rC], w=[prt])
                    dst = WT[b][:, g0:g0 + n, :].rearrange("p a b -> p (a b)")
                    if g0 == 0:
                        P.op("act", ACT(dst, ptb[:, 0:n * 128], AF.Copy), r=[prt], w=[rWT[b]])
                    else:
                        P.op("dve", CP(dst, ptb[:, 0:n * 128]), r=[prt], w=[rWT[b]])
                pcol = slice((qb % 4) * 128, (qb % 4 + 1) * 128)
                P.op("pe", [MM(po[:, pcol], VP[hp][:, jb, :], WT[b][:, jb, :], st=(hp == 0 and jb == 0), sp=(hp == 1 and jb == qb))
                            for jb in range(nb_)], r=[rVP, rWT[b]], w=[pro])
                if hp == 1:
                    P.op("dve", CP(OT[s_][:, tq], po[:, pcol]), r=[pro], w=[rOT[s_]])
                    if qb % 4 == 3:
                        I = qb // 4
                        qsl = slice(I * 512, (I + 1) * 512)
                        for dc in range(KC):
                            ps2, pr2 = tbank()
                            P.op("pe", MM(ps2[:], WO[s_][:, dc * 128:(dc + 1) * 128], OT[s_][:, qsl]), r=[rWO[s_], rOT[s_]], w=[pr2])
                            P.op("dve", STT(XT[:, dc, qsl], ps2[:], modv(layer, 2, dc), XT[:, dc, qsl], ALU.mult, ALU.add),
                                 r=[pr2, rMOD], w=[rXT[I]])

            units = [(c, qb, hp) for c in range(KC) for qb in range(16) for hp in range(2)]
            load_weights(0)
            load_weights(1)
            nu = len(units)
            for i in range(nu + 2):
                if 2 <= i:
                    stageC1(i - 2, *units[i - 2])
                if 1 <= i <= nu:
                    stageB(i - 1, *units[i - 1])
                if i < nu:
                    c, qb, hp = units[i]
                    if qb == 0 and hp == 0 and c == 0:
                        qk_proj(c)
                    stageA(i, c, qb, hp)
                    j = qb * 2 + hp
                    if c + 1 < KC and j % 4 == 3:
                        qk_piece(c + 1, j // 4)
                if 2 <= i:
                    c, qb, hp = units[i - 2]
                    if qb == 0 and hp == 0:
                        v_proj(c)
                    stageC(i - 2, c, qb, hp)
                    if qb == 15 and hp == 1 and c + 2 < KC:
                        load_weights(c + 2)
            P.barrier()

        b7 = [0]

        def psum7():
            i = b7[0]
            b7[0] = (i + 1) % 7
            return banks[i], bres[i]

        mixers = [mixer_conv, mixer_hgrn, mixer_pool, mixer_sb]
        for i in range(DEPTH):
            if ("mix%d" % i) in phases:
                norm_phase(lambda c, i=i: DER[:, i * 16 + c:i * 16 + c + 1], lambda c, i=i: modv(i, 0, c), "mix")
                P.barrier()
                mixers[i](i)
            if ("hdump%d" % i) in phases:
                norm_phase(lambda c, i=i: DER[:, i * 16 + c:i * 16 + c + 1], lambda c, i=i: modv(i, 0, c), "mix")
                P.barrier()
                for tt in range(4):
                    P.op("act", ACT(XT[:, :, tt * 512:(tt + 1) * 512], HT[:, :, tt * 512:(tt + 1) * 512], AF.Copy),
                         r=[rHT[tt]], w=[rXT[tt]])
                P.barrier()
            if ("fdump%d" % i) in phases:
                LOG, rLOG, _ = norm_phase(lambda c, i=i: DER[:, i * 16 + 8 + c:i * 16 + 9 + c], lambda c, i=i: modv(i, 3, c),
                                          "ffn", layer=i)
                P.barrier()
                for tt in range(4):
                    P.op("act", ACT(XT[:, :, tt * 512:(tt + 1) * 512], HT[:, :, tt * 512:(tt + 1) * 512], AF.Copy),
                         r=[rHT[tt]], w=[rXT[tt]])
                P.op("act", ACT(XT[:, 0, 0:576], LOG[:].rearrange("p a b -> p (a b)"), AF.Copy), r=[rLOG], w=[rXT[0]])
                P.barrier()
            if ("ffn%d" % i) in phases:
                LOG, rLOG, _ = norm_phase(lambda c, i=i: DER[:, i * 16 + 8 + c:i * 16 + 9 + c], lambda c, i=i: modv(i, 3, c),
                                          "ffn", layer=i)
                moe_phase(i, LOG, rLOG)
        if dbg:
            rOUT = P.res("OUT")
            for i in range(4):
                P.dma("sp", out_d[:, :, i * 512:(i + 1) * 512], XT[:, :, i * 512:(i + 1) * 512], r=[rXT[i]], w=[], dres=rOUT)
        else:
            norm_phase(lambda c: V("fin", c), None, "final")
        P.barrier()
        block = es.enter_context(nc.Block())
        P.replay(block)
    return nc


ALL_PHASES = ["mods"] + [p for i in range(DEPTH) for p in ("mix%d" % i, "ffn%d" % i)]


def _consts():
    ident = np.eye(128, dtype=np.float32)
    j = np.arange(128)[:, None]
    s = np.arange(128)[None, :]
    tri = (j > s).astype(np.float32)
    mask2 = ((j <= s) & ((j // 64) == (s // 64))).astype(np.float32)
    cA = np.concatenate([ident, tri, mask2], axis=1)
    t = np.arange(512)[None, :]
    sbm = np.concatenate([((r * 128 + j) < t).astype(np.float32) for r in range(4)], axis=1)
    scm = np.broadcast_to((np.arange(S) % 64 != 0).astype(np.float32)[None, :], (128, S)).copy()
    invc = np.zeros((128, 64), np.float32)
    for gi, w in enumerate((2, 4, 8, 16)):
        invc[:, gi * 16:(gi + 1) * 16] = 1.0 / np.minimum(np.arange(16) + 1, w)
    cB = np.zeros((128, NCB), np.float32)
    cB[:, 0:16] = 256.0 * np.arange(16)[None, :]
    cB[:, 16:80] = np.arange(64)[None, :]
    cB[:, 80] = np.arange(128)
    return cA, sbm, scm, invc, cB


_CACHE = {}


def kernel(**inp):
    return run(inp, ALL_PHASES, False)


def run(inp, phases, dbg, cores=8):
    f = lambda a: np.ascontiguousarray(np.asarray(a, np.float32))
    key = (tuple(phases), dbg)
    if key not in _CACHE:
        _CACHE[key] = build_program(phases, dbg)
    nc = _CACHE[key]
    cA, sbm, scm, invc, cB = _consts()
    x = f(inp["x"])
    shared = {
        "cA": cA, "sbmask": sbm, "scanmask": scm, "invc": invc, "cB": cB,
        "ada_w": f(inp["ada_w"]),
        "conv_w_in": f(inp["conv_w_in"][0]), "conv_w_out": f(inp["conv_w_out"][0]),
        "hgrn_w_in": f(inp["hgrn_w_in"][0]), "hgrn_w_out": f(inp["hgrn_w_out"][0]),
        "pool_w": f(inp["pool_w"][0]),
        "sb_w_qkv": f(inp["sb_w_qkv"][0]), "sb_w_out": f(inp["sb_w_out"][0]),
    }
    shared["wgL"] = np.ascontiguousarray(
        f(inp["moe_w_gate"]).reshape(DEPTH, NEXP, KC, 128, FH).transpose(0, 1, 3, 2, 4)).reshape(DEPTH * NEXP * 128, KC * FH)
    shared["wuL"] = np.ascontiguousarray(
        f(inp["moe_w_up"]).reshape(DEPTH, NEXP, KC, 128, FH).transpose(0, 1, 3, 2, 4)).reshape(DEPTH * NEXP * 128, KC * FH)
    shared["wdL"] = np.ascontiguousarray(
        f(inp["moe_w_down"]).reshape(DEPTH, NEXP, 4, 128, D).transpose(0, 1, 3, 2, 4)).reshape(DEPTH * NEXP * 128, 4 * D)
    wr = np.concatenate([f(inp["moe_w_rg"]), f(inp["moe_w_re"])], axis=2)
    shared["wr"] = np.ascontiguousarray(wr.reshape(DEPTH, KC, 128, 36).transpose(2, 0, 1, 3))
    br = np.concatenate([f(inp["moe_b_rg"]), f(inp["moe_b_re"])], axis=1).reshape(1, DEPTH * 36)
    shared["br"] = np.ascontiguousarray(np.broadcast_to(br, (128, DEPTH * 36)))
    in_maps = []
    for b in range(cores):
        vec = np.zeros((128, NVEC), np.float32)

        def put(name, arr):
            a = _fm(arr)
            vec[:, VOFF[name]:VOFF[name] + a.shape[1]] = a
        for i in range(DEPTH):
            put("gmix%d" % i, inp["norm_mix_g"][i])
            put("gffn%d" % i, inp["norm_ffn_g"][i])
            put("adab%d" % i, inp["ada_b"][i])
            put("lbl%d" % i, inp["hgrn_lb_logits"][i])
        put("fin", inp["final_g"])
        put("cbin", inp["conv_b_in"][0])
        put("cbdw", inp["conv_b_dw"][0])
        put("clng", inp["conv_ln_g"][0])
        put("clnb", inp["conv_ln_b"][0])
        put("hng", inp["hgrn_norm_g"][0])
        put("pb", inp["pool_b"][0])
        put("psc", inp["pool_scale"][0])
        wdw = f(inp["conv_w_dw"][0])
        vec[:, VOFF["wdw"]:VOFF["wdw"] + 248] = wdw.reshape(31, KC, 128).transpose(2, 1, 0).reshape(128, 248)
        put("c", inp["c"][b])
        m = dict(shared)
        m["vecs"] = vec
        m["xT"] = np.ascontiguousarray(x[b].T.reshape(KC, 128, S).transpose(1, 0, 2))
        in_maps.append(m)
    res = run_bass_kernel_spmd(nc, in_maps, core_ids=list(range(cores)))
    outs = []
    for b in range(cores):
        oT = res.results[b]["outT"]
        outs.append(np.ascontiguousarray(oT.transpose(1, 0, 2).reshape(D, S).T))
    return np.stack(outs, axis=0).astype(np.float32)
```
